# Optimizing a Trainium2 kernel written in Bass

```python
import math
import jax, jax.numpy as jnp
from jax import lax
import numpy as np

D_MODEL = 1024
BATCH = 8
SEQ = 4096
DEPTH = 1

N_ATT_HEADS = 4
ATT_HEAD_DIM = 64
ATT_V_DIM = 2 * ATT_HEAD_DIM
QK_WIDTH = N_ATT_HEADS * 2 * ATT_HEAD_DIM
ATT_WIDTH = N_ATT_HEADS * ATT_V_DIM
Q_BLOCK = 128
ROPE_THETA = 10000.0
SSM_WIDTH = D_MODEL - ATT_WIDTH
SSM_GROUP = 16
SSM_GROUPS = SSM_WIDTH // SSM_GROUP
SSM_STATE = 64
DT_MIN = 1e-3
DT_MAX = 1e-1
MIX_WIDTH = ATT_WIDTH + SSM_WIDTH
IN_WIDTH = 2 * QK_WIDTH + ATT_WIDTH + SSM_WIDTH
N_EXPERTS = 256
TOP_K = 8
N_GROUPS = 8
TOPK_GROUPS = 4
EXPERT_DIM = 256
SHARED_DIM = 256
ROUTED_SCALE = 2.5
MOE_BLOCK = 128
EPS = 1e-6

kernel_name = 'hymba_diffattn_s5_moe_adaln'


def rms_norm(t, g):
    tf = t.astype(jnp.float32)
    y = tf * lax.rsqrt(jnp.mean(tf * tf, axis=-1, keepdims=True) + EPS)
    return (y * g.astype(jnp.float32)).astype(t.dtype)


def modulate(h, shift, scale):
    return h * (1.0 + scale[:, None, :]) + shift[:, None, :]


def rope_tables(seq_len):
    inv_freq = 1.0 / (ROPE_THETA ** (jnp.arange(0, ATT_HEAD_DIM, 2, dtype=jnp.float32) / ATT_HEAD_DIM))
    ang = jnp.arange(seq_len, dtype=jnp.float32)[:, None] * inv_freq[None, :]
    return jnp.cos(ang)[None, :, None, None, :], jnp.sin(ang)[None, :, None, None, :]


def apply_rope(t, cos, sin):
    t = t.astype(jnp.float32)
    t1, t2 = jnp.split(t, 2, axis=-1)
    return jnp.concatenate([t1 * cos - t2 * sin, t1 * sin + t2 * cos], axis=-1)


def lambda_init(layer):
    return 0.8 - 0.6 * math.exp(-0.3 * layer)


def diff_attention(q, k, v, q_g, k_g, lq1, lk1, lq2, lk2, sub_g, cos, sin, lam_init):
    bsz, seq_len, _ = q.shape
    out_dtype = q.dtype
    q = apply_rope(rms_norm(q.reshape(bsz, seq_len, N_ATT_HEADS, 2, ATT_HEAD_DIM), q_g), cos, sin)
    k = apply_rope(rms_norm(k.reshape(bsz, seq_len, N_ATT_HEADS, 2, ATT_HEAD_DIM), k_g), cos, sin)
    v = v.reshape(bsz, seq_len, N_ATT_HEADS, ATT_V_DIM).astype(jnp.float32)
    f32 = jnp.float32
    lam = (jnp.exp(jnp.sum(lq1.astype(f32) * lk1.astype(f32)))
           - jnp.exp(jnp.sum(lq2.astype(f32) * lk2.astype(f32))) + lam_init)
    scale = ATT_HEAD_DIM ** -0.5
    k_pos = jnp.arange(seq_len)

    def one_block(i):
        q_blk = lax.dynamic_slice_in_dim(q, i * Q_BLOCK, Q_BLOCK, axis=1)
        s = jnp.einsum('bqhcd,bkhcd->bhcqk', q_blk, k) * scale
        q_pos = i * Q_BLOCK + jnp.arange(Q_BLOCK)
        causal = k_pos[None, :] <= q_pos[:, None]
        p = jax.nn.softmax(jnp.where(causal, s, -jnp.inf), axis=-1)
        w = p[:, :, 0] - lam * p[:, :, 1]
        return jnp.einsum('bhqk,bkhe->bqhe', w, v)

    o = lax.map(one_block, jnp.arange(seq_len // Q_BLOCK))
    o = jnp.moveaxis(o, 0, 1).reshape(bsz, seq_len, N_ATT_HEADS, ATT_V_DIM)
    o = rms_norm(o, sub_g) * (1.0 - lam_init)
    return o.reshape(bsz, seq_len, ATT_WIDTH).astype(out_dtype)


def complex_scan_combine(earlier, later):
    a1r, a1i, b1r, b1i = earlier
    a2r, a2i, b2r, b2i = later
    return (a2r * a1r - a2i * a1i,
            a2r * a1i + a2i * a1r,
            a2r * b1r - a2i * b1i + b2r,
            a2r * b1i + a2i * b1r + b2i)


def s5_branch(u, a_re, a_im, log_dt, b_re, b_im, c_re, c_im, d_skip, w_glu, norm_g):
    bsz, seq_len, _ = u.shape
    f32 = jnp.float32
    uf = u.astype(f32).reshape(bsz, seq_len, SSM_GROUPS, SSM_GROUP)
    a_re = a_re.astype(f32)
    a_im = a_im.astype(f32)
    dt = jnp.exp(log_dt.astype(f32))[:, None]
    mag = jnp.exp(a_re * dt)
    abar_re = mag * jnp.cos(a_im * dt)
    abar_im = mag * jnp.sin(a_im * dt)
    den = a_re * a_re + a_im * a_im
    nr = abar_re - 1.0
    f_re = ((nr * a_re + abar_im * a_im) / den)[..., None]
    f_im = ((abar_im * a_re - nr * a_im) / den)[..., None]
    b_re = b_re.astype(f32)
    b_im = b_im.astype(f32)
    bb_re = f_re * b_re - f_im * b_im
    bb_im = f_re * b_im + f_im * b_re
    bu_re = jnp.einsum('blgc,gpc->blgp', uf, bb_re)
    bu_im = jnp.einsum('blgc,gpc->blgp', uf, bb_im)
    a_seq_re = jnp.broadcast_to(abar_re, (1, seq_len, SSM_GROUPS, SSM_STATE))
    a_seq_im = jnp.broadcast_to(abar_im, (1, seq_len, SSM_GROUPS, SSM_STATE))
    _, _, s_re, s_im = lax.associative_scan(complex_scan_combine, (a_seq_re, a_seq_im, bu_re, bu_im), axis=1)
    y = (jnp.einsum('blgp,gcp->blgc', s_re, c_re.astype(f32))
         - jnp.einsum('blgp,gcp->blgc', s_im, c_im.astype(f32))
         + d_skip.astype(f32) * uf)
    y = jax.nn.gelu(y.reshape(bsz, seq_len, SSM_WIDTH))
    y = y * jax.nn.sigmoid(y @ w_glu.astype(f32))
    return rms_norm(y, norm_g).astype(u.dtype)


def swiglu(t, w_g, w_u, w_d):
    return (jax.nn.silu(t @ w_g) * (t @ w_u)) @ w_d


def route(hf, w_router, r_bias):
    n_tok = hf.shape[0]
    per_group = N_EXPERTS // N_GROUPS
    s = jax.nn.sigmoid((hf @ w_router).astype(jnp.float32))
    sb = s + r_bias.astype(jnp.float32)
    grp_score = lax.top_k(sb.reshape(n_tok, N_GROUPS, per_group), 2)[0].sum(axis=-1)
    _, g_idx = lax.top_k(grp_score, TOPK_GROUPS)
    g_mask = jax.nn.one_hot(g_idx, N_GROUPS, dtype=jnp.float32).sum(axis=-2) > 0
    e_mask = jnp.repeat(g_mask, per_group, axis=-1)
    _, e_idx = lax.top_k(jnp.where(e_mask, sb, -jnp.inf), TOP_K)
    w = jnp.take_along_axis(s, e_idx, axis=-1)
    w = w / jnp.sum(w, axis=-1, keepdims=True) * ROUTED_SCALE
    return e_idx, w


def routed_experts(hf, e_idx, gate_w, w_g, w_u, w_d):
    n_tok = hf.shape[0]
    n_assign = n_tok * TOP_K
    n_blocks = (n_assign + N_EXPERTS * (MOE_BLOCK - 1) + MOE_BLOCK - 1) // MOE_BLOCK
    e_flat = e_idx.reshape(n_assign)
    t_flat = jnp.repeat(jnp.arange(n_tok, dtype=jnp.int32), TOP_K)
    g_flat = gate_w.reshape(n_assign).astype(hf.dtype)
    order = jnp.argsort(e_flat)
    e_sorted = e_flat[order]
    counts = jnp.bincount(e_flat, length=N_EXPERTS)
    starts = jnp.cumsum(counts) - counts
    padded = (counts + MOE_BLOCK - 1) // MOE_BLOCK * MOE_BLOCK
    pad_ends = jnp.cumsum(padded)
    pad_starts = pad_ends - padded
    dest = pad_starts[e_sorted] + jnp.arange(n_assign, dtype=jnp.int32) - starts[e_sorted]
    slot_tok = jnp.zeros((n_blocks * MOE_BLOCK,), jnp.int32).at[dest].set(t_flat[order])
    slot_gate = jnp.zeros((n_blocks * MOE_BLOCK,), hf.dtype).at[dest].set(g_flat[order])
    block_start = jnp.arange(n_blocks, dtype=jnp.int32) * MOE_BLOCK
    block_expert = jnp.minimum(jnp.searchsorted(pad_ends, block_start, side='right'), N_EXPERTS - 1)

    def one_block(acc, blk):
        tok, gate, e = blk
        yb = swiglu(hf[tok], w_g[e], w_u[e], w_d[e]) * gate[:, None]
        return acc.at[tok].add(yb), None

    out, _ = lax.scan(one_block, jnp.zeros_like(hf),
                      (slot_tok.reshape(n_blocks, MOE_BLOCK), slot_gate.reshape(n_blocks, MOE_BLOCK), block_expert))
    return out


def setup_inputs(seed: int = 0) -> dict:
    key = jax.random.key(seed)
    ks = jax.random.split(key, 33)
    nrm = jax.random.normal
    L, D = DEPTH, D_MODEL
    G, P, C = SSM_GROUPS, SSM_STATE, SSM_GROUP
    E, DE, DS = N_EXPERTS, EXPERT_DIM, SHARED_DIM
    x = nrm(ks[0], (BATCH, SEQ, D), jnp.float32)
    c = nrm(ks[1], (BATCH, D), jnp.float32)
    norm1_g = 1.0 + 0.02 * nrm(ks[2], (L, D))
    norm2_g = 1.0 + 0.02 * nrm(ks[3], (L, D))
    w_ada = 0.5 * D ** -0.5 * nrm(ks[4], (L, D, 6 * D))
    b_ada = 0.02 * nrm(ks[5], (L, 6 * D))
    w_in = D ** -0.5 * nrm(ks[6], (L, D, IN_WIDTH))
    q_norm_g = 1.0 + 0.02 * nrm(ks[7], (L, ATT_HEAD_DIM))
    k_norm_g = 1.0 + 0.02 * nrm(ks[8], (L, ATT_HEAD_DIM))
    lambda_q1 = 0.1 * nrm(ks[9], (L, ATT_HEAD_DIM))
    lambda_k1 = 0.1 * nrm(ks[10], (L, ATT_HEAD_DIM))
    lambda_q2 = 0.1 * nrm(ks[11], (L, ATT_HEAD_DIM))
    lambda_k2 = 0.1 * nrm(ks[12], (L, ATT_HEAD_DIM))
    subln_g = 1.0 + 0.02 * nrm(ks[13], (L, ATT_V_DIM))
    ssm_a_re = -0.5 * (1.0 + 0.05 * jax.random.uniform(ks[14], (L, G, P), minval=-1.0, maxval=1.0))
    ssm_a_im = math.pi * jnp.arange(P, dtype=jnp.float32) + 0.01 * nrm(ks[15], (L, G, P))
    ssm_log_dt = jax.random.uniform(ks[16], (L, G), minval=math.log(DT_MIN), maxval=math.log(DT_MAX))
    ssm_b_re = (2 * C) ** -0.5 * nrm(ks[17], (L, G, P, C))
    ssm_b_im = (2 * C) ** -0.5 * nrm(ks[18], (L, G, P, C))
    ssm_c_re = (2 * P) ** -0.5 * nrm(ks[19], (L, G, C, P))
    ssm_c_im = (2 * P) ** -0.5 * nrm(ks[20], (L, G, C, P))
    ssm_d = nrm(ks[21], (L, G, C))
    w_glu = SSM_WIDTH ** -0.5 * nrm(ks[22], (L, SSM_WIDTH, SSM_WIDTH))
    ssm_norm_g = 1.0 + 0.02 * nrm(ks[23], (L, SSM_WIDTH))
    w_out = MIX_WIDTH ** -0.5 * nrm(ks[24], (L, MIX_WIDTH, D))
    w_router = D ** -0.5 * nrm(ks[25], (L, D, E))
    router_bias = 0.01 * nrm(ks[26], (L, E))
    w_gate_e = D ** -0.5 * nrm(ks[27], (L, E, D, DE))
    w_up_e = D ** -0.5 * nrm(ks[28], (L, E, D, DE))
    w_down_e = DE ** -0.5 * nrm(ks[29], (L, E, DE, D))
    w_gate_s = D ** -0.5 * nrm(ks[30], (L, D, DS))
    w_up_s = D ** -0.5 * nrm(ks[31], (L, D, DS))
    w_down_s = DS ** -0.5 * nrm(ks[32], (L, DS, D))
    return {'x': x, 'c': c, 'norm1_g': norm1_g, 'norm2_g': norm2_g, 'w_ada': w_ada, 'b_ada': b_ada,
            'w_in': w_in, 'q_norm_g': q_norm_g, 'k_norm_g': k_norm_g,
            'lambda_q1': lambda_q1, 'lambda_k1': lambda_k1, 'lambda_q2': lambda_q2, 'lambda_k2': lambda_k2,
            'subln_g': subln_g, 'ssm_a_re': ssm_a_re, 'ssm_a_im': ssm_a_im, 'ssm_log_dt': ssm_log_dt,
            'ssm_b_re': ssm_b_re, 'ssm_b_im': ssm_b_im, 'ssm_c_re': ssm_c_re, 'ssm_c_im': ssm_c_im,
            'ssm_d': ssm_d, 'w_glu': w_glu, 'ssm_norm_g': ssm_norm_g, 'w_out': w_out,
            'w_router': w_router, 'router_bias': router_bias,
            'w_gate_e': w_gate_e, 'w_up_e': w_up_e, 'w_down_e': w_down_e,
            'w_gate_s': w_gate_s, 'w_up_s': w_up_s, 'w_down_s': w_down_s}


def reference(x, c, norm1_g, norm2_g, w_ada, b_ada, w_in, q_norm_g, k_norm_g,
              lambda_q1, lambda_k1, lambda_q2, lambda_k2, subln_g,
              ssm_a_re, ssm_a_im, ssm_log_dt, ssm_b_re, ssm_b_im, ssm_c_re, ssm_c_im,
              ssm_d, w_glu, ssm_norm_g, w_out, w_router, router_bias,
              w_gate_e, w_up_e, w_down_e, w_gate_s, w_up_s, w_down_s):
    bsz, seq_len, _ = x.shape
    cos, sin = rope_tables(seq_len)
    split_at = [QK_WIDTH, 2 * QK_WIDTH, 2 * QK_WIDTH + ATT_WIDTH]
    for layer in range(DEPTH):
        mod = jax.nn.silu(c) @ w_ada[layer] + b_ada[layer]
        sh1, sc1, g1, sh2, sc2, g2 = jnp.split(mod, 6, axis=-1)
        h = modulate(rms_norm(x, norm1_g[layer]), sh1, sc1)
        q, k, v, u = jnp.split(h @ w_in[layer], split_at, axis=-1)
        att = diff_attention(q, k, v, q_norm_g[layer], k_norm_g[layer],
                             lambda_q1[layer], lambda_k1[layer], lambda_q2[layer], lambda_k2[layer],
                             subln_g[layer], cos, sin, lambda_init(layer))
        ssm = s5_branch(u, ssm_a_re[layer], ssm_a_im[layer], ssm_log_dt[layer],
                        ssm_b_re[layer], ssm_b_im[layer], ssm_c_re[layer], ssm_c_im[layer],
                        ssm_d[layer], w_glu[layer], ssm_norm_g[layer])
        mix = jnp.concatenate([att, ssm], axis=-1) @ w_out[layer]
        x = x + g1[:, None, :] * mix
        h = modulate(rms_norm(x, norm2_g[layer]), sh2, sc2).reshape(bsz * seq_len, D_MODEL)
        e_idx, gate_w = route(h, w_router[layer], router_bias[layer])
        ffn = (routed_experts(h, e_idx, gate_w, w_gate_e[layer], w_up_e[layer], w_down_e[layer])
               + swiglu(h, w_gate_s[layer], w_up_s[layer], w_down_s[layer]))
        x = x + g2[:, None, :] * ffn.reshape(bsz, seq_len, D_MODEL)
    return x
```

```python
import contextlib
import math
import numpy as np
import ml_dtypes
import concourse.bass as bass
import concourse.mybir as mybir
from concourse.bass_utils import run_bass_kernel_spmd

F32 = mybir.dt.float32
BF16 = mybir.dt.bfloat16
I32 = mybir.dt.int32
U32 = mybir.dt.uint32
ALU = mybir.AluOpType
AF = mybir.ActivationFunctionType
AX = mybir.AxisListType

import os
NOPOOL = os.environ.get("NOPOOL", "1") == "1"
ENG = ("pe", "act", "dve", "pool", "sp")
NDMA = 48
NDMA_HW = 32
EPS = 1e-6
T = 4096
D = 1024
NT = T // 128
CAP = 256
NE = 256


class Sched:
    def __init__(self, nc):
        self.nc = nc
        self.ops = {e: [] for e in ENG}
        self.last_w = {}
        self.readers = {}
        self.known = {e: {} for e in ENG}
        self.known_dma = {e: set() for e in ENG}
        self.dmas = []
        self.sem_last = [None] * NDMA
        self.sem_cnt = [0] * NDMA
        self.next_sem = 0
        self.next_sw = 0
        self.live_dma = set()

    def _deps(self, eng, reads, writes):
        deps = []
        for t in reads:
            r = self.last_w.get(t)
            if r is not None:
                deps.append(r)
        for t in writes:
            r = self.last_w.get(t)
            if r is not None:
                deps.append(r)
            deps.extend(self.readers.get(t, ()))
        waits = []
        for d in deps:
            if d[0] == "e":
                _, e2, idx = d
                if e2 == eng and eng in ("pe", "sp"):
                    continue
                if self.known[eng].get(e2, -1) >= idx:
                    continue
                self.known[eng][e2] = idx
                self.ops[e2][idx]["need"] = True
                waits.append(d)
            else:
                did = d[1]
                if did in self.known_dma[eng]:
                    continue
                self.known_dma[eng].add(did)
                waits.append(d)
        return waits

    def _commit(self, ref, reads, writes):
        for t in reads:
            self.readers.setdefault(t, []).append(ref)
        for t in writes:
            self.last_w[t] = ref
            self.readers[t] = []

    def op(self, eng, fn, r=(), w=()):
        if eng == "pool" and NOPOOL:
            eng = "dve"
        waits = self._deps(eng, r, w)
        idx = len(self.ops[eng])
        self.ops[eng].append(dict(fn=fn, waits=waits, need=False, dma=None))
        self._commit(("e", eng, idx), r, w)

    def dma(self, eng, fn, r=(), w=()):
        waits = self._deps(eng, r, w)
        if eng == "pool":
            s = NDMA_HW + self.next_sw
            self.next_sw = (self.next_sw + 1) % (NDMA - NDMA_HW)
        else:
            s = self.next_sem
            self.next_sem = (s + 1) % NDMA_HW
        prev = self.sem_last[s]
        if prev is not None and prev not in self.known_dma[eng]:
            self.known_dma[eng].add(prev)
            waits.append(("d", prev))
        self.sem_cnt[s] += 16
        did = len(self.dmas)
        self.dmas.append(dict(sem=s, val=self.sem_cnt[s]))
        self.sem_last[s] = did
        self.ops[eng].append(dict(fn=fn, waits=waits, need=False, dma=did))
        self._commit(("d", did), r, w)
        self.live_dma.add(did)

    def barrier(self):
        waits = []
        for e in ENG:
            if e != "sp" and self.ops[e]:
                idx = len(self.ops[e]) - 1
                while idx >= 0 and self.ops[e][idx]["dma"] is not None:
                    idx -= 1
                if idx < 0 or self.known["sp"].get(e, -1) >= idx:
                    continue
                self.known["sp"][e] = idx
                self.ops[e][idx]["need"] = True
                waits.append(("e", e, idx))
        for did in sorted(self.live_dma):
            if did not in self.known_dma["sp"]:
                self.known_dma["sp"].add(did)
                waits.append(("d", did))
        self.live_dma = set()
        idx = len(self.ops["sp"])
        self.ops["sp"].append(dict(fn=None, waits=waits, need=True, dma=None))
        ref = ("e", "sp", idx)
        for e in ENG:
            if e == "sp":
                continue
            self.known[e]["sp"] = idx
            self.ops[e].append(dict(fn=None, waits=[ref], need=False, dma=None))
            for e2 in ENG:
                if e2 != e and self.ops[e2]:
                    self.known[e][e2] = max(self.known[e].get(e2, -1), len(self.ops[e2]) - 1)
            self.known_dma[e] = set(range(len(self.dmas)))
        self.known_dma["sp"] = set(range(len(self.dmas)))
        self.last_w = {}
        self.readers = {}

    def emit(self):
        nc = self.nc
        with contextlib.ExitStack() as st:
            esem = {e: st.enter_context(nc.semaphore("s_" + e)) for e in ENG}
            dsem = [st.enter_context(nc.semaphore("d%d" % i)) for i in range(NDMA)]
            val = {}
            for e in ENG:
                c = 0
                for i, o in enumerate(self.ops[e]):
                    if o["need"]:
                        c += 1
                        val[(e, i)] = c
            block = st.enter_context(nc.Block())

            def run(e, eng):
                for i, o in enumerate(self.ops[e]):
                    for wt in o["waits"]:
                        if wt[0] == "e":
                            eng.wait_ge(esem[wt[1]], val[(wt[1], wt[2])])
                        else:
                            d = self.dmas[wt[1]]
                            eng.wait_ge(dsem[d["sem"]], d["val"])
                    if o["fn"] is None:
                        if o["need"]:
                            eng.sem_inc(esem[e], 1)
                        continue
                    ins = o["fn"](eng)
                    if o["dma"] is not None:
                        ins.then_inc(dsem[self.dmas[o["dma"]]["sem"]], 16)
                    elif o["need"]:
                        ins.then_inc(esem[e], 1)

            @block.tensor
            def _(eng):
                run("pe", eng)

            @block.scalar
            def _(eng):
                run("act", eng)

            @block.vector
            def _(eng):
                run("dve", eng)

            @block.gpsimd
            def _(eng):
                run("pool", eng)

            @block.sync
            def _(eng):
                run("sp", eng)


IN_SPECS = [
    ("x", [T, D], F32), ("vs1", [128, 128], F32),
    ("w_ada", [D, 6 * D], F32), ("w_in", [D, 2048], F32), ("w_glu", [512, 512], F32), ("w_out", [D, D], F32),
    ("w_router", [D, 256], F32), ("w_gate_s", [D, 256], F32), ("w_up_s", [D, 256], F32), ("w_down_s", [256, D], F32),
    ("rbias", [128, 256], F32),
    ("w_gate_e", [NE, D, 256], F32), ("w_up_e", [NE, D, 256], F32), ("w_down_e", [NE, 256, D], F32),
    ("ident_f", [128, 128], F32), ("ident_b", [128, 128], BF16),
    ("ones_f", [128, 128], F32),
    ("ropecs", [128, NT, 64], F32), ("gqk", [128, 128], F32),
    ("lamv", [128, 256], F32), ("gsub", [128, 128], F32), ("tri", [128, 128], BF16),
    ("w3mask", [128, 4, 512], BF16), ("w3eye", [128, 4, 512], BF16),
    ("ssm_cols", [128, 3, 16], F32), ("ssm_b", [128, 2, 16, 16], F32), ("ssm_c", [128, 2, 16, 16], F32), ("ssm_dcol", [128, 16], F32),
]


def build(stage=99, dbg=()):
    nc = bass.Bass("TRN2", target_bir_lowering=False)
    S = Sched(nc)
    I = {}
    for name, shp, dt in IN_SPECS:
        if stage < 7 and name in ("w_gate_e", "w_up_e", "w_down_e"):
            continue
        I[name] = nc.dram_tensor(name, shp, dt, kind="ExternalInput").ap()
    out = nc.dram_tensor("out", [T, D], F32, kind="ExternalOutput").ap()

    def dma(eng, o, i, r=(), w=()):
        S.dma(eng, lambda e: e.dma_start(out=o, in_=i), r, w)

    def mm(o, l, rh, st_, sp_, r=(), w=()):
        S.op("pe", lambda e: e.matmul(o, l, rh, start=st_, stop=sp_), r, w)

    def tr(o, i, idn, r=(), w=()):
        S.op("pe", lambda e: e.transpose(o, i, idn), r, w)

    def act(o, i, func, r=(), w=(), **kw):
        S.op("act", lambda e: e.activation(out=o, in_=i, func=func, **kw), r, w)

    def ts(eng, o, i, s1, s2, op0, op1=None, r=(), w=(), **kw):
        if op1 is None:
            S.op(eng, lambda e: e.tensor_scalar(out=o, in0=i, scalar1=s1, scalar2=None, op0=op0, **kw), r, w)
        else:
            S.op(eng, lambda e: e.tensor_scalar(out=o, in0=i, scalar1=s1, scalar2=s2, op0=op0, op1=op1, **kw), r, w)

    def tt(eng, o, a, b, op, r=(), w=()):
        S.op(eng, lambda e: e.tensor_tensor(out=o, in0=a, in1=b, op=op), r, w)

    def stt(eng, o, a, s, b, op0, op1, r=(), w=()):
        S.op(eng, lambda e: e.scalar_tensor_tensor(out=o, in0=a, scalar=s, in1=b, op0=op0, op1=op1), r, w)

    def cp(eng, o, i, r=(), w=()):
        if eng == "act":
            S.op(eng, lambda e: e.activation(out=o, in_=i, func=AF.Copy), r, w)
        else:
            S.op(eng, lambda e: e.tensor_copy(out=o, in_=i), r, w)

    import os
    KDIS = os.environ.get("KDIS", "").split(",")

    def dbg_dump(name, src, shp, dt, r, force=False):
        if name in dbg or force:
            d = nc.dram_tensor("dbg_" + name, shp, dt, kind="ExternalOutput").ap()
            dma("sp", d, src, r=r, w=["dbg_" + name])

    with contextlib.ExitStack() as st:
        def sb(name, shp, dt=F32):
            return st.enter_context(nc.sbuf_tensor("sb_" + name, shp, dt))

        ps = [st.enter_context(nc.psum_tensor("ps%d" % b, [128, 512], F32)) for b in range(8)]
        PS = [("ps", b) for b in range(8)]
        ident_f = sb("ident_f", [128, 128]); ident_b = sb("ident_b", [128, 128], BF16)
        ones_f = sb("ones_f", [128, 128]); vc1 = sb("vc1", [128, 128])
        ARENA_KIB = 192
        arena = sb("arena", [128, ARENA_KIB * 256])

        def carve(off_kib, shp, dt=F32):
            n = 1
            for d_ in shp[1:]:
                n *= d_
            nbytes = n * (2 if dt == BF16 else 4)
            o4 = int(round(off_kib * 256))
            assert abs(o4 - off_kib * 256) < 1e-9 and nbytes % 4 == 0
            assert o4 * 4 + nbytes <= ARENA_KIB * 1024, (off_kib, shp)
            v = arena[:, o4:o4 + nbytes // 4]
            if dt != F32:
                v = v.bitcast(dt)
            if len(shp) == 2:
                return v
            names = " ".join("d%d" % i for i in range(1, len(shp)))
            kw = {"d%d" % i: shp[i] for i in range(2, len(shp))}
            return v.rearrange("p (%s) -> p %s" % (names, names), **kw)

        dma("sp", ident_f[:], I["ident_f"], w=["ident_f"])
        dma("sp", ident_b[:], I["ident_b"], w=["ident_b"])
        dma("sp", ones_f[:], I["ones_f"], w=["ones_f"])
        oz = sb("oz", [128, 2])
        S.op("dve", lambda e: e.memset(oz[:, 0:1], 1.0), w=["zc"])
        S.op("dve", lambda e: e.memset(oz[:, 1:2], 0.0), r=["zc"], w=["zc"])
        onec = oz[:, 0:1]
        zc = oz[:, 1:2]

        def evac_bf(eng, o, i, r=(), w=()):
            rr = list(r) + ["ones_f", "zc"]
            if True:
                S.op("act", lambda e: e.activation(out=o, in_=i, func=AF.Identity, scale=onec, bias=zc), rr, w)
            else:
                S.op("dve", lambda e: e.tensor_scalar(out=o, in0=i, scalar1=onec, scalar2=zc, op0=ALU.mult, op1=ALU.add), rr, w)

        vs1 = sb("vs1", [128, 128])
        dma("sp", vs1[:], I["vs1"], w=["vs1"])
        act(vs1[0:8, :], vs1[0:8, :], AF.Silu, r=["vs1"], w=["vs1"])
        tr(ps[0][:, 0:128], vs1[:], ident_f[:], r=["vs1", "ident_f"], w=[PS[0]])
        cp("dve", vc1[:], ps[0][:, 0:128], r=[PS[0]], w=["vc1"])
        mod = sb("mod", [128, 48])
        wada = [carve(24 * i, [128, 8, 768]) for i in range(2)]
        wada_src = I["w_ada"].rearrange("(ko p) n -> p ko n", p=128)
        for blk in range(8):
            wt = wada[blk % 2]
            tk = ("wada", blk % 2)
            dma("sp", wt, wada_src[:, :, blk * 768:(blk + 1) * 768], w=[tk])
            for jj in range(6):
                j = blk * 6 + jj
                for k in range(8):
                    mm(ps[1][:, j:j + 1], wt[:, k, jj * 128:(jj + 1) * 128], vc1[:, k:k + 1], k == 0, k == 7, r=[tk, "vc1"], w=[PS[1]])
        tt("dve", mod[:], ps[1][:, 0:48], vc1[:, 24:72], ALU.add, r=[PS[1], "vc1"], w=["mod"])
        ab = sb("ab", [128, 32])
        stt("dve", ab[:, 0:8], mod[:, 8:16], 1.0, vc1[:, 8:16], ALU.add, ALU.mult, r=["mod", "vc1"], w=["ab"])
        cp("dve", ab[:, 8:16], mod[:, 0:8], r=["mod", "ab"], w=["ab"])
        stt("dve", ab[:, 16:24], mod[:, 32:40], 1.0, vc1[:, 16:24], ALU.add, ALU.mult, r=["mod", "vc1", "ab"], w=["ab"])
        cp("dve", ab[:, 24:32], mod[:, 24:32], r=["mod", "ab"], w=["ab"])
        dbg_dump("mod", mod[:], [128, 48], F32, ["mod"])
        S.barrier()
        if stage <= 0:
            S.emit(); return nc

        win = carve(0, [128, 8, 2048], BF16)
        qT = carve(32, [128, 4, T], BF16); kT = carve(64, [128, 4, T], BF16)
        vext = carve(96, [128, NT, 4 * 130], BF16)
        hT = carve(130, [128, 8, 2048], BF16)
        qkr = [carve(162 + 2 * i, [128, 1024], BF16) for i in range(2)]
        xn = [carve(166 + 2 * i, [128, D], BF16) for i in range(2)]
        xt = [carve(170 + 4 * i, [128, D]) for i in range(2)]
        ust = [carve(178 + i, [128, 512], BF16) for i in range(2)]
        sqb = carve(180, [128, 1024]); t1 = carve(184, [128, 1024])
        m1 = sqb[:, 0:512]; m2 = sqb[:, 512:1024]; m3 = carve(188, [128, 512]); m4 = carve(190, [128, 512])
        gqk = sb("gqk", [128, 128])
        stat = sb("stat", [128, 2 * NT]); st16 = sb("st16", [128, NT, 16])
        rcs = [sb("rcs%d" % i, [128, 64]) for i in range(2)]
        u_d = nc.dram_tensor("scr_u", [T, 512], BF16, kind="ExternalOutput").ap()
        win_src = I["w_in"].rearrange("(ko p) n -> p ko n", p=128)
        for k in range(8):
            for hf in range(2):
                if "wincast" not in KDIS:
                    dma("pool", win[:, k, hf * 1024:(hf + 1) * 1024], win_src[:, k, hf * 1024:(hf + 1) * 1024], w=[("win", k, hf)])
        WIN = [("win", k, hf) for k in range(8) for hf in range(2)]
        dma("sp", gqk[:], I["gqk"], w=["gqk"])
        if "memset" not in KDIS:
            S.op("pool", lambda e: e.memset(vext.rearrange("p n (h e) -> p n h e", e=130)[:, :, :, 128:129], 1.0), w=["vext1"])

        def phaseB_tile(i):
            s = i % 2
            x_t, xn_t = xt[s], xn[s]
            tx, tn = ("xt", s), ("xn", s)
            pb = 2 + (i % 2)
            il = i % 16
            ss, rs = stat[:, 2 * i:2 * i + 1], stat[:, 2 * i + 1:2 * i + 2]
            dma("sp", x_t, I["x"][i * 128:(i + 1) * 128, :], w=[tx])
            act(xn_t, x_t, AF.Square, r=[tx], w=[tn, ("stat", i)], accum_out=ss, scale=1.0 / math.sqrt(D))
            ts("dve", rs, ss, EPS, None, ALU.add, r=[("stat", i)], w=[("stat", i)])
            act(rs, rs, AF.Sqrt, r=[("stat", i)], w=[("stat", i)])
            S.op("dve", lambda e: e.reciprocal(out=rs, in_=rs), r=[("stat", i)], w=[("stat", i)])
            act(xn_t, x_t, AF.Copy, r=[tx, ("stat", i)], w=[tn], scale=rs)
            pv = ps[pb][:].bitcast(BF16)
            for c in range(8):
                tr(pv[:, c * 128:(c + 1) * 128], xn_t[:, c * 128:(c + 1) * 128], ident_b[:], r=[tn, "ident_b"], w=[PS[pb]])
            for c in range(8):
                o = hT[:, c, il * 128:(il + 1) * 128]
                src = pv[:, c * 128:(c + 1) * 128]
                if c % 2 == 0:
                    ts("dve", o, src, ab[:, c:c + 1], ab[:, 8 + c:9 + c], ALU.mult, ALU.add, r=[PS[pb], "ab"], w=[("hT", il)])
                else:
                    act(o, src, AF.Identity, r=[PS[pb], "ab"], w=[("hT", il)], scale=ab[:, c:c + 1], bias=ab[:, 8 + c:9 + c])

        def phaseC_tile(i):
            s = i % 2
            il = i % 16
            bq, bk = (0, 1) if s == 0 else (4, 5)
            bt = 2 + s
            tok = slice(i * 128, (i + 1) * 128)
            tl = slice(il * 128, (il + 1) * 128)
            hdep = [("hT", il)]
            dma("sp", rcs[s][:], I["ropecs"][:, i, :], w=[("rcs", s)])
            for c in range(8):
                mm(ps[bq][:], hT[:, c, tl], win[:, c, 0:512], c == 0, c == 7, r=hdep + WIN, w=[PS[bq]])
            for c in range(8):
                mm(ps[bk][:], hT[:, c, tl], win[:, c, 512:1024], c == 0, c == 7, r=hdep + WIN, w=[PS[bk]])
            for c in range(8):
                mm(ps[6][:], hT[:, c, tl], win[:, c, 1024:1536], c == 0, c == 7, r=hdep + WIN, w=[PS[6]])
            for c in range(8):
                mm(ps[7][:], hT[:, c, tl], win[:, c, 1536:2048], c == 0, c == 7, r=hdep + WIN, w=[PS[7]])
            act(vext[:, i, :].rearrange("p (h e) -> p h e", e=130)[:, :, 0:128], ps[6][:].rearrange("p (h e) -> p h e", e=128), AF.Copy,
                r=[PS[6]], w=[("vext", i)])
            cp("dve", ust[s][:], ps[7][:], r=[PS[7]], w=[("ust", s)])
            if "ubmd" not in KDIS:
                dma("sp", u_d[tok, :], ust[s][:], r=[("ust", s)], w=[("u_d", i // 16)])
            KC = int(os.environ.get("KC", "9"))
            if KC < 2:
                return
            act(sqb[:, 0:512], ps[bq][:], AF.Square, r=[PS[bq]], w=["sqb"])
            act(sqb[:, 512:1024], ps[bk][:], AF.Square, r=[PS[bk], "sqb"], w=["sqb"])
            s16 = st16[:, i, :]
            S.op("dve", lambda e: e.tensor_reduce(out=s16, in_=sqb.rearrange("p (a b) -> p a b", b=64), axis=AX.X, op=ALU.add), r=["sqb"], w=[("st16", i)])
            ts("dve", s16, s16, 1.0 / 64, EPS, ALU.mult, ALU.add, r=[("st16", i)], w=[("st16", i)])
            act(s16, s16, AF.Sqrt, r=[("st16", i)], w=[("st16", i)])
            S.op("dve", lambda e: e.reciprocal(out=s16, in_=s16), r=[("st16", i)], w=[("st16", i)])
            tt("dve", t1[:, 0:512].rearrange("p (a b) -> p a b", b=64), ps[bq][:].rearrange("p (a b) -> p a b", b=64),
               st16[:, i, 0:8].unsqueeze(2).to_broadcast([128, 8, 64]), ALU.mult, r=[PS[bq], ("st16", i)], w=["t1"])
            tt("dve", t1[:, 512:1024].rearrange("p (a b) -> p a b", b=64), ps[bk][:].rearrange("p (a b) -> p a b", b=64),
               st16[:, i, 8:16].unsqueeze(2).to_broadcast([128, 8, 64]), ALU.mult, r=[PS[bk], ("st16", i), "t1"], w=["t1"])
            tt("dve", t1.rearrange("p (k a d) -> p k a d", k=2, d=64), t1.rearrange("p (k a d) -> p k a d", k=2, d=64),
               gqk[:].rearrange("p (k d) -> p k d", d=64).unsqueeze(2).to_broadcast([128, 2, 8, 64]), ALU.mult, r=["t1", "gqk"], w=["t1"])
            if KC < 3:
                return
            tv = t1.rearrange("p (a two d) -> p a two d", two=2, d=32)
            ta, tb = tv[:, :, 0, :], tv[:, :, 1, :]
            cosb = rcs[s][:, 0:32].unsqueeze(1).to_broadcast([128, 16, 32])
            sinb = rcs[s][:, 32:64].unsqueeze(1).to_broadcast([128, 16, 32])
            qk_t = qkr[s]
            ov = qk_t.rearrange("p (a two d) -> p a two d", two=2, d=32)
            v3 = lambda t: t.rearrange("p (a d) -> p a d", d=32)
            rc = [("rcs", s)]
            tt("dve", v3(m1), ta, cosb, ALU.mult, r=["t1", "sqb"] + rc, w=["sqb"])
            tt("dve", v3(m2), tb, sinb, ALU.mult, r=["t1", "sqb"] + rc, w=["sqb"])
            tt("dve", ov[:, :, 0, :], v3(m1), v3(m2), ALU.subtract, r=["sqb"], w=[("qkr", s)])
            pe_ = "dve" if "pool" in KDIS else "pool"
            tt(pe_, v3(m3), ta, sinb, ALU.mult, r=["t1"] + rc, w=["m3"])
            tt(pe_, v3(m4), tb, cosb, ALU.mult, r=["t1"] + rc, w=["m4"])
            tt(pe_, ov[:, :, 1, :], v3(m3), v3(m4), ALU.add, r=["m3", "m4", ("qkr", s)], w=[("qkr", s)])
            if KC < 4:
                return
            pv = ps[bt][:].bitcast(BF16)
            for c in range(8):
                tr(pv[:, c * 128:(c + 1) * 128], qk_t[:, c * 128:(c + 1) * 128], ident_b[:], r=[("qkr", s), "ident_b"], w=[PS[bt]])
            pv3 = pv.rearrange("p (c t) -> p c t", t=128)
            KV = os.environ.get("KV", "abc")
            for c in range(4):
                if "b" in KV:
                    evac_bf("act", qT[:, c, tok], pv3[:, c, :], r=[PS[bt]], w=[("qT", i)])
                if "c" in KV:
                    evac_bf("act", kT[:, c, tok], pv3[:, 4 + c, :], r=[PS[bt]], w=[("kT", i)])

        for half in range(2):
            for i in range(16 * half, 16 * half + 16):
                phaseB_tile(i)
            if half == 0:
                for c in range(8):
                    dbg_dump("hT%d" % c, hT[:, c, :], [128, 2048], BF16, [("hT", i) for i in range(16)])
            for i in range(16 * half, 16 * half + 16):
                if stage >= 2:
                    phaseC_tile(i)
        dbg_dump("qTs", qT[:, 1, 1024:2048], [128, 1024], BF16, [("qT", i) for i in range(NT)])
        dbg_dump("kTs", kT[:, 2, 3072:4096], [128, 1024], BF16, [("kT", i) for i in range(NT)])
        for h in range(4):
            dbg_dump("qT%d" % h, qT[:, h, :], [128, T], BF16, [("qT", i) for i in range(NT)])
            dbg_dump("kT%d" % h, kT[:, h, :], [128, T], BF16, [("kT", i) for i in range(NT)])
        for q4 in range(4):
            dbg_dump("vext%d" % q4, vext[:, q4 * 8:(q4 + 1) * 8, :], [128, 8, 520], BF16, [("vext", i) for i in range(NT)] + ["vext1"])
        negb = sb("negb", [128, 4])
        S.op("dve", lambda e: e.tensor_reduce(out=negb[:, 0:1], in_=gqk[:, 0:64], axis=AX.X, op=ALU.max, apply_absolute_value=True), r=["gqk"], w=["negb"])
        S.op("dve", lambda e: e.tensor_reduce(out=negb[:, 1:2], in_=gqk[:, 64:128], axis=AX.X, op=ALU.max, apply_absolute_value=True), r=["gqk", "negb"], w=["negb"])
        stt("dve", negb[:, 2:3], negb[:, 0:1], -8.0, negb[:, 1:2], ALU.mult, ALU.mult, r=["negb"], w=["negb"])
        S.barrier()
        if stage <= 2:
            S.emit(); return nc

        PI = math.pi
        scol = carve(138, [128, 3, 16])
        sw = carve(138.25, [128, 16, 16])
        bbp = carve(139.25, [128, 2, 16, 32])
        ccp = carve(143.25, [128, 2, 16, 32])
        pw = carve(147.25, [128, 2, 16, 17])
        pwn = carve(149.375, [128, 2, 16, 17])
        pwr = carve(151.5, [128, 2, 16, 16])
        a2k = carve(153.5, [128, 2, 16, 8])
        dcol = carve(154.5, [128, 16])
        btmp = carve(154.75, [128, 2, 16, 16])
        ctmp = carve(156.75, [128, 2, 16, 16])
        dma("sp", scol, I["ssm_cols"], w=["scol"])
        dma("sp", btmp, I["ssm_b"], w=["btmp"])
        dma("sp", ctmp, I["ssm_c"], w=["ctmp"])
        dma("sp", dcol, I["ssm_dcol"], w=["dcol"])
        SW = lambda k: sw[:, k, :]
        are, aim, ldt = scol[:, 0, :], scol[:, 1, :], scol[:, 2, :]
        W_ = ["sw"]

        def dv(o, a, b, op):
            tt("dve", o, a, b, op, r=W_ + ["scol"], w=W_)

        def dsc(o, a, s1, s2, op0, op1=None):
            ts("dve", o, a, s1, s2, op0, op1, r=W_ + ["scol"], w=W_)
        act(SW(0), ldt, AF.Exp, r=["scol"], w=W_)
        dv(SW(1), are, SW(0), ALU.mult)
        act(SW(2), SW(1), AF.Exp, r=W_, w=W_)
        dv(SW(3), aim, SW(0), ALU.mult)
        def range_reduce(dst, shift):
            dsc(dst, SW(3), shift, None, ALU.add)
            for _ in range(8):
                dsc(SW(14), dst, PI, None, ALU.is_gt)
                stt("dve", dst, SW(14), -2 * PI, dst, ALU.mult, ALU.add, r=W_, w=W_)
        range_reduce(SW(4), 0.0)
        range_reduce(SW(5), 0.5 * PI)
        act(SW(4), SW(4), AF.Sin, r=W_, w=W_)
        act(SW(5), SW(5), AF.Sin, r=W_, w=W_)
        dv(SW(6), SW(2), SW(5), ALU.mult)
        dv(SW(7), SW(2), SW(4), ALU.mult)
        dv(SW(8), are, are, ALU.mult)
        dv(SW(9), aim, aim, ALU.mult)
        dv(SW(8), SW(8), SW(9), ALU.add)
        S.op("dve", lambda e: e.reciprocal(out=SW(8), in_=SW(8)), r=W_, w=W_)
        dsc(SW(9), SW(6), -1.0, None, ALU.add)
        dv(SW(10), SW(9), are, ALU.mult)
        dv(SW(11), SW(7), aim, ALU.mult)
        dv(SW(10), SW(10), SW(11), ALU.add)
        dv(SW(10), SW(10), SW(8), ALU.mult)
        dv(SW(11), SW(7), are, ALU.mult)
        dv(SW(12), SW(9), aim, ALU.mult)
        dv(SW(11), SW(11), SW(12), ALU.subtract)
        dv(SW(11), SW(11), SW(8), ALU.mult)
        dv(SW(12), SW(2), SW(2), ALU.mult)
        S.op("dve", lambda e: e.reciprocal(out=SW(12), in_=SW(12)), r=W_, w=W_)
        dv(SW(13), SW(6), SW(12), ALU.mult)
        dv(SW(14), SW(7), SW(12), ALU.mult)
        dsc(SW(14), SW(14), -1.0, None, ALU.mult)
        S.op("pool", lambda e: e.memset(bbp, 0.0), w=["bbp"])
        S.op("pool", lambda e: e.memset(ccp, 0.0), w=["ccp"])
        fre_b = SW(10).unsqueeze(2).to_broadcast([128, 16, 16])
        fim_b = SW(11).unsqueeze(2).to_broadcast([128, 16, 16])
        bre, bim = btmp[:, 0], btmp[:, 1]
        t_a = ctmp
        x1_, x2_ = pwr[:, 0], pwr[:, 1]
        tt("dve", x1_, bre, fre_b, ALU.mult, r=["btmp"] + W_, w=["pwr"])
        tt("dve", x2_, bim, fim_b, ALU.mult, r=["btmp"] + W_ + ["pwr"], w=["pwr"])
        tt("dve", x1_, x1_, x2_, ALU.subtract, r=["pwr"], w=["pwr"])
        for hh in range(2):
            cp("dve", bbp[64 * hh:64 * hh + 64, 0, :, 16 * hh:16 * hh + 16], x1_[64 * hh:64 * hh + 64], r=["pwr", "bbp"], w=["bbp"])
        tt("dve", x1_, bim, fre_b, ALU.mult, r=["btmp", "bbp"] + W_ + ["pwr"], w=["pwr"])
        tt("dve", x2_, bre, fim_b, ALU.mult, r=["btmp"] + W_ + ["pwr"], w=["pwr"])
        tt("dve", x1_, x1_, x2_, ALU.add, r=["pwr"], w=["pwr"])
        for hh in range(2):
            cp("dve", bbp[64 * hh:64 * hh + 64, 1, :, 16 * hh:16 * hh + 16], x1_[64 * hh:64 * hh + 64], r=["pwr", "bbp"], w=["bbp"])
            for ri in range(2):
                cp("dve", ccp[64 * hh:64 * hh + 64, ri, :, 16 * hh:16 * hh + 16], ctmp[64 * hh:64 * hh + 64, ri], r=["ctmp", "ccp"], w=["ccp"])
        PWT = ["pw", "pwn", "pwr", "a2k"]

        def cmul_col(ore, oim, xre, xim, yre, yim, tmp1, tmp2):
            tt("dve", tmp1, xre, yre, ALU.mult, r=PWT + W_, w=PWT + W_)
            tt("dve", tmp2, xim, yim, ALU.mult, r=PWT + W_, w=PWT + W_)
            tt("dve", oim, xre, yim, ALU.mult, r=PWT + W_, w=PWT + W_)
            tt("dve", ore, tmp1, tmp2, ALU.subtract, r=PWT + W_, w=PWT + W_)
            tt("dve", tmp1, xim, yre, ALU.mult, r=PWT + W_, w=PWT + W_)
            tt("dve", oim, oim, tmp1, ALU.add, r=PWT + W_, w=PWT + W_)
        S.op("dve", lambda e: e.memset(pw[:, 0, :, 0:1], 1.0), r=PWT, w=PWT)
        S.op("dve", lambda e: e.memset(pw[:, 1, :, 0:1], 0.0), r=PWT, w=PWT)
        S.op("dve", lambda e: e.memset(pwn[:, 0, :, 0:1], 1.0), r=PWT, w=PWT)
        S.op("dve", lambda e: e.memset(pwn[:, 1, :, 0:1], 0.0), r=PWT, w=PWT)
        for k in range(16):
            cmul_col(pw[:, 0, :, k + 1], pw[:, 1, :, k + 1], pw[:, 0, :, k], pw[:, 1, :, k], SW(6), SW(7), SW(0), SW(1))
            cmul_col(pwn[:, 0, :, k + 1], pwn[:, 1, :, k + 1], pwn[:, 0, :, k], pwn[:, 1, :, k], SW(13), SW(14), SW(0), SW(1))
        for tau in range(16):
            cp("dve", pwr[:, :, :, tau], pw[:, :, :, 15 - tau], r=PWT, w=PWT)
        cp("dve", a2k[:, :, :, 0], pw[:, :, :, 16], r=PWT, w=PWT)
        for l in range(7):
            cmul_col(a2k[:, 0, :, l + 1], a2k[:, 1, :, l + 1], a2k[:, 0, :, l], a2k[:, 1, :, l], a2k[:, 0, :, l], a2k[:, 1, :, l], SW(0), SW(1))
        if "ssmpar" in dbg:
            dbg_dump("pw", pw, [128, 2, 16, 17], F32, PWT, True)
            dbg_dump("pwn", pwn, [128, 2, 16, 17], F32, PWT, True)
            dbg_dump("a2k", a2k, [128, 2, 16, 8], F32, PWT, True)
            dbg_dump("bbp", bbp, [128, 2, 16, 32], F32, ["bbp"], True)

        attT = carve(0, [128, 4, T], BF16)
        pt = [carve(130 + i, [128, 512], BF16) for i in range(2)]
        lamv = carve(132, [128, 256]); lamp = carve(133, [128, 128])
        gsub = carve(133.5, [128, 128]); tri = carve(134, [128, 128], BF16)
        osb = [carve(135 + 0.5 * i, [128, 128]) for i in range(2)]
        onb = [carve(136 + 0.25 * i, [128, 128], BF16) for i in range(2)]
        osq = carve(136.5, [128, 128], BF16)
        lamc = sb("lamc", [128, 8])
        ast = carve(137, [128, 128, 4])
        dma("sp", lamv, I["lamv"], w=["lamv"])
        dma("sp", gsub, I["gsub"], w=["gsub"])
        dma("sp", tri, I["tri"], w=["tri"])
        tt("dve", lamp, lamv.rearrange("p (a two d) -> p a two d", two=2, d=64)[:, :, 0, :], lamv.rearrange("p (a two d) -> p a two d", two=2, d=64)[:, :, 1, :],
           ALU.mult, r=["lamv"], w=["lamp"])
        S.op("dve", lambda e: e.tensor_reduce(out=lamc[:, 0:2], in_=lamp.rearrange("p (a d) -> p a d", d=64), axis=AX.X, op=ALU.add), r=["lamp"], w=["lamc"])
        act(lamc[:, 2:4], lamc[:, 0:2], AF.Exp, r=["lamc"], w=["lamc"])
        stt("dve", lamc[:, 4:5], lamc[:, 3:4], -0.2, lamc[:, 2:3], ALU.add, ALU.subtract, r=["lamc"], w=["lamc"])
        ts("dve", gsub, gsub, 0.8, None, ALU.mult, r=["gsub"], w=["gsub"])
        neglam = lamc[:, 4:5]
        step = 0
        for h in range(0 if "att" not in KDIS else 4, 4):
            for Q2 in range(16):
                nkt = 2 * Q2 + 2
                for kt in range(nkt):
                    s = step % 2
                    step += 1
                    for c in range(2):
                        mm(ps[2 * s + c][:, 0:256], kT[64 * c:64 * c + 64, h, kt * 128:(kt + 1) * 128], qT[64 * c:64 * c + 64, h, Q2 * 256:(Q2 + 1) * 256],
                           True, True, r=[("kT", kt), ("qT", 2 * Q2), ("qT", 2 * Q2 + 1)], w=[PS[2 * s + c]])
                    for c in range(2):
                        act(pt[s][:, c * 256:(c + 1) * 256], ps[2 * s + c][:, 0:256], AF.Exp, r=[PS[2 * s + c], "negb", ("pt", s)], w=[("pt", s)], scale=0.125, bias=negb[:, 2:3])
                    KA = int(os.environ.get("KA", "9"))
                    if kt >= 2 * Q2 and KA >= 2:
                        r0 = kt - 2 * Q2
                        pv_ = pt[s].rearrange("p (c q) -> p c q", q=256)[:, :, r0 * 128:(r0 + 1) * 128]
                        tt("pool", pv_, pv_, tri.unsqueeze(1).to_broadcast([128, 2, 128]), ALU.mult, r=[("pt", s), "tri"], w=[("pt", s)])
                    for r in range(2):
                        if 2 * Q2 + r < kt or KA < 3:
                            continue
                        for c in range(2):
                            b = 4 + 2 * c + r
                            mm(ps[b][:, 0:129], pt[s][:, c * 256 + r * 128:c * 256 + (r + 1) * 128], vext[:, kt, h * 130:h * 130 + 129],
                               kt == 0, kt == 2 * Q2 + r, r=[("pt", s), ("vext", kt), "vext1"], w=[PS[b]])
                for r in range(2 if KA >= 4 else 0):
                    qt = 2 * Q2 + r
                    u_ = (h * 32 + qt) % 2
                    a4 = ast[:, qt, :] if h == 0 else ast[:, (h * 32 + qt) % 128, :]
                    tkn = ("ast", (h * 32 + qt) % 128)
                    b0, b1 = 4 + r, 6 + r
                    S.op("dve", (lambda a4, b0: lambda e: e.reciprocal(out=a4[:, 0:1], in_=ps[b0][:, 128:129]))(a4, b0), r=[PS[b0]], w=[tkn])
                    S.op("dve", (lambda a4, b1: lambda e: e.reciprocal(out=a4[:, 1:2], in_=ps[b1][:, 128:129]))(a4, b1), r=[PS[b1], tkn], w=[tkn])
                    tt("dve", a4[:, 1:2], a4[:, 1:2], neglam, ALU.mult, r=[tkn, "lamc"], w=[tkn])
                    ts("dve", osb[u_], ps[b0][:, 0:128], a4[:, 0:1], None, ALU.mult, r=[PS[b0], tkn], w=[("osb", u_)])
                    stt("dve", osb[u_], ps[b1][:, 0:128], a4[:, 1:2], osb[u_], ALU.mult, ALU.add, r=[PS[b1], tkn, ("osb", u_)], w=[("osb", u_)])
                    act(osq, osb[u_], AF.Square, r=[("osb", u_)], w=["osq", tkn], accum_out=a4[:, 2:3], scale=1.0 / math.sqrt(128.0))
                    ts("dve", a4[:, 2:3], a4[:, 2:3], EPS, None, ALU.add, r=[tkn], w=[tkn])
                    act(a4[:, 2:3], a4[:, 2:3], AF.Sqrt, r=[tkn], w=[tkn])
                    S.op("dve", (lambda a4: lambda e: e.reciprocal(out=a4[:, 2:3], in_=a4[:, 2:3]))(a4), r=[tkn], w=[tkn])
                    stt("dve", onb[u_], osb[u_], a4[:, 2:3], gsub, ALU.mult, ALU.mult, r=[("osb", u_), tkn, "gsub"], w=[("onb", u_)])
                    bt = u_
                    pvb = ps[bt][:].bitcast(BF16)
                    tr(pvb[:, 0:128], onb[u_], ident_b[:], r=[("onb", u_), "ident_b"], w=[PS[bt]])
                    evac_bf("act", attT[:, h, qt * 128:(qt + 1) * 128], pvb[:, 0:128], r=[PS[bt]], w=[("attT", h)])
        for h in range(4):
            dbg_dump("attT%d" % h, attT[:, h, :], [128, T], BF16, [("attT", h)])
        S.barrier()
        if stage <= 3:
            S.emit(); return nc

        ubm = carve(32, [128, 2, 16, 512], BF16)
        ybm = carve(64, [128, 2, 16, 512], BF16)
        w3mask = carve(96, [128, 4, 512], BF16); w3eye = carve(100, [128, 4, 512], BF16)
        for jt in range(2):
            for t4 in range(4):
                dma("sp", ubm[:, jt, 4 * t4:4 * t4 + 4, :], u_d[jt * 2048:(jt + 1) * 2048, :].rearrange("(b t) c -> b t c", t=16)[:, 4 * t4:4 * t4 + 4, :],
                    w=["ubm%d%d" % (jt, t4)])
        UBM = ["ubm%d%d" % (jt, t4) for jt in range(2) for t4 in range(4)]
        dma("sp", w3mask, I["w3mask"], w=["w3mask"])
        dma("sp", w3eye, I["w3eye"], w=["w3eye"])
        xsc = {"dve": (carve(172, [128, 16, 32]), carve(174, [128, 16, 32])), "pool": (carve(176, [128, 16, 32]), carve(178, [128, 16, 32]))}
        g1b = [carve(180 + 2 * i, [128, 512]) for i in range(2)]
        g2b = [carve(184 + 2 * i, [128, 512]) for i in range(2)]

        def cprod(eng, ore, oim, are_, aim_, bre_, bim_, negim=False):
            xa, xb = xsc[eng]
            tk = "xsc_" + eng
            rr = PWT + ["bbp", "ccp"]
            tt(eng, xa, are_, bre_, ALU.mult, r=rr + [tk], w=[tk])
            tt(eng, xb, aim_, bim_, ALU.mult, r=rr + [tk], w=[tk])
            tt(eng, ore[0], xa, xb, ALU.subtract, r=[tk], w=[ore[1]])
            tt(eng, xa, are_, bim_, ALU.mult, r=rr + [tk], w=[tk])
            tt(eng, xb, aim_, bre_, ALU.mult, r=rr + [tk], w=[tk])
            if negim:
                tt(eng, xa, xa, xb, ALU.add, r=[tk], w=[tk])
                ts(eng, oim[0], xa, -1.0, None, ALU.mult, r=[tk], w=[oim[1]])
            else:
                tt(eng, oim[0], xa, xb, ALU.add, r=[tk], w=[oim[1]])

        def ssm_j(j):
            s = j % 2
            base = 104 + 16 * s
            utj = carve(base, [128, 4, 256], BF16)
            w1t = carve(base + 2, [128, 2, 512], BF16)
            bh = carve(base + 4, [128, 2, 512], BF16)
            w2 = carve(base + 6, [128, 2, 512], BF16)
            w1l = carve(base + 8, [128, 2, 4, 128], BF16)
            w3 = carve(base + 10, [128, 4, 512], BF16)
            sbase = 160 + 6 * s
            sre = carve(sbase, [128, 257]); sim_ = carve(sbase + 1.25, [128, 257])
            tre = carve(sbase + 2.5, [128, 256]); tim = carve(sbase + 3.5, [128, 256])
            sbb = carve(sbase + 4.5, [128, 2, 258], BF16)
            tim2 = carve(188 + s, [128, 256])
            K = lambda n: (n, s)
            bt = 2 + s
            pv = ps[bt][:].bitcast(BF16)
            ustg = carve(base + 14, [128, 2, 512], BF16)
            for jt in range(2):
                cp("pool", ustg[:, jt, :].rearrange("p (t c) -> p t c", c=32), ubm[:, jt, :, 32 * j:32 * j + 32], r=UBM, w=[K("ustg")])
            for t4 in range(4):
                for jt in range(2):
                    tr(pv[:, (t4 * 2 + jt) * 128:(t4 * 2 + jt + 1) * 128], ustg[:, jt, t4 * 128:(t4 + 1) * 128], ident_b[:],
                       r=[K("ustg"), "ident_b"], w=[PS[bt]])
            evac_bf("act", utj.rearrange("p a b -> p (a b)"), pv, r=[PS[bt]], w=[K("utj")])
            KS = int(os.environ.get("KS", "9"))
            if KS < 2:
                return
            v16 = lambda ap: ap.rearrange("p (t c) -> p t c", c=32)
            PR = lambda tab, ri, lo: tab[:, ri, j, lo:lo + 16].unsqueeze(2).to_broadcast([128, 16, 32])
            BB = lambda tab, ri: tab[:, ri, j, :].unsqueeze(1).to_broadcast([128, 16, 32])
            cprod("dve", (v16(w1t[:, 0, :]), K("w1t")), (v16(w1t[:, 1, :]), K("w1t")), PR(pwr, 0, 0), PR(pwr, 1, 0), BB(bbp, 0), BB(bbp, 1))
            cprod("pool", (v16(bh[:, 0, :]), K("bh")), (v16(bh[:, 1, :]), K("bh")), PR(pwn, 0, 1), PR(pwn, 1, 1), BB(bbp, 0), BB(bbp, 1))
            cprod("pool", (v16(w2[:, 0, :]), K("w2")), (v16(w2[:, 1, :]), K("w2")), PR(pw, 0, 1), PR(pw, 1, 1), BB(ccp, 0), BB(ccp, 1), negim=True)
            bw = s
            pvw = ps[bw][:].bitcast(BF16)
            for ri in range(2):
                for t4 in range(4):
                    tr(pvw[:, (ri * 4 + t4) * 128:(ri * 4 + t4 + 1) * 128], w1t[:, ri, t4 * 128:(t4 + 1) * 128], ident_b[:], r=[K("w1t"), "ident_b"], w=[PS[bw]])
            evac_bf("act", w1l.rearrange("p a b c -> p (a b c)"), pvw, r=[PS[bw]], w=[K("w1l")])
            for t4 in range(4):
                mm(ps[4][:], bh[:, 0, t4 * 128:(t4 + 1) * 128], w2[:, 0, :], True, False, r=[K("bh"), K("w2")], w=[PS[4]])
                mm(ps[4][:], bh[:, 1, t4 * 128:(t4 + 1) * 128], w2[:, 1, :], False, True, r=[K("bh"), K("w2")], w=[PS[4]])
                tt("dve", w3[:, t4, :], ps[4][:], w3mask[:, t4, :], ALU.mult, r=[PS[4], "w3mask"], w=[K("w3")])
                stt("dve", w3[:, t4, :], w3eye[:, t4, :], dcol[:, j:j + 1], w3[:, t4, :], ALU.mult, ALU.add, r=[K("w3"), "w3eye", "dcol"], w=[K("w3")])
            if KS < 3:
                return
            for ri in range(2):
                bv = 5 - ri
                for t4 in range(4):
                    mm(ps[bv][:, 0:256], w1l[:, ri, t4, :], utj[:, t4, :], t4 == 0, t4 == 3, r=[K("w1l"), K("utj")], w=[PS[bv]])
            S.op("dve", lambda e: e.memset(sre[:, 0:1], 0.0), w=[K("sre")])
            S.op("pool", lambda e: e.memset(sim_[:, 0:1], 0.0), w=[K("sim")])
            cp("dve", sre[:, 1:257], ps[5][:, 0:256], r=[PS[5]], w=[K("sre")])
            cp("act", sim_[:, 1:257], ps[4][:, 0:256], r=[PS[4]], w=[K("sim")])
            for l in range(8):
                d = 1 << l
                n = 256 - d
                ar_, ai_ = a2k[:, 0, j, l:l + 1], a2k[:, 1, j, l:l + 1]
                ts("dve", tre[:, 0:n], sim_[:, 1:1 + n], ai_, None, ALU.mult, r=[K("sim")] + PWT, w=[K("tre")])
                stt("dve", tre[:, 0:n], sre[:, 1:1 + n], ar_, tre[:, 0:n], ALU.mult, ALU.subtract, r=[K("sre"), K("tre")] + PWT, w=[K("tre")])
                ts("pool", tim[:, 0:n], sre[:, 1:1 + n], ai_, None, ALU.mult, r=[K("sre")] + PWT, w=[K("tim")])
                ts("pool", tim2[:, 0:n], sim_[:, 1:1 + n], ar_, None, ALU.mult, r=[K("sim")] + PWT, w=[K("tim2")])
                tt("pool", tim[:, 0:n], tim[:, 0:n], tim2[:, 0:n], ALU.add, r=[K("tim"), K("tim2")], w=[K("tim")])
                tt("dve", sre[:, 1 + d:257], sre[:, 1 + d:257], tre[:, 0:n], ALU.add, r=[K("sre"), K("tre")], w=[K("sre")])
                tt("pool", sim_[:, 1 + d:257], sim_[:, 1 + d:257], tim[:, 0:n], ALU.add, r=[K("sim"), K("tim")], w=[K("sim")])
            cp("dve", sbb[:, 0, 0:257], sre[:, 0:257], r=[K("sre")], w=[K("sbb")])
            cp("pool", sbb[:, 1, 0:257], sim_[:, 0:257], r=[K("sim"), K("sbb")], w=[K("sbb")])
            if KS < 4:
                return
            for jt in range(2):
                by = 6 + jt
                bl = slice(jt * 128, (jt + 1) * 128)
                mm(ps[by][:], sbb[:, 0, bl], w2[:, 0, :], True, False, r=[K("sbb"), K("w2")], w=[PS[by]])
                mm(ps[by][:], sbb[:, 1, bl], w2[:, 1, :], False, False, r=[K("sbb"), K("w2")], w=[PS[by]])
                for t4 in range(4):
                    mm(ps[by][:], utj[:, t4, bl], w3[:, t4, :], False, t4 == 3, r=[K("utj"), K("w3")], w=[PS[by]])
                if "ssm_y" in dbg:
                    cp("dve", ybm[:, jt, :, 32 * j:32 * j + 32], ps[by][:].rearrange("p (t c) -> p t c", c=32), r=[PS[by]], w=["ybm"])
                    continue
                g1, g2 = g1b[jt], g2b[jt]
                act(g1, ps[by][:], AF.Square, r=[PS[by]], w=[("g1", jt)])
                ts("dve", g1, g1, 0.044715, 1.0, ALU.mult, ALU.add, r=[("g1", jt)], w=[("g1", jt)])
                tt("dve", g1, g1, ps[by][:], ALU.mult, r=[("g1", jt), PS[by]], w=[("g1", jt)])
                act(g2, g1, AF.Sigmoid, r=[("g1", jt)], w=[("g2", jt)], scale=2.0 * math.sqrt(2.0 / math.pi))
                tt("dve", ybm[:, jt, :, 32 * j:32 * j + 32], g2.rearrange("p (t c) -> p t c", c=32), ps[by][:].rearrange("p (t c) -> p t c", c=32), ALU.mult,
                   r=[("g2", jt), PS[by]], w=["ybm"])

        for j in range(16):
            ssm_j(j)
        if "ybm" in dbg or "ssm_y" in dbg:
            for jt in range(2):
                for t4 in range(4):
                    dbg_dump("ybm%d_%d" % (jt, t4), ybm[:, jt, 4 * t4:4 * t4 + 4, :], [128, 4, 512], BF16, ["ybm"], True)
        S.barrier()
        if stage <= 4:
            S.emit(); return nc

        yT = carve(32, [128, 4, T], BF16)
        ssmT = carve(100, [128, 4, T], BF16)
        wglu = carve(96, [128, 4, 512], BF16)
        ones_b = carve(132, [128, 128], BF16)
        sgb = [carve(133 + i, [128, 512], BF16) for i in range(2)]
        sqb2 = [carve(135 + i, [128, 512], BF16) for i in range(2)]
        rsb = [carve(137 + 2 * i, [128, 512]) for i in range(2)]
        S.op("pool", lambda e: e.memset(ones_b, 1.0), w=["ones_b"])
        dma("pool", wglu, I["w_glu"].rearrange("(ko p) n -> p ko n", p=128), w=["wglu"])
        gi = 0
        for jt in range(2):
            for ct in range(4):
                for half in range(2):
                    b = gi % 4
                    pv = ps[b][:].bitcast(BF16)
                    for t8 in range(8):
                        tr(pv[:, t8 * 128:(t8 + 1) * 128], ybm[:, jt, half * 8 + t8, ct * 128:(ct + 1) * 128], ident_b[:], r=["ybm", "ident_b"], w=[PS[b]])
                    o = yT[:, ct, jt * 2048:(jt + 1) * 2048].rearrange("p (b t) -> p t b", t=16)[:, half * 8:half * 8 + 8, :]
                    src = pv.rearrange("p (t b) -> p t b", b=128)
                    for t8 in range(8):
                        evac_bf("act" if (gi + t8) % 2 == 0 else "dve", o[:, t8, :], src[:, t8, :], r=[PS[b]], w=[("yT", jt)])
                    gi += 1
        for tb in range(8):
            tbs = slice(tb * 512, (tb + 1) * 512)
            jt = tb // 4
            u_ = tb % 2
            bt_ = 6 + u_
            for ft in range(4):
                b = 4 + ft % 2
                for kt in range(4):
                    mm(ps[b][:], wglu[:, kt, ft * 128:(ft + 1) * 128], yT[:, kt, tbs], kt == 0, kt == 3, r=["wglu", ("yT", jt)], w=[PS[b]])
                sg, sq = sgb[ft % 2], sqb2[ft % 2]
                act(sg, ps[b][:], AF.Sigmoid, r=[PS[b]], w=[("sg", ft % 2)])
                tt("dve", ssmT[:, ft, tbs], yT[:, ft, tbs], sg, ALU.mult, r=[("yT", jt), ("sg", ft % 2)], w=[("ssmT", tb)])
                tt("pool", sq, ssmT[:, ft, tbs], ssmT[:, ft, tbs], ALU.mult, r=[("ssmT", tb)], w=[("sq2", ft % 2)])
                mm(ps[bt_][:], ones_b, sq, ft == 0, ft == 3, r=["ones_b", ("sq2", ft % 2)], w=[PS[bt_]])
            rs = rsb[u_]
            ts("dve", rs, ps[bt_][:], 1.0 / 512, EPS, ALU.mult, ALU.add, r=[PS[bt_]], w=[("rs", u_)])
            act(rs, rs, AF.Sqrt, r=[("rs", u_)], w=[("rs", u_)])
            S.op("dve", (lambda rs: lambda e: e.reciprocal(out=rs, in_=rs))(rs), r=[("rs", u_)], w=[("rs", u_)])
            for ft in range(4):
                stt("dve", ssmT[:, ft, tbs], ssmT[:, ft, tbs], vc1[:, 72 + ft:73 + ft], rs, ALU.mult, ALU.mult,
                    r=[("ssmT", tb), ("rs", u_), "vc1"], w=[("ssmT", tb)])
        for ct in range(4):
            dbg_dump("ssmT%d" % ct, ssmT[:, ct, :], [128, T], BF16, [("ssmT", tb) for tb in range(8)])
        S.barrier()
        if stage <= 5:
            S.emit(); return nc

        wout = carve(32, [128, 8, 1024], BF16)
        bct = {"g1": carve(48, [128, 1024]), "a2": carve(52, [128, 1024]), "b2": carve(56, [128, 1024]), "g2": carve(60, [128, 1024])}
        wr = carve(64, [128, 8, 256], BF16)
        wgu = carve(68, [128, 8, 512], BF16)
        wds = carve(76, [128, 2, 1024], BF16)
        rbias = carve(80, [128, 256])
        gatesT = carve(82, [128, 2, T], BF16)
        h2T_d = nc.dram_tensor("scr_h2T", [128, 8, T], BF16, kind="ExternalOutput").ap()
        diag = [carve(81 + 0.5 * i, [128, 128]) for i in range(2)]
        xt2 = [carve(132 + 4 * i, [128, 1024]) for i in range(2)]
        x1t = [carve(140 + 4 * i, [128, 1024]) for i in range(2)]
        h2tok = [carve(152 + 2 * i, [128, 1024], BF16) for i in range(2)]
        h2T = [carve(156 + 2 * i, [128, 8, 128], BF16) for i in range(2)]
        rsc = [carve(176 + i, [128, 256]) for i in range(8)]
        mbf = carve(168, [128, 256], BF16)
        ash = carve(168.5, [128, 256], BF16); ashT = carve(169, [128, 2, 128], BF16)
        sm = carve(185, [128, 128])
        x1p_d = out
        wo_src = I["w_out"].rearrange("(ko p) n -> p ko n", p=128)
        for k in range(8):
            dma("pool", wout[:, k, :], wo_src[:, k, :], w=[("wout", k)])
        WOUT = [("wout", k) for k in range(8)]
        dma("pool", wr, I["w_router"].rearrange("(ko p) n -> p ko n", p=128), w=["wr"])
        dma("pool", wgu[:, :, 0:256], I["w_gate_s"].rearrange("(ko p) n -> p ko n", p=128), w=["wgu0"])
        dma("pool", wgu[:, :, 256:512], I["w_up_s"].rearrange("(ko p) n -> p ko n", p=128), w=["wgu1"])
        dma("pool", wds, I["w_down_s"].rearrange("(ko p) n -> p ko n", p=128), w=["wds"])
        dma("sp", rbias, I["rbias"], w=["rbias"])
        bi = 0
        for name, col in (("g1", mod[:, 16:24]), ("a2", ab[:, 16:24]), ("b2", ab[:, 24:32]), ("g2", mod[:, 40:48])):
            for hf in range(2):
                for j4 in range(4):
                    jj = hf * 4 + j4
                    dg = diag[bi % 2]
                    ts("dve", dg, ident_f[:], col[:, jj:jj + 1], None, ALU.mult, r=["ident_f", "mod", "ab"], w=[("diag", bi % 2)])
                    mm(ps[hf][:, j4 * 128:(j4 + 1) * 128], ones_f[:], dg, True, True, r=["ones_f", ("diag", bi % 2)], w=[PS[hf]])
                    bi += 1
                cp("act", bct[name][:, hf * 512:(hf + 1) * 512], ps[hf][:], r=[PS[hf]], w=["bc_" + name])

        def phaseF_tile(i):
            s = i % 2
            tok = slice(i * 128, (i + 1) * 128)
            x_t, x1, h2t, h2T_ = xt2[s], x1t[s], h2tok[s], h2T[s]
            K = lambda n: (n, s)
            smc = lambda a, b=None: sm[:, (64 * s + a):(64 * s + (a + 1 if b is None else b))]
            dma("sp", x_t, I["x"][tok, :], w=[K("xt2")])
            for nb in range(2):
                for kt in range(8):
                    lhs = attT[:, kt, tok] if kt < 4 else ssmT[:, kt - 4, tok]
                    mm(ps[nb][:], lhs, wout[:, kt, nb * 512:(nb + 1) * 512], kt == 0, kt == 7, r=WOUT, w=[PS[nb]])
                nbs = slice(nb * 512, (nb + 1) * 512)
                tt("dve", x1[:, nbs], ps[nb][:], bct["g1"][:, nbs], ALU.mult, r=[PS[nb], "bc_g1"], w=[K("x1")])
            tt("pool", x1, x1, x_t, ALU.add, r=[K("x1"), K("xt2")], w=[K("x1")])
            act(h2t, x1, AF.Square, r=[K("x1")], w=[K("h2t"), K("sm")], accum_out=smc(0), scale=1.0 / math.sqrt(D))
            ts("dve", smc(1), smc(0), EPS, None, ALU.add, r=[K("sm")], w=[K("sm")])
            act(smc(1), smc(1), AF.Sqrt, r=[K("sm")], w=[K("sm")])
            S.op("dve", lambda e: e.reciprocal(out=smc(1), in_=smc(1)), r=[K("sm")], w=[K("sm")])
            stt("dve", x_t, x1, smc(1), bct["a2"], ALU.mult, ALU.mult, r=[K("x1"), K("sm"), "bc_a2", K("xt2")], w=[K("xt2")])
            tt("pool", h2t, x_t, bct["b2"], ALU.add, r=[K("xt2"), "bc_b2"], w=[K("h2t")])
            if "h2" in dbg and i < 2:
                dbg_dump("h2_%d" % i, h2t, [128, 1024], BF16, [K("h2t")], True)
                dbg_dump("x1_%d" % i, x1, [128, 1024], F32, [K("x1")], True)
            pv = ps[2][:].bitcast(BF16)
            for c in range(8):
                tr(pv[:, c * 128:(c + 1) * 128], h2t[:, c * 128:(c + 1) * 128], ident_b[:], r=[K("h2t"), "ident_b"], w=[PS[2]])
            evac_bf("act", h2T_.rearrange("p a b -> p (a b)"), pv, r=[PS[2]], w=[K("h2T")])
            sc, sbv, msb, Mf, posf, tmp, sgs = rsc[0], rsc[1], rsc[2], rsc[3], rsc[4], rsc[5], rsc[6]
            for c in range(8):
                mm(ps[3][:, 0:256], h2T_[:, c, :], wr[:, c, :], c == 0, c == 7, r=[K("h2T"), "wr"], w=[PS[3]])
            act(sc, ps[3][:, 0:256], AF.Sigmoid, r=[PS[3]], w=["sc"])
            tt("dve", sbv, sc, rbias, ALU.add, r=["sc", "rbias"], w=["sbv"])
            mx8 = carve(184, [128, 8, 8])
            for g in range(8):
                S.op("dve", (lambda g: lambda e: e.max(out=mx8[:, g, :], in_=sbv[:, g * 32:(g + 1) * 32]))(g), r=["sbv"], w=["mx8"])
            gs, g8, gm, pen = smc(8, 16), smc(16, 24), smc(24, 32), smc(32, 40)
            tt("dve", gs, mx8[:, :, 0], mx8[:, :, 1], ALU.add, r=["mx8"], w=[K("sm")])
            S.op("dve", lambda e: e.max(out=g8, in_=gs), r=[K("sm")], w=[K("sm")])
            ts("dve", gm, gs, g8[:, 3:4], None, ALU.is_ge, r=[K("sm")], w=[K("sm")])
            ts("dve", pen, gm, -1.0, 1e30, ALU.add, ALU.mult, r=[K("sm")], w=[K("sm")])
            v8 = lambda t: t.rearrange("p (g e) -> p g e", e=32)
            tt("dve", v8(msb), v8(sbv), gm.unsqueeze(2).to_broadcast([128, 8, 32]), ALU.mult, r=["sbv", K("sm")], w=["msb"])
            tt("dve", v8(msb), v8(msb), pen.unsqueeze(2).to_broadcast([128, 8, 32]), ALU.add, r=["msb", K("sm")], w=["msb"])
            top8 = smc(40, 48)
            S.op("dve", lambda e: e.max(out=top8, in_=msb), r=["msb"], w=[K("sm")])
            ts("dve", Mf, msb, top8[:, 7:8], None, ALU.is_ge, r=["msb", K("sm")], w=["Mf"])
            wsum = smc(2)
            S.op("dve", lambda e: e.scalar_tensor_tensor(out=tmp, in0=Mf, scalar=1.0, in1=sc, op0=ALU.mult, op1=ALU.mult, accum_out=wsum),
                 r=["Mf", "sc", "tmp"], w=["tmp", K("sm")])
            S.op("dve", lambda e: e.reciprocal(out=wsum, in_=wsum), r=[K("sm")], w=[K("sm")])
            ts("dve", wsum, wsum, 2.5, None, ALU.mult, r=[K("sm")], w=[K("sm")])
            ts("dve", mbf, tmp, wsum, None, ALU.mult, r=["tmp", K("sm")], w=["mbf"])
            if "route" in dbg:
                dbg_dump("gd%d" % i, mbf, [128, 256], BF16, ["mbf"], True)
            pv4 = ps[4][:].bitcast(BF16)
            for ec in range(2):
                tr(pv4[:, ec * 128:(ec + 1) * 128], mbf[:, ec * 128:(ec + 1) * 128], ident_b[:], r=["mbf", "ident_b"], w=[PS[4]])
            for ec in range(2):
                evac_bf("dve", gatesT[:, ec, tok], pv4[:, ec * 128:(ec + 1) * 128], r=[PS[4]], w=["gatesT"])
            dma("sp", h2T_d[:, :, tok], h2T_, r=[K("h2T")], w=[("h2T_d", i)])
            for c in range(8):
                mm(ps[5][:], h2T_[:, c, :], wgu[:, c, :], c == 0, c == 7, r=[K("h2T"), "wgu0", "wgu1"], w=[PS[5]])
            act(sgs, ps[5][:, 0:256], AF.Sigmoid, r=[PS[5]], w=["sgs"])
            tt("dve", sgs, sgs, ps[5][:, 0:256], ALU.mult, r=["sgs", PS[5]], w=["sgs"])
            tt("dve", ash, sgs, ps[5][:, 256:512], ALU.mult, r=["sgs", PS[5]], w=["ash"])
            pv3 = ps[3][:].bitcast(BF16)
            for fk in range(2):
                tr(pv3[:, 512 + fk * 128:512 + (fk + 1) * 128], ash[:, fk * 128:(fk + 1) * 128], ident_b[:], r=["ash", "ident_b"], w=[PS[3]])
            evac_bf("dve", ashT.rearrange("p a b -> p (a b)"), pv3[:, 512:768], r=[PS[3]], w=["ashT"])
            for nb in range(2):
                for fk in range(2):
                    mm(ps[6 + nb][:], ashT[:, fk, :], wds[:, fk, nb * 512:(nb + 1) * 512], fk == 0, fk == 1, r=["ashT", "wds"], w=[PS[6 + nb]])
                nbs = slice(nb * 512, (nb + 1) * 512)
                tt("dve", x_t[:, nbs], ps[6 + nb][:], bct["g2"][:, nbs], ALU.mult, r=[PS[6 + nb], "bc_g2", K("xt2")], w=[K("xt2")])
            tt("pool", x1, x1, x_t, ALU.add, r=[K("x1"), K("xt2")], w=[K("x1")])
            dma("sp", x1p_d[tok, :], x1, r=[K("x1")], w=[("x1p_d", i)])

        for i in range(NT):
            phaseF_tile(i)
        S.barrier()
        if stage <= 6:
            S.emit(); return nc

        NEXP = int(os.environ.get("KNE", str(NE)))
        h2Th = carve(0, [128, 8, 2048], BF16)
        wring = [(carve(32 + 12 * r, [128, 8, 256], BF16), carve(36 + 12 * r, [128, 8, 256], BF16), carve(40 + 12 * r, [128, 2, 1024], BF16)) for r in range(3)]
        selb = [carve(68 + 0.25 * i, [128, 128], BF16) for i in range(2)]
        ab_ = [carve(69 + 2 * i, [128, 2, 512], BF16) for i in range(2)]
        accT = carve(100, [128, 8, 2048])
        sgb_ = [carve(164 + 4 * i, [128, 2, 512]) for i in range(2)]
        t2b_ = [carve(172 + 4 * i, [128, 2, 512]) for i in range(2)]
        gsb_ = [carve(180 + i, [128, 512], BF16) for i in range(2)]
        xo = [carve(182 + 4 * i, [128, 1024]) for i in range(2)]
        wge = I["w_gate_e"]; wue = I["w_up_e"]; wde = I["w_down_e"]
        gstep = 0
        for hh in range(2):
            for c in range(8):
                dma("sp", h2Th[:, c, :], h2T_d[:, c, hh * 2048:(hh + 1) * 2048], w=[("h2Th", c)])
            H2 = [("h2Th", c) for c in range(8)]
            for e in range(NEXP):
                r = (hh * NEXP + e) % 3
                wg, wu, wd = wring[r]
                dma("pool", wg, wge[e].rearrange("(ko p) n -> p ko n", p=128), w=[("wg", r)])
                dma("pool", wu, wue[e].rearrange("(ko p) n -> p ko n", p=128), w=[("wu", r)])
                dma("pool", wd, wde[e].rearrange("(ko p) n -> p ko n", p=128), w=[("wd", r)])
                ec, ej = e // 128, e % 128
                sl = selb[e % 2]
                evac_bf("dve", sl, ident_b[:, ej:ej + 1].to_broadcast([128, 128]), r=["ident_b"], w=[("sel", e % 2)])
                for tb in range(4):
                    u_ = gstep % 2
                    gstep += 1
                    tbs = slice(tb * 512, (tb + 1) * 512)
                    gts = slice(hh * 2048 + tb * 512, hh * 2048 + (tb + 1) * 512)
                    sg, a_, gs, t2 = sgb_[u_], ab_[u_], gsb_[u_], t2b_[u_]
                    for fk in range(2):
                        for c in range(8):
                            mm(ps[fk][:], wg[:, c, fk * 128:(fk + 1) * 128], h2Th[:, c, tbs], c == 0, c == 7, r=[("wg", r)] + H2, w=[PS[fk]])
                    for fk in range(2):
                        for c in range(8):
                            mm(ps[2 + fk][:], wu[:, c, fk * 128:(fk + 1) * 128], h2Th[:, c, tbs], c == 0, c == 7, r=[("wu", r)] + H2, w=[PS[2 + fk]])
                    mm(ps[4][:], sl, gatesT[:, ec, gts], True, True, r=[("sel", e % 2), "gatesT"], w=[PS[4]])
                    for fk in range(2):
                        act(sg[:, fk, :], ps[fk][:], AF.Sigmoid, r=[PS[fk]], w=[("sg", u_)])
                    for fk in range(2):
                        tt("dve", t2[:, fk, :], sg[:, fk, :], ps[fk][:], ALU.mult, r=[("sg", u_), PS[fk]], w=[("t2", u_)])
                    for fk in range(2):
                        tt("dve", t2[:, fk, :], t2[:, fk, :], ps[2 + fk][:], ALU.mult, r=[("t2", u_), PS[2 + fk]], w=[("t2", u_)])
                    act(gs, ps[4][:], AF.Copy, r=[PS[4]], w=[("gs", u_)])
                    for fk in range(2):
                        tt("pool", a_[:, fk, :], t2[:, fk, :], gs, ALU.mult, r=[("t2", u_), ("gs", u_)], w=[("a", u_)])
                    for dc in range(8):
                        bD = 5 + dc % 3
                        for fk in range(2):
                            mm(ps[bD][:], wd[:, fk, dc * 128:(dc + 1) * 128], a_[:, fk, :], fk == 0, fk == 1, r=[("wd", r), ("a", u_)], w=[PS[bD]])
                        if e == 0:
                            if dc % 2 == 0:
                                cp("dve", accT[:, dc, tbs], ps[bD][:], r=[PS[bD]], w=[("acc", dc, tb)])
                            else:
                                act(accT[:, dc, tbs], ps[bD][:], AF.Copy, r=[PS[bD]], w=[("acc", dc, tb)])
                        else:
                            tt("dve", accT[:, dc, tbs], accT[:, dc, tbs], ps[bD][:], ALU.add, r=[PS[bD], ("acc", dc, tb)], w=[("acc", dc, tb)])
            for dc in range(8):
                act(accT[:, dc, :], accT[:, dc, :], AF.Copy, r=[("acc", dc, tb) for tb in range(4)], w=[("acc", dc, tb) for tb in range(4)], scale=mod[:, 40 + dc:41 + dc])
            for tl in range(16):
                i = hh * 16 + tl
                tok = slice(i * 128, (i + 1) * 128)
                xo_ = xo[i % 2]
                dma("sp", xo_, x1p_d[tok, :], w=[("xo", i % 2)])
                for dc in range(8):
                    nb = dc // 4
                    tr(ps[nb][:, (dc % 4) * 128:(dc % 4 + 1) * 128], accT[:, dc, tl * 128:(tl + 1) * 128], ident_f[:],
                       r=[("acc", dc, tl // 4), "ident_f"], w=[PS[nb]])
                for nb in range(2):
                    nbs = slice(nb * 512, (nb + 1) * 512)
                    tt("dve", xo_[:, nbs], xo_[:, nbs], ps[nb][:], ALU.add, r=[("xo", i % 2), PS[nb]], w=[("xo", i % 2)])
                dma("sp", out[tok, :], xo_, r=[("xo", i % 2)], w=[("out", i)])
        S.barrier()
        S.emit()
    return nc


def host_consts():
    ident = np.eye(128, dtype=np.float32)
    inv_freq = (1.0 / (10000.0 ** (np.arange(0, 64, 2, dtype=np.float32) / 64.0))).astype(np.float32)
    ang = np.arange(T, dtype=np.float32)[:, None] * inv_freq[None, :]
    cs = np.concatenate([np.cos(ang), np.sin(ang)], axis=1).astype(np.float32)
    ropecs = np.ascontiguousarray(cs.reshape(NT, 128, 64).transpose(1, 0, 2))
    tri = np.triu(np.ones((128, 128), np.float32)).astype(ml_dtypes.bfloat16)
    tl = np.arange(4)[:, None, None, None, None, None]; t4 = np.arange(4)[None, None, None, :, None, None]
    hc_r = np.arange(32)[None, :, None, None, None, None]
    tau = np.arange(16)[None, None, None, None, :, None]; hc_c = np.arange(32)[None, None, None, None, None, :]
    causal = np.broadcast_to((tau >= 4 * t4 + tl), (4, 32, 1, 4, 16, 32)).reshape(128, 4, 512)
    eye = np.broadcast_to((tau == 4 * t4 + tl) & (hc_r == hc_c), (4, 32, 1, 4, 16, 32)).reshape(128, 4, 512)
    iota_e = np.ascontiguousarray(np.broadcast_to(np.arange(256, dtype=np.float32)[None, :], (128, 256)))
    triu = np.triu(np.ones((128, 128), np.float32), 1).astype(ml_dtypes.bfloat16)
    return dict(ident_f=ident, ident_b=ident.astype(ml_dtypes.bfloat16), ones_f=np.ones((128, 128), np.float32), ropecs=ropecs, tri=tri,

                w3mask=np.ascontiguousarray(causal.astype(np.float32)).astype(ml_dtypes.bfloat16),
                w3eye=np.ascontiguousarray(eye.astype(np.float32)).astype(ml_dtypes.bfloat16))


def make_in_maps(inp, cores):
    cst = host_consts()
    maps = []
    f = lambda a: np.ascontiguousarray(np.asarray(a, dtype=np.float32))
    gqk = np.concatenate([f(inp["q_norm_g"])[0], f(inp["k_norm_g"])[0]])
    gqk = np.ascontiguousarray(np.broadcast_to(gqk[None, :], (128, 128)))
    lamv = np.concatenate([f(inp[k])[0] for k in ("lambda_q1", "lambda_k1", "lambda_q2", "lambda_k2")])
    lamv = np.ascontiguousarray(np.broadcast_to(lamv[None, :], (128, 256)))
    gsub = np.ascontiguousarray(np.broadcast_to(f(inp["subln_g"])[0][None, :], (128, 128)))
    sp = lambda a: np.ascontiguousarray(a.reshape(16, 128).T)
    ssm_cols = np.ascontiguousarray(np.stack([sp(f(inp["ssm_a_re"])[0]), sp(f(inp["ssm_a_im"])[0]),
                                              sp(np.repeat(f(inp["ssm_log_dt"])[0][:, None], 64, 1))], 1))
    bl = lambda a: a.reshape(16, 128, 16).transpose(1, 0, 2)
    ssm_b = np.ascontiguousarray(np.stack([bl(f(inp["ssm_b_re"])[0]), bl(f(inp["ssm_b_im"])[0])], 1))
    cl = lambda a: a.reshape(16, 2, 16, 64).transpose(1, 3, 0, 2).reshape(128, 16, 16)
    ssm_c = np.ascontiguousarray(np.stack([cl(f(inp["ssm_c_re"])[0]), cl(f(inp["ssm_c_im"])[0])], 1))
    ssm_dcol = np.ascontiguousarray(np.tile(f(inp["ssm_d"])[0].reshape(16, 32), (1, 4)).T)
    rbias = np.ascontiguousarray(np.broadcast_to(f(inp["router_bias"])[0][None, :], (128, 256)))
    for b in cores:
        vs1 = np.zeros((128, 128), np.float32)
        vs1[0:8] = f(inp["c"])[b].reshape(8, 128)
        vs1[8:16] = f(inp["norm1_g"])[0].reshape(8, 128)
        vs1[16:24] = f(inp["norm2_g"])[0].reshape(8, 128)
        vs1[24:72] = f(inp["b_ada"])[0].reshape(48, 128)
        vs1[72:76] = f(inp["ssm_norm_g"])[0].reshape(4, 128)
        m = dict(x=f(inp["x"])[b], vs1=vs1, w_ada=f(inp["w_ada"])[0], w_in=f(inp["w_in"])[0], w_glu=f(inp["w_glu"])[0], gqk=gqk, w_out=f(inp["w_out"])[0], w_router=f(inp["w_router"])[0],
                 w_gate_s=f(inp["w_gate_s"])[0], w_up_s=f(inp["w_up_s"])[0], w_down_s=f(inp["w_down_s"])[0], rbias=rbias, lamv=lamv, gsub=gsub, ssm_cols=ssm_cols, ssm_b=ssm_b, ssm_c=ssm_c, ssm_dcol=ssm_dcol)
        if "w_gate_e" in inp:
            m.update(w_gate_e=f(inp["w_gate_e"])[0], w_up_e=f(inp["w_up_e"])[0], w_down_e=f(inp["w_down_e"])[0])
        m.update(cst)
        maps.append(m)
    return maps


def kernel(**inputs):
    nc = build()
    maps = make_in_maps(inputs, list(range(8)))
    res = run_bass_kernel_spmd(nc, maps, core_ids=list(range(8)))
    return np.stack([r["out"] for r in res.results], axis=0)
```

```python
import contextlib
import math
import numpy as np
import ml_dtypes
import concourse.bass as bass
import concourse.mybir as mybir
from concourse.bass_utils import run_bass_kernel_spmd

F32 = mybir.dt.float32
BF16 = mybir.dt.bfloat16
I32 = mybir.dt.int32
U32 = mybir.dt.uint32
ALU = mybir.AluOpType
AF = mybir.ActivationFunctionType
AX = mybir.AxisListType

import os
NOPOOL = os.environ.get("NOPOOL", "1") == "1"
ENG = ("pe", "act", "dve", "pool", "sp")
NDMA = 48
NDMA_HW = 32
EPS = 1e-6
T = 4096
D = 1024
NT = T // 128
CAP = 256
NE = 256


class Sched:
    def __init__(self, nc):
        self.nc = nc
        self.ops = {e: [] for e in ENG}
        self.last_w = {}
        self.readers = {}
        self.known = {e: {} for e in ENG}
        self.known_dma = {e: set() for e in ENG}
        self.dmas = []
        self.sem_last = [None] * NDMA
        self.sem_cnt = [0] * NDMA
        self.next_sem = 0
        self.next_sw = 0
        self.live_dma = set()

    def _deps(self, eng, reads, writes):
        deps = []
        for t in reads:
            r = self.last_w.get(t)
            if r is not None:
                deps.append(r)
        for t in writes:
            r = self.last_w.get(t)
            if r is not None:
                deps.append(r)
            deps.extend(self.readers.get(t, ()))
        waits = []
        for d in deps:
            if d[0] == "e":
                _, e2, idx = d
                if e2 == eng and eng in ("pe", "sp"):
                    continue
                if self.known[eng].get(e2, -1) >= idx:
                    continue
                self.known[eng][e2] = idx
                self.ops[e2][idx]["need"] = True
                waits.append(d)
            else:
                did = d[1]
                if did in self.known_dma[eng]:
                    continue
                self.known_dma[eng].add(did)
                waits.append(d)
        return waits

    def _commit(self, ref, reads, writes):
        for t in reads:
            self.readers.setdefault(t, []).append(ref)
        for t in writes:
            self.last_w[t] = ref
            self.readers[t] = []

    def op(self, eng, fn, r=(), w=()):
        if eng == "pool" and NOPOOL:
            eng = "dve"
        waits = self._deps(eng, r, w)
        idx = len(self.ops[eng])
        self.ops[eng].append(dict(fn=fn, waits=waits, need=False, dma=None))
        self._commit(("e", eng, idx), r, w)

    def dma(self, eng, fn, r=(), w=()):
        waits = self._deps(eng, r, w)
        if eng == "pool":
            s = NDMA_HW + self.next_sw
            self.next_sw = (self.next_sw + 1) % (NDMA - NDMA_HW)
        else:
            s = self.next_sem
            self.next_sem = (s + 1) % NDMA_HW
        prev = self.sem_last[s]
        if prev is not None and prev not in self.known_dma[eng]:
            self.known_dma[eng].add(prev)
            waits.append(("d", prev))
        self.sem_cnt[s] += 16
        did = len(self.dmas)
        self.dmas.append(dict(sem=s, val=self.sem_cnt[s]))
        self.sem_last[s] = did
        self.ops[eng].append(dict(fn=fn, waits=waits, need=False, dma=did))
        self._commit(("d", did), r, w)
        self.live_dma.add(did)

    def barrier(self):
        waits = []
        for e in ENG:
            if e != "sp" and self.ops[e]:
                idx = len(self.ops[e]) - 1
                while idx >= 0 and self.ops[e][idx]["dma"] is not None:
                    idx -= 1
                if idx < 0 or self.known["sp"].get(e, -1) >= idx:
                    continue
                self.known["sp"][e] = idx
                self.ops[e][idx]["need"] = True
                waits.append(("e", e, idx))
        for did in sorted(self.live_dma):
            if did not in self.known_dma["sp"]:
                self.known_dma["sp"].add(did)
                waits.append(("d", did))
        self.live_dma = set()
        idx = len(self.ops["sp"])
        self.ops["sp"].append(dict(fn=None, waits=waits, need=True, dma=None))
        ref = ("e", "sp", idx)
        for e in ENG:
            if e == "sp":
                continue
            self.known[e]["sp"] = idx
            self.ops[e].append(dict(fn=None, waits=[ref], need=False, dma=None))
            for e2 in ENG:
                if e2 != e and self.ops[e2]:
                    self.known[e][e2] = max(self.known[e].get(e2, -1), len(self.ops[e2]) - 1)
            self.known_dma[e] = set(range(len(self.dmas)))
        self.known_dma["sp"] = set(range(len(self.dmas)))
        self.last_w = {}
        self.readers = {}

    def emit(self):
        nc = self.nc
        with contextlib.ExitStack() as st:
            esem = {e: st.enter_context(nc.semaphore("s_" + e)) for e in ENG}
            dsem = [st.enter_context(nc.semaphore("d%d" % i)) for i in range(NDMA)]
            val = {}
            for e in ENG:
                c = 0
                for i, o in enumerate(self.ops[e]):
                    if o["need"]:
                        c += 1
                        val[(e, i)] = c
            block = st.enter_context(nc.Block())

            def run(e, eng):
                for i, o in enumerate(self.ops[e]):
                    for wt in o["waits"]:
                        if wt[0] == "e":
                            eng.wait_ge(esem[wt[1]], val[(wt[1], wt[2])])
                        else:
                            d = self.dmas[wt[1]]
                            eng.wait_ge(dsem[d["sem"]], d["val"])
                    if o["fn"] is None:
                        if o["need"]:
                            eng.sem_inc(esem[e], 1)
                        continue
                    ins = o["fn"](eng)
                    if o["dma"] is not None:
                        ins.then_inc(dsem[self.dmas[o["dma"]]["sem"]], 16)
                    elif o["need"]:
                        ins.then_inc(esem[e], 1)

            @block.tensor
            def _(eng):
                run("pe", eng)

            @block.scalar
            def _(eng):
                run("act", eng)

            @block.vector
            def _(eng):
                run("dve", eng)

            @block.gpsimd
            def _(eng):
                run("pool", eng)

            @block.sync
            def _(eng):
                run("sp", eng)


IN_SPECS = [
    ("x", [T, D], F32), ("vs1", [128, 128], F32),
    ("w_ada", [D, 6 * D], F32), ("w_in", [D, 2048], F32), ("w_glu", [512, 512], F32), ("w_out", [D, D], F32),
    ("w_router", [D, 256], F32), ("w_gate_s", [D, 256], F32), ("w_up_s", [D, 256], F32), ("w_down_s", [256, D], F32),
    ("rbias", [128, 256], F32),
    ("w_gate_e", [NE, D, 256], F32), ("w_up_e", [NE, D, 256], F32), ("w_down_e", [NE, 256, D], F32),
    ("ident_f", [128, 128], F32), ("ident_b", [128, 128], BF16),
    ("ones_f", [128, 128], F32),
    ("ropecs", [128, NT, 64], F32), ("gqk", [128, 128], F32),
    ("lamv", [128, 256], F32), ("gsub", [128, 128], F32), ("tri", [128, 128], BF16),
    ("w3mask", [128, 4, 512], BF16), ("w3eye", [128, 4, 512], BF16),
    ("ssm_cols", [128, 3, 16], F32), ("ssm_b", [128, 2, 16, 16], F32), ("ssm_c", [128, 2, 16, 16], F32), ("ssm_dcol", [128, 16], F32),
]


def build(stage=99, dbg=()):
    nc = bass.Bass("TRN2", target_bir_lowering=False)
    S = Sched(nc)
    I = {}
    for name, shp, dt in IN_SPECS:
        if stage < 7 and name in ("w_gate_e", "w_up_e", "w_down_e"):
            continue
        I[name] = nc.dram_tensor(name, shp, dt, kind="ExternalInput").ap()
    out = nc.dram_tensor("out", [T, D], F32, kind="ExternalOutput").ap()

    def dma(eng, o, i, r=(), w=()):
        S.dma(eng, lambda e: e.dma_start(out=o, in_=i), r, w)

    def mm(o, l, rh, st_, sp_, r=(), w=()):
        S.op("pe", lambda e: e.matmul(o, l, rh, start=st_, stop=sp_), r, w)

    def tr(o, i, idn, r=(), w=()):
        S.op("pe", lambda e: e.transpose(o, i, idn), r, w)

    def act(o, i, func, r=(), w=(), **kw):
        S.op("act", lambda e: e.activation(out=o, in_=i, func=func, **kw), r, w)

    def ts(eng, o, i, s1, s2, op0, op1=None, r=(), w=(), **kw):
        if op1 is None:
            S.op(eng, lambda e: e.tensor_scalar(out=o, in0=i, scalar1=s1, scalar2=None, op0=op0, **kw), r, w)
        else:
            S.op(eng, lambda e: e.tensor_scalar(out=o, in0=i, scalar1=s1, scalar2=s2, op0=op0, op1=op1, **kw), r, w)

    def tt(eng, o, a, b, op, r=(), w=()):
        S.op(eng, lambda e: e.tensor_tensor(out=o, in0=a, in1=b, op=op), r, w)

    def stt(eng, o, a, s, b, op0, op1, r=(), w=()):
        S.op(eng, lambda e: e.scalar_tensor_tensor(out=o, in0=a, scalar=s, in1=b, op0=op0, op1=op1), r, w)

    def cp(eng, o, i, r=(), w=()):
        if eng == "act":
            S.op(eng, lambda e: e.activation(out=o, in_=i, func=AF.Copy), r, w)
        else:
            S.op(eng, lambda e: e.tensor_copy(out=o, in_=i), r, w)

    import os
    KDIS = os.environ.get("KDIS", "").split(",")

    def dbg_dump(name, src, shp, dt, r, force=False):
        if name in dbg or force:
            d = nc.dram_tensor("dbg_" + name, shp, dt, kind="ExternalOutput").ap()
            dma("sp", d, src, r=r, w=["dbg_" + name])

    with contextlib.ExitStack() as st:
        def sb(name, shp, dt=F32):
            return st.enter_context(nc.sbuf_tensor("sb_" + name, shp, dt))

        ps = [st.enter_context(nc.psum_tensor("ps%d" % b, [128, 512], F32)) for b in range(8)]
        PS = [("ps", b) for b in range(8)]
        ident_f = sb("ident_f", [128, 128]); ident_b = sb("ident_b", [128, 128], BF16)
        ones_f = sb("ones_f", [128, 128]); vc1 = sb("vc1", [128, 128])
        ARENA_KIB = 192
        arena = sb("arena", [128, ARENA_KIB * 256])

        def carve(off_kib, shp, dt=F32):
            n = 1
            for d_ in shp[1:]:
                n *= d_
            nbytes = n * (2 if dt == BF16 else 4)
            o4 = int(round(off_kib * 256))
            assert abs(o4 - off_kib * 256) < 1e-9 and nbytes % 4 == 0
            assert o4 * 4 + nbytes <= ARENA_KIB * 1024, (off_kib, shp)
            v = arena[:, o4:o4 + nbytes // 4]
            if dt != F32:
                v = v.bitcast(dt)
            if len(shp) == 2:
                return v
            names = " ".join("d%d" % i for i in range(1, len(shp)))
            kw = {"d%d" % i: shp[i] for i in range(2, len(shp))}
            return v.rearrange("p (%s) -> p %s" % (names, names), **kw)

        dma("sp", ident_f[:], I["ident_f"], w=["ident_f"])
        dma("sp", ident_b[:], I["ident_b"], w=["ident_b"])
        dma("sp", ones_f[:], I["ones_f"], w=["ones_f"])
        oz = sb("oz", [128, 2])
        S.op("dve", lambda e: e.memset(oz[:, 0:1], 1.0), w=["zc"])
        S.op("dve", lambda e: e.memset(oz[:, 1:2], 0.0), r=["zc"], w=["zc"])
        onec = oz[:, 0:1]
        zc = oz[:, 1:2]

        def evac_bf(eng, o, i, r=(), w=()):
            rr = list(r) + ["ones_f", "zc"]
            if True:
                S.op("act", lambda e: e.activation(out=o, in_=i, func=AF.Identity, scale=onec, bias=zc), rr, w)
            else:
                S.op("dve", lambda e: e.tensor_scalar(out=o, in0=i, scalar1=onec, scalar2=zc, op0=ALU.mult, op1=ALU.add), rr, w)

        vs1 = sb("vs1", [128, 128])
        dma("sp", vs1[:], I["vs1"], w=["vs1"])
        act(vs1[0:8, :], vs1[0:8, :], AF.Silu, r=["vs1"], w=["vs1"])
        tr(ps[0][:, 0:128], vs1[:], ident_f[:], r=["vs1", "ident_f"], w=[PS[0]])
        cp("dve", vc1[:], ps[0][:, 0:128], r=[PS[0]], w=["vc1"])
        mod = sb("mod", [128, 48])
        wada = [carve(24 * i, [128, 8, 768]) for i in range(2)]
        wada_src = I["w_ada"].rearrange("(ko p) n -> p ko n", p=128)
        for blk in range(8):
            wt = wada[blk % 2]
            tk = ("wada", blk % 2)
            dma("sp", wt, wada_src[:, :, blk * 768:(blk + 1) * 768], w=[tk])
            for jj in range(6):
                j = blk * 6 + jj
                for k in range(8):
                    mm(ps[1][:, j:j + 1], wt[:, k, jj * 128:(jj + 1) * 128], vc1[:, k:k + 1], k == 0, k == 7, r=[tk, "vc1"], w=[PS[1]])
        tt("dve", mod[:], ps[1][:, 0:48], vc1[:, 24:72], ALU.add, r=[PS[1], "vc1"], w=["mod"])
        ab = sb("ab", [128, 32])
        stt("dve", ab[:, 0:8], mod[:, 8:16], 1.0, vc1[:, 8:16], ALU.add, ALU.mult, r=["mod", "vc1"], w=["ab"])
        cp("dve", ab[:, 8:16], mod[:, 0:8], r=["mod", "ab"], w=["ab"])
        stt("dve", ab[:, 16:24], mod[:, 32:40], 1.0, vc1[:, 16:24], ALU.add, ALU.mult, r=["mod", "vc1", "ab"], w=["ab"])
        cp("dve", ab[:, 24:32], mod[:, 24:32], r=["mod", "ab"], w=["ab"])
        dbg_dump("mod", mod[:], [128, 48], F32, ["mod"])
        S.barrier()
        if stage <= 0:
            S.emit(); return nc

        win = carve(0, [128, 8, 2048], BF16)
        qT = carve(32, [128, 4, T], BF16); kT = carve(64, [128, 4, T], BF16)
        vext = carve(96, [128, NT, 4 * 130], BF16)
        hT = carve(130, [128, 8, 2048], BF16)
        qkr = [carve(162 + 2 * i, [128, 1024], BF16) for i in range(2)]
        xn = [carve(166 + 2 * i, [128, D], BF16) for i in range(2)]
        xt = [carve(170 + 4 * i, [128, D]) for i in range(2)]
        ust = [carve(178 + i, [128, 512], BF16) for i in range(2)]
        sqb = carve(180, [128, 1024]); t1 = carve(184, [128, 1024])
        m1 = sqb[:, 0:512]; m2 = sqb[:, 512:1024]; m3 = carve(188, [128, 512]); m4 = carve(190, [128, 512])
        gqk = sb("gqk", [128, 128])
        stat = sb("stat", [128, 2 * NT]); st16 = sb("st16", [128, NT, 16])
        rcs = [sb("rcs%d" % i, [128, 64]) for i in range(2)]
        u_d = nc.dram_tensor("scr_u", [T, 512], BF16, kind="ExternalOutput").ap()
        win_src = I["w_in"].rearrange("(ko p) n -> p ko n", p=128)
        for k in range(8):
            for hf in range(2):
                if "wincast" not in KDIS:
                    dma("pool", win[:, k, hf * 1024:(hf + 1) * 1024], win_src[:, k, hf * 1024:(hf + 1) * 1024], w=[("win", k, hf)])
        WIN = [("win", k, hf) for k in range(8) for hf in range(2)]
        dma("sp", gqk[:], I["gqk"], w=["gqk"])
        if "memset" not in KDIS:
            S.op("pool", lambda e: e.memset(vext.rearrange("p n (h e) -> p n h e", e=130)[:, :, :, 128:129], 1.0), w=["vext1"])

        def phaseB_tile(i):
            s = i % 2
            x_t, xn_t = xt[s], xn[s]
            tx, tn = ("xt", s), ("xn", s)
            pb = 2 + (i % 2)
            il = i % 16
            ss, rs = stat[:, 2 * i:2 * i + 1], stat[:, 2 * i + 1:2 * i + 2]
            dma("sp", x_t, I["x"][i * 128:(i + 1) * 128, :], w=[tx])
            act(xn_t, x_t, AF.Square, r=[tx], w=[tn, ("stat", i)], accum_out=ss, scale=1.0 / math.sqrt(D))
            ts("dve", rs, ss, EPS, None, ALU.add, r=[("stat", i)], w=[("stat", i)])
            act(rs, rs, AF.Sqrt, r=[("stat", i)], w=[("stat", i)])
            S.op("dve", lambda e: e.reciprocal(out=rs, in_=rs), r=[("stat", i)], w=[("stat", i)])
            act(xn_t, x_t, AF.Copy, r=[tx, ("stat", i)], w=[tn], scale=rs)
            pv = ps[pb][:].bitcast(BF16)
            for c in range(8):
                tr(pv[:, c * 128:(c + 1) * 128], xn_t[:, c * 128:(c + 1) * 128], ident_b[:], r=[tn, "ident_b"], w=[PS[pb]])
            for c in range(8):
                o = hT[:, c, il * 128:(il + 1) * 128]
                src = pv[:, c * 128:(c + 1) * 128]
                if c % 2 == 0:
                    ts("dve", o, src, ab[:, c:c + 1], ab[:, 8 + c:9 + c], ALU.mult, ALU.add, r=[PS[pb], "ab"], w=[("hT", il)])
                else:
                    act(o, src, AF.Identity, r=[PS[pb], "ab"], w=[("hT", il)], scale=ab[:, c:c + 1], bias=ab[:, 8 + c:9 + c])

        def phaseC_tile(i):
            s = i % 2
            il = i % 16
            bq, bk = (0, 1) if s == 0 else (4, 5)
            bt = 2 + s
            tok = slice(i * 128, (i + 1) * 128)
            tl = slice(il * 128, (il + 1) * 128)
            hdep = [("hT", il)]
            dma("sp", rcs[s][:], I["ropecs"][:, i, :], w=[("rcs", s)])
            for c in range(8):
                mm(ps[bq][:], hT[:, c, tl], win[:, c, 0:512], c == 0, c == 7, r=hdep + WIN, w=[PS[bq]])
            for c in range(8):
                mm(ps[bk][:], hT[:, c, tl], win[:, c, 512:1024], c == 0, c == 7, r=hdep + WIN, w=[PS[bk]])
            for c in range(8):
                mm(ps[6][:], hT[:, c, tl], win[:, c, 1024:1536], c == 0, c == 7, r=hdep + WIN, w=[PS[6]])
            for c in range(8):
                mm(ps[7][:], hT[:, c, tl], win[:, c, 1536:2048], c == 0, c == 7, r=hdep + WIN, w=[PS[7]])
            act(vext[:, i, :].rearrange("p (h e) -> p h e", e=130)[:, :, 0:128], ps[6][:].rearrange("p (h e) -> p h e", e=128), AF.Copy,
                r=[PS[6]], w=[("vext", i)])
            cp("dve", ust[s][:], ps[7][:], r=[PS[7]], w=[("ust", s)])
            if "ubmd" not in KDIS:
                dma("sp", u_d[tok, :], ust[s][:], r=[("ust", s)], w=[("u_d", i // 16)])
            KC = int(os.environ.get("KC", "9"))
            if KC < 2:
                return
            act(sqb[:, 0:512], ps[bq][:], AF.Square, r=[PS[bq]], w=["sqb"])
            act(sqb[:, 512:1024], ps[bk][:], AF.Square, r=[PS[bk], "sqb"], w=["sqb"])
            s16 = st16[:, i, :]
            S.op("dve", lambda e: e.tensor_reduce(out=s16, in_=sqb.rearrange("p (a b) -> p a b", b=64), axis=AX.X, op=ALU.add), r=["sqb"], w=[("st16", i)])
            ts("dve", s16, s16, 1.0 / 64, EPS, ALU.mult, ALU.add, r=[("st16", i)], w=[("st16", i)])
            act(s16, s16, AF.Sqrt, r=[("st16", i)], w=[("st16", i)])
            S.op("dve", lambda e: e.reciprocal(out=s16, in_=s16), r=[("st16", i)], w=[("st16", i)])
            tt("dve", t1[:, 0:512].rearrange("p (a b) -> p a b", b=64), ps[bq][:].rearrange("p (a b) -> p a b", b=64),
               st16[:, i, 0:8].unsqueeze(2).to_broadcast([128, 8, 64]), ALU.mult, r=[PS[bq], ("st16", i)], w=["t1"])
            tt("dve", t1[:, 512:1024].rearrange("p (a b) -> p a b", b=64), ps[bk][:].rearrange("p (a b) -> p a b", b=64),
               st16[:, i, 8:16].unsqueeze(2).to_broadcast([128, 8, 64]), ALU.mult, r=[PS[bk], ("st16", i), "t1"], w=["t1"])
            tt("dve", t1.rearrange("p (k a d) -> p k a d", k=2, d=64), t1.rearrange("p (k a d) -> p k a d", k=2, d=64),
               gqk[:].rearrange("p (k d) -> p k d", d=64).unsqueeze(2).to_broadcast([128, 2, 8, 64]), ALU.mult, r=["t1", "gqk"], w=["t1"])
            if KC < 3:
                return
            tv = t1.rearrange("p (a two d) -> p a two d", two=2, d=32)
            ta, tb = tv[:, :, 0, :], tv[:, :, 1, :]
            cosb = rcs[s][:, 0:32].unsqueeze(1).to_broadcast([128, 16, 32])
            sinb = rcs[s][:, 32:64].unsqueeze(1).to_broadcast([128, 16, 32])
            qk_t = qkr[s]
            ov = qk_t.rearrange("p (a two d) -> p a two d", two=2, d=32)
            v3 = lambda t: t.rearrange("p (a d) -> p a d", d=32)
            rc = [("rcs", s)]
            tt("dve", v3(m1), ta, cosb, ALU.mult, r=["t1", "sqb"] + rc, w=["sqb"])
            tt("dve", v3(m2), tb, sinb, ALU.mult, r=["t1", "sqb"] + rc, w=["sqb"])
            tt("dve", ov[:, :, 0, :], v3(m1), v3(m2), ALU.subtract, r=["sqb"], w=[("qkr", s)])
            pe_ = "dve" if "pool" in KDIS else "pool"
            tt(pe_, v3(m3), ta, sinb, ALU.mult, r=["t1"] + rc, w=["m3"])
            tt(pe_, v3(m4), tb, cosb, ALU.mult, r=["t1"] + rc, w=["m4"])
            tt(pe_, ov[:, :, 1, :], v3(m3), v3(m4), ALU.add, r=["m3", "m4", ("qkr", s)], w=[("qkr", s)])
            if KC < 4:
                return
            pv = ps[bt][:].bitcast(BF16)
            for c in range(8):
                tr(pv[:, c * 128:(c + 1) * 128], qk_t[:, c * 128:(c + 1) * 128], ident_b[:], r=[("qkr", s), "ident_b"], w=[PS[bt]])
            pv3 = pv.rearrange("p (c t) -> p c t", t=128)
            KV = os.environ.get("KV", "abc")
            for c in range(4):
                if "b" in KV:
                    evac_bf("act", qT[:, c, tok], pv3[:, c, :], r=[PS[bt]], w=[("qT", i)])
                if "c" in KV:
                    evac_bf("act", kT[:, c, tok], pv3[:, 4 + c, :], r=[PS[bt]], w=[("kT", i)])

        for half in range(2):
            for i in range(16 * half, 16 * half + 16):
                phaseB_tile(i)
            if half == 0:
                for c in range(8):
                    dbg_dump("hT%d" % c, hT[:, c, :], [128, 2048], BF16, [("hT", i) for i in range(16)])
            for i in range(16 * half, 16 * half + 16):
                if stage >= 2:
                    phaseC_tile(i)
        dbg_dump("qTs", qT[:, 1, 1024:2048], [128, 1024], BF16, [("qT", i) for i in range(NT)])
        dbg_dump("kTs", kT[:, 2, 3072:4096], [128, 1024], BF16, [("kT", i) for i in range(NT)])
        for h in range(4):
            dbg_dump("qT%d" % h, qT[:, h, :], [128, T], BF16, [("qT", i) for i in range(NT)])
            dbg_dump("kT%d" % h, kT[:, h, :], [128, T], BF16, [("kT", i) for i in range(NT)])
        for q4 in range(4):
            dbg_dump("vext%d" % q4, vext[:, q4 * 8:(q4 + 1) * 8, :], [128, 8, 520], BF16, [("vext", i) for i in range(NT)] + ["vext1"])
        negb = sb("negb", [128, 4])
        S.op("dve", lambda e: e.tensor_reduce(out=negb[:, 0:1], in_=gqk[:, 0:64], axis=AX.X, op=ALU.max, apply_absolute_value=True), r=["gqk"], w=["negb"])
        S.op("dve", lambda e: e.tensor_reduce(out=negb[:, 1:2], in_=gqk[:, 64:128], axis=AX.X, op=ALU.max, apply_absolute_value=True), r=["gqk", "negb"], w=["negb"])
        stt("dve", negb[:, 2:3], negb[:, 0:1], -8.0, negb[:, 1:2], ALU.mult, ALU.mult, r=["negb"], w=["negb"])
        S.barrier()
        if stage <= 2:
            S.emit(); return nc

        PI = math.pi
        scol = carve(138, [128, 3, 16])
        sw = carve(138.25, [128, 16, 16])
        bbp = carve(139.25, [128, 2, 16, 32])
        ccp = carve(143.25, [128, 2, 16, 32])
        pw = carve(147.25, [128, 2, 16, 17])
        pwn = carve(149.375, [128, 2, 16, 17])
        pwr = carve(151.5, [128, 2, 16, 16])
        a2k = carve(153.5, [128, 2, 16, 8])
        dcol = carve(154.5, [128, 16])
        btmp = carve(154.75, [128, 2, 16, 16])
        ctmp = carve(156.75, [128, 2, 16, 16])
        dma("sp", scol, I["ssm_cols"], w=["scol"])
        dma("sp", btmp, I["ssm_b"], w=["btmp"])
        dma("sp", ctmp, I["ssm_c"], w=["ctmp"])
        dma("sp", dcol, I["ssm_dcol"], w=["dcol"])
        SW = lambda k: sw[:, k, :]
        are, aim, ldt = scol[:, 0, :], scol[:, 1, :], scol[:, 2, :]
        W_ = ["sw"]

        def dv(o, a, b, op):
            tt("dve", o, a, b, op, r=W_ + ["scol"], w=W_)

        def dsc(o, a, s1, s2, op0, op1=None):
            ts("dve", o, a, s1, s2, op0, op1, r=W_ + ["scol"], w=W_)
        act(SW(0), ldt, AF.Exp, r=["scol"], w=W_)
        dv(SW(1), are, SW(0), ALU.mult)
        act(SW(2), SW(1), AF.Exp, r=W_, w=W_)
        dv(SW(3), aim, SW(0), ALU.mult)
        def range_reduce(dst, shift):
            dsc(dst, SW(3), shift, None, ALU.add)
            for _ in range(8):
                dsc(SW(14), dst, PI, None, ALU.is_gt)
                stt("dve", dst, SW(14), -2 * PI, dst, ALU.mult, ALU.add, r=W_, w=W_)
        range_reduce(SW(4), 0.0)
        range_reduce(SW(5), 0.5 * PI)
        act(SW(4), SW(4), AF.Sin, r=W_, w=W_)
        act(SW(5), SW(5), AF.Sin, r=W_, w=W_)
        dv(SW(6), SW(2), SW(5), ALU.mult)
        dv(SW(7), SW(2), SW(4), ALU.mult)
        dv(SW(8), are, are, ALU.mult)
        dv(SW(9), aim, aim, ALU.mult)
        dv(SW(8), SW(8), SW(9), ALU.add)
        S.op("dve", lambda e: e.reciprocal(out=SW(8), in_=SW(8)), r=W_, w=W_)
        dsc(SW(9), SW(6), -1.0, None, ALU.add)
        dv(SW(10), SW(9), are, ALU.mult)
        dv(SW(11), SW(7), aim, ALU.mult)
        dv(SW(10), SW(10), SW(11), ALU.add)
        dv(SW(10), SW(10), SW(8), ALU.mult)
        dv(SW(11), SW(7), are, ALU.mult)
        dv(SW(12), SW(9), aim, ALU.mult)
        dv(SW(11), SW(11), SW(12), ALU.subtract)
        dv(SW(11), SW(11), SW(8), ALU.mult)
        dv(SW(12), SW(2), SW(2), ALU.mult)
        S.op("dve", lambda e: e.reciprocal(out=SW(12), in_=SW(12)), r=W_, w=W_)
        dv(SW(13), SW(6), SW(12), ALU.mult)
        dv(SW(14), SW(7), SW(12), ALU.mult)
        dsc(SW(14), SW(14), -1.0, None, ALU.mult)
        S.op("pool", lambda e: e.memset(bbp, 0.0), w=["bbp"])
        S.op("pool", lambda e: e.memset(ccp, 0.0), w=["ccp"])
        fre_b = SW(10).unsqueeze(2).to_broadcast([128, 16, 16])
        fim_b = SW(11).unsqueeze(2).to_broadcast([128, 16, 16])
        bre, bim = btmp[:, 0], btmp[:, 1]
        t_a = ctmp
        x1_, x2_ = pwr[:, 0], pwr[:, 1]
        tt("dve", x1_, bre, fre_b, ALU.mult, r=["btmp"] + W_, w=["pwr"])
        tt("dve", x2_, bim, fim_b, ALU.mult, r=["btmp"] + W_ + ["pwr"], w=["pwr"])
        tt("dve", x1_, x1_, x2_, ALU.subtract, r=["pwr"], w=["pwr"])
        for hh in range(2):
            cp("dve", bbp[64 * hh:64 * hh + 64, 0, :, 16 * hh:16 * hh + 16], x1_[64 * hh:64 * hh + 64], r=["pwr", "bbp"], w=["bbp"])
        tt("dve", x1_, bim, fre_b, ALU.mult, r=["btmp", "bbp"] + W_ + ["pwr"], w=["pwr"])
        tt("dve", x2_, bre, fim_b, ALU.mult, r=["btmp"] + W_ + ["pwr"], w=["pwr"])
        tt("dve", x1_, x1_, x2_, ALU.add, r=["pwr"], w=["pwr"])
        for hh in range(2):
            cp("dve", bbp[64 * hh:64 * hh + 64, 1, :, 16 * hh:16 * hh + 16], x1_[64 * hh:64 * hh + 64], r=["pwr", "bbp"], w=["bbp"])
            for ri in range(2):
                cp("dve", ccp[64 * hh:64 * hh + 64, ri, :, 16 * hh:16 * hh + 16], ctmp[64 * hh:64 * hh + 64, ri], r=["ctmp", "ccp"], w=["ccp"])
        PWT = ["pw", "pwn", "pwr", "a2k"]

        def cmul_col(ore, oim, xre, xim, yre, yim, tmp1, tmp2):
            tt("dve", tmp1, xre, yre, ALU.mult, r=PWT + W_, w=PWT + W_)
            tt("dve", tmp2, xim, yim, ALU.mult, r=PWT + W_, w=PWT + W_)
            tt("dve", oim, xre, yim, ALU.mult, r=PWT + W_, w=PWT + W_)
            tt("dve", ore, tmp1, tmp2, ALU.subtract, r=PWT + W_, w=PWT + W_)
            tt("dve", tmp1, xim, yre, ALU.mult, r=PWT + W_, w=PWT + W_)
            tt("dve", oim, oim, tmp1, ALU.add, r=PWT + W_, w=PWT + W_)
        S.op("dve", lambda e: e.memset(pw[:, 0, :, 0:1], 1.0), r=PWT, w=PWT)
        S.op("dve", lambda e: e.memset(pw[:, 1, :, 0:1], 0.0), r=PWT, w=PWT)
        S.op("dve", lambda e: e.memset(pwn[:, 0, :, 0:1], 1.0), r=PWT, w=PWT)
        S.op("dve", lambda e: e.memset(pwn[:, 1, :, 0:1], 0.0), r=PWT, w=PWT)
        for k in range(16):
            cmul_col(pw[:, 0, :, k + 1], pw[:, 1, :, k + 1], pw[:, 0, :, k], pw[:, 1, :, k], SW(6), SW(7), SW(0), SW(1))
            cmul_col(pwn[:, 0, :, k + 1], pwn[:, 1, :, k + 1], pwn[:, 0, :, k], pwn[:, 1, :, k], SW(13), SW(14), SW(0), SW(1))
        for tau in range(16):
            cp("dve", pwr[:, :, :, tau], pw[:, :, :, 15 - tau], r=PWT, w=PWT)
        cp("dve", a2k[:, :, :, 0], pw[:, :, :, 16], r=PWT, w=PWT)
        for l in range(7):
            cmul_col(a2k[:, 0, :, l + 1], a2k[:, 1, :, l + 1], a2k[:, 0, :, l], a2k[:, 1, :, l], a2k[:, 0, :, l], a2k[:, 1, :, l], SW(0), SW(1))
        if "ssmpar" in dbg:
            dbg_dump("pw", pw, [128, 2, 16, 17], F32, PWT, True)
            dbg_dump("pwn", pwn, [128, 2, 16, 17], F32, PWT, True)
            dbg_dump("a2k", a2k, [128, 2, 16, 8], F32, PWT, True)
            dbg_dump("bbp", bbp, [128, 2, 16, 32], F32, ["bbp"], True)

        attT = carve(0, [128, 4, T], BF16)
        pt = [carve(130 + i, [128, 512], BF16) for i in range(2)]
        lamv = carve(132, [128, 256]); lamp = carve(133, [128, 128])
        gsub = carve(133.5, [128, 128]); tri = carve(134, [128, 128], BF16)
        osb = [carve(135 + 0.5 * i, [128, 128]) for i in range(2)]
        onb = [carve(136 + 0.25 * i, [128, 128], BF16) for i in range(2)]
        osq = carve(136.5, [128, 128], BF16)
        lamc = sb("lamc", [128, 8])
        ast = carve(137, [128, 128, 4])
        dma("sp", lamv, I["lamv"], w=["lamv"])
        dma("sp", gsub, I["gsub"], w=["gsub"])
        dma("sp", tri, I["tri"], w=["tri"])
        tt("dve", lamp, lamv.rearrange("p (a two d) -> p a two d", two=2, d=64)[:, :, 0, :], lamv.rearrange("p (a two d) -> p a two d", two=2, d=64)[:, :, 1, :],
           ALU.mult, r=["lamv"], w=["lamp"])
        S.op("dve", lambda e: e.tensor_reduce(out=lamc[:, 0:2], in_=lamp.rearrange("p (a d) -> p a d", d=64), axis=AX.X, op=ALU.add), r=["lamp"], w=["lamc"])
        act(lamc[:, 2:4], lamc[:, 0:2], AF.Exp, r=["lamc"], w=["lamc"])
        stt("dve", lamc[:, 4:5], lamc[:, 3:4], -0.2, lamc[:, 2:3], ALU.add, ALU.subtract, r=["lamc"], w=["lamc"])
        ts("dve", gsub, gsub, 0.8, None, ALU.mult, r=["gsub"], w=["gsub"])
        neglam = lamc[:, 4:5]
        step = 0
        for h in range(0 if "att" not in KDIS else 4, 4):
            for Q2 in range(16):
                nkt = 2 * Q2 + 2
                for kt in range(nkt):
                    s = step % 2
                    step += 1
                    for c in range(2):
                        mm(ps[2 * s + c][:, 0:256], kT[64 * c:64 * c + 64, h, kt * 128:(kt + 1) * 128], qT[64 * c:64 * c + 64, h, Q2 * 256:(Q2 + 1) * 256],
                           True, True, r=[("kT", kt), ("qT", 2 * Q2), ("qT", 2 * Q2 + 1)], w=[PS[2 * s + c]])
                    for c in range(2):
                        act(pt[s][:, c * 256:(c + 1) * 256], ps[2 * s + c][:, 0:256], AF.Exp, r=[PS[2 * s + c], "negb", ("pt", s)], w=[("pt", s)], scale=0.125, bias=negb[:, 2:3])
                    KA = int(os.environ.get("KA", "9"))
                    if kt >= 2 * Q2 and KA >= 2:
                        r0 = kt - 2 * Q2
                        pv_ = pt[s].rearrange("p (c q) -> p c q", q=256)[:, :, r0 * 128:(r0 + 1) * 128]
                        tt("pool", pv_, pv_, tri.unsqueeze(1).to_broadcast([128, 2, 128]), ALU.mult, r=[("pt", s), "tri"], w=[("pt", s)])
                    for r in range(2):
                        if 2 * Q2 + r < kt or KA < 3:
                            continue
                        for c in range(2):
                            b = 4 + 2 * c + r
                            mm(ps[b][:, 0:129], pt[s][:, c * 256 + r * 128:c * 256 + (r + 1) * 128], vext[:, kt, h * 130:h * 130 + 129],
                               kt == 0, kt == 2 * Q2 + r, r=[("pt", s), ("vext", kt), "vext1"], w=[PS[b]])
                for r in range(2 if KA >= 4 else 0):
                    qt = 2 * Q2 + r
                    u_ = (h * 32 + qt) % 2
                    a4 = ast[:, qt, :] if h == 0 else ast[:, (h * 32 + qt) % 128, :]
                    tkn = ("ast", (h * 32 + qt) % 128)
                    b0, b1 = 4 + r, 6 + r
                    S.op("dve", (lambda a4, b0: lambda e: e.reciprocal(out=a4[:, 0:1], in_=ps[b0][:, 128:129]))(a4, b0), r=[PS[b0]], w=[tkn])
                    S.op("dve", (lambda a4, b1: lambda e: e.reciprocal(out=a4[:, 1:2], in_=ps[b1][:, 128:129]))(a4, b1), r=[PS[b1], tkn], w=[tkn])
                    tt("dve", a4[:, 1:2], a4[:, 1:2], neglam, ALU.mult, r=[tkn, "lamc"], w=[tkn])
                    ts("dve", osb[u_], ps[b0][:, 0:128], a4[:, 0:1], None, ALU.mult, r=[PS[b0], tkn], w=[("osb", u_)])
                    stt("dve", osb[u_], ps[b1][:, 0:128], a4[:, 1:2], osb[u_], ALU.mult, ALU.add, r=[PS[b1], tkn, ("osb", u_)], w=[("osb", u_)])
                    act(osq, osb[u_], AF.Square, r=[("osb", u_)], w=["osq", tkn], accum_out=a4[:, 2:3], scale=1.0 / math.sqrt(128.0))
                    ts("dve", a4[:, 2:3], a4[:, 2:3], EPS, None, ALU.add, r=[tkn], w=[tkn])
                    act(a4[:, 2:3], a4[:, 2:3], AF.Sqrt, r=[tkn], w=[tkn])
                    S.op("dve", (lambda a4: lambda e: e.reciprocal(out=a4[:, 2:3], in_=a4[:, 2:3]))(a4), r=[tkn], w=[tkn])
                    stt("dve", onb[u_], osb[u_], a4[:, 2:3], gsub, ALU.mult, ALU.mult, r=[("osb", u_), tkn, "gsub"], w=[("onb", u_)])
                    bt = u_
                    pvb = ps[bt][:].bitcast(BF16)
                    tr(pvb[:, 0:128], onb[u_], ident_b[:], r=[("onb", u_), "ident_b"], w=[PS[bt]])
                    evac_bf("act", attT[:, h, qt * 128:(qt + 1) * 128], pvb[:, 0:128], r=[PS[bt]], w=[("attT", h)])
        for h in range(4):
            dbg_dump("attT%d" % h, attT[:, h, :], [128, T], BF16, [("attT", h)])
        S.barrier()
        if stage <= 3:
            S.emit(); return nc

        ubm = carve(32, [128, 2, 16, 512], BF16)
        ybm = carve(64, [128, 2, 16, 512], BF16)
        w3mask = carve(96, [128, 4, 512], BF16); w3eye = carve(100, [128, 4, 512], BF16)
        for jt in range(2):
            for t4 in range(4):
                dma("sp", ubm[:, jt, 4 * t4:4 * t4 + 4, :], u_d[jt * 2048:(jt + 1) * 2048, :].rearrange("(b t) c -> b t c", t=16)[:, 4 * t4:4 * t4 + 4, :],
                    w=["ubm%d%d" % (jt, t4)])
        UBM = ["ubm%d%d" % (jt, t4) for jt in range(2) for t4 in range(4)]
        dma("sp", w3mask, I["w3mask"], w=["w3mask"])
        dma("sp", w3eye, I["w3eye"], w=["w3eye"])
        xsc = {"dve": (carve(172, [128, 16, 32]), carve(174, [128, 16, 32])), "pool": (carve(176, [128, 16, 32]), carve(178, [128, 16, 32]))}
        g1b = [carve(180 + 2 * i, [128, 512]) for i in range(2)]
        g2b = [carve(184 + 2 * i, [128, 512]) for i in range(2)]

        def cprod(eng, ore, oim, are_, aim_, bre_, bim_, negim=False):
            xa, xb = xsc[eng]
            tk = "xsc_" + eng
            rr = PWT + ["bbp", "ccp"]
            tt(eng, xa, are_, bre_, ALU.mult, r=rr + [tk], w=[tk])
            tt(eng, xb, aim_, bim_, ALU.mult, r=rr + [tk], w=[tk])
            tt(eng, ore[0], xa, xb, ALU.subtract, r=[tk], w=[ore[1]])
            tt(eng, xa, are_, bim_, ALU.mult, r=rr + [tk], w=[tk])
            tt(eng, xb, aim_, bre_, ALU.mult, r=rr + [tk], w=[tk])
            if negim:
                tt(eng, xa, xa, xb, ALU.add, r=[tk], w=[tk])
                ts(eng, oim[0], xa, -1.0, None, ALU.mult, r=[tk], w=[oim[1]])
            else:
                tt(eng, oim[0], xa, xb, ALU.add, r=[tk], w=[oim[1]])

        def ssm_j(j):
            s = j % 2
            base = 104 + 16 * s
            utj = carve(base, [128, 4, 256], BF16)
            w1t = carve(base + 2, [128, 2, 512], BF16)
            bh = carve(base + 4, [128, 2, 512], BF16)
            w2 = carve(base + 6, [128, 2, 512], BF16)
            w1l = carve(base + 8, [128, 2, 4, 128], BF16)
            w3 = carve(base + 10, [128, 4, 512], BF16)
            sbase = 160 + 6 * s
            sre = carve(sbase, [128, 257]); sim_ = carve(sbase + 1.25, [128, 257])
            tre = carve(sbase + 2.5, [128, 256]); tim = carve(sbase + 3.5, [128, 256])
            sbb = carve(sbase + 4.5, [128, 2, 258], BF16)
            tim2 = carve(188 + s, [128, 256])
            K = lambda n: (n, s)
            bt = 2 + s
            pv = ps[bt][:].bitcast(BF16)
            ustg = carve(base + 14, [128, 2, 512], BF16)
            for jt in range(2):
                cp("pool", ustg[:, jt, :].rearrange("p (t c) -> p t c", c=32), ubm[:, jt, :, 32 * j:32 * j + 32], r=UBM, w=[K("ustg")])
            for t4 in range(4):
                for jt in range(2):
                    tr(pv[:, (t4 * 2 + jt) * 128:(t4 * 2 + jt + 1) * 128], ustg[:, jt, t4 * 128:(t4 + 1) * 128], ident_b[:],
                       r=[K("ustg"), "ident_b"], w=[PS[bt]])
            evac_bf("act", utj.rearrange("p a b -> p (a b)"), pv, r=[PS[bt]], w=[K("utj")])
            KS = int(os.environ.get("KS", "9"))
            if KS < 2:
                return
            v16 = lambda ap: ap.rearrange("p (t c) -> p t c", c=32)
            PR = lambda tab, ri, lo: tab[:, ri, j, lo:lo + 16].unsqueeze(2).to_broadcast([128, 16, 32])
            BB = lambda tab, ri: tab[:, ri, j, :].unsqueeze(1).to_broadcast([128, 16, 32])
            cprod("dve", (v16(w1t[:, 0, :]), K("w1t")), (v16(w1t[:, 1, :]), K("w1t")), PR(pwr, 0, 0), PR(pwr, 1, 0), BB(bbp, 0), BB(bbp, 1))
            cprod("pool", (v16(bh[:, 0, :]), K("bh")), (v16(bh[:, 1, :]), K("bh")), PR(pwn, 0, 1), PR(pwn, 1, 1), BB(bbp, 0), BB(bbp, 1))
            cprod("pool", (v16(w2[:, 0, :]), K("w2")), (v16(w2[:, 1, :]), K("w2")), PR(pw, 0, 1), PR(pw, 1, 1), BB(ccp, 0), BB(ccp, 1), negim=True)
            bw = s
            pvw = ps[bw][:].bitcast(BF16)
            for ri in range(2):
                for t4 in range(4):
                    tr(pvw[:, (ri * 4 + t4) * 128:(ri * 4 + t4 + 1) * 128], w1t[:, ri, t4 * 128:(t4 + 1) * 128], ident_b[:], r=[K("w1t"), "ident_b"], w=[PS[bw]])
            evac_bf("act", w1l.rearrange("p a b c -> p (a b c)"), pvw, r=[PS[bw]], w=[K("w1l")])
            for t4 in range(4):
                mm(ps[4][:], bh[:, 0, t4 * 128:(t4 + 1) * 128], w2[:, 0, :], True, False, r=[K("bh"), K("w2")], w=[PS[4]])
                mm(ps[4][:], bh[:, 1, t4 * 128:(t4 + 1) * 128], w2[:, 1, :], False, True, r=[K("bh"), K("w2")], w=[PS[4]])
                tt("dve", w3[:, t4, :], ps[4][:], w3mask[:, t4, :], ALU.mult, r=[PS[4], "w3mask"], w=[K("w3")])
                stt("dve", w3[:, t4, :], w3eye[:, t4, :], dcol[:, j:j + 1], w3[:, t4, :], ALU.mult, ALU.add, r=[K("w3"), "w3eye", "dcol"], w=[K("w3")])
            if KS < 3:
                return
            for ri in range(2):
                bv = 5 - ri
                for t4 in range(4):
                    mm(ps[bv][:, 0:256], w1l[:, ri, t4, :], utj[:, t4, :], t4 == 0, t4 == 3, r=[K("w1l"), K("utj")], w=[PS[bv]])
            S.op("dve", lambda e: e.memset(sre[:, 0:1], 0.0), w=[K("sre")])
            S.op("pool", lambda e: e.memset(sim_[:, 0:1], 0.0), w=[K("sim")])
            cp("dve", sre[:, 1:257], ps[5][:, 0:256], r=[PS[5]], w=[K("sre")])
            cp("act", sim_[:, 1:257], ps[4][:, 0:256], r=[PS[4]], w=[K("sim")])
            for l in range(8):
                d = 1 << l
                n = 256 - d
                ar_, ai_ = a2k[:, 0, j, l:l + 1], a2k[:, 1, j, l:l + 1]
                ts("dve", tre[:, 0:n], sim_[:, 1:1 + n], ai_, None, ALU.mult, r=[K("sim")] + PWT, w=[K("tre")])
                stt("dve", tre[:, 0:n], sre[:, 1:1 + n], ar_, tre[:, 0:n], ALU.mult, ALU.subtract, r=[K("sre"), K("tre")] + PWT, w=[K("tre")])
                ts("pool", tim[:, 0:n], sre[:, 1:1 + n], ai_, None, ALU.mult, r=[K("sre")] + PWT, w=[K("tim")])
                ts("pool", tim2[:, 0:n], sim_[:, 1:1 + n], ar_, None, ALU.mult, r=[K("sim")] + PWT, w=[K("tim2")])
                tt("pool", tim[:, 0:n], tim[:, 0:n], tim2[:, 0:n], ALU.add, r=[K("tim"), K("tim2")], w=[K("tim")])
                tt("dve", sre[:, 1 + d:257], sre[:, 1 + d:257], tre[:, 0:n], ALU.add, r=[K("sre"), K("tre")], w=[K("sre")])
                tt("pool", sim_[:, 1 + d:257], sim_[:, 1 + d:257], tim[:, 0:n], ALU.add, r=[K("sim"), K("tim")], w=[K("sim")])
            cp("dve", sbb[:, 0, 0:257], sre[:, 0:257], r=[K("sre")], w=[K("sbb")])
            cp("pool", sbb[:, 1, 0:257], sim_[:, 0:257], r=[K("sim"), K("sbb")], w=[K("sbb")])
            if KS < 4:
                return
            for jt in range(2):
                by = 6 + jt
                bl = slice(jt * 128, (jt + 1) * 128)
                mm(ps[by][:], sbb[:, 0, bl], w2[:, 0, :], True, False, r=[K("sbb"), K("w2")], w=[PS[by]])
                mm(ps[by][:], sbb[:, 1, bl], w2[:, 1, :], False, False, r=[K("sbb"), K("w2")], w=[PS[by]])
                for t4 in range(4):
                    mm(ps[by][:], utj[:, t4, bl], w3[:, t4, :], False, t4 == 3, r=[K("utj"), K("w3")], w=[PS[by]])
                if "ssm_y" in dbg:
                    cp("dve", ybm[:, jt, :, 32 * j:32 * j + 32], ps[by][:].rearrange("p (t c) -> p t c", c=32), r=[PS[by]], w=["ybm"])
                    continue
                g1, g2 = g1b[jt], g2b[jt]
                act(g1, ps[by][:], AF.Square, r=[PS[by]], w=[("g1", jt)])
                ts("dve", g1, g1, 0.044715, 1.0, ALU.mult, ALU.add, r=[("g1", jt)], w=[("g1", jt)])
                tt("dve", g1, g1, ps[by][:], ALU.mult, r=[("g1", jt), PS[by]], w=[("g1", jt)])
                act(g2, g1, AF.Sigmoid, r=[("g1", jt)], w=[("g2", jt)], scale=2.0 * math.sqrt(2.0 / math.pi))
                tt("dve", ybm[:, jt, :, 32 * j:32 * j + 32], g2.rearrange("p (t c) -> p t c", c=32), ps[by][:].rearrange("p (t c) -> p t c", c=32), ALU.mult,
                   r=[("g2", jt), PS[by]], w=["ybm"])

        for j in range(16):
            ssm_j(j)
        if "ybm" in dbg or "ssm_y" in dbg:
            for jt in range(2):
                for t4 in range(4):
                    dbg_dump("ybm%d_%d" % (jt, t4), ybm[:, jt, 4 * t4:4 * t4 + 4, :], [128, 4, 512], BF16, ["ybm"], True)
        S.barrier()
        if stage <= 4:
            S.emit(); return nc

        yT = carve(32, [128, 4, T], BF16)
        ssmT = carve(100, [128, 4, T], BF16)
        wglu = carve(96, [128, 4, 512], BF16)
        ones_b = carve(132, [128, 128], BF16)
        sgb = [carve(133 + i, [128, 512], BF16) for i in range(2)]
        sqb2 = [carve(135 + i, [128, 512], BF16) for i in range(2)]
        rsb = [carve(137 + 2 * i, [128, 512]) for i in range(2)]
        S.op("pool", lambda e: e.memset(ones_b, 1.0), w=["ones_b"])
        dma("pool", wglu, I["w_glu"].rearrange("(ko p) n -> p ko n", p=128), w=["wglu"])
        gi = 0
        for jt in range(2):
            for ct in range(4):
                for half in range(2):
                    b = gi % 4
                    pv = ps[b][:].bitcast(BF16)
                    for t8 in range(8):
                        tr(pv[:, t8 * 128:(t8 + 1) * 128], ybm[:, jt, half * 8 + t8, ct * 128:(ct + 1) * 128], ident_b[:], r=["ybm", "ident_b"], w=[PS[b]])
                    o = yT[:, ct, jt * 2048:(jt + 1) * 2048].rearrange("p (b t) -> p t b", t=16)[:, half * 8:half * 8 + 8, :]
                    src = pv.rearrange("p (t b) -> p t b", b=128)
                    for t8 in range(8):
                        evac_bf("act" if (gi + t8) % 2 == 0 else "dve", o[:, t8, :], src[:, t8, :], r=[PS[b]], w=[("yT", jt)])
                    gi += 1
        for tb in range(8):
            tbs = slice(tb * 512, (tb + 1) * 512)
            jt = tb // 4
            u_ = tb % 2
            bt_ = 6 + u_
            for ft in range(4):
                b = 4 + ft % 2
                for kt in range(4):
                    mm(ps[b][:], wglu[:, kt, ft * 128:(ft + 1) * 128], yT[:, kt, tbs], kt == 0, kt == 3, r=["wglu", ("yT", jt)], w=[PS[b]])
                sg, sq = sgb[ft % 2], sqb2[ft % 2]
                act(sg, ps[b][:], AF.Sigmoid, r=[PS[b]], w=[("sg", ft % 2)])
                tt("dve", ssmT[:, ft, tbs], yT[:, ft, tbs], sg, ALU.mult, r=[("yT", jt), ("sg", ft % 2)], w=[("ssmT", tb)])
                tt("pool", sq, ssmT[:, ft, tbs], ssmT[:, ft, tbs], ALU.mult, r=[("ssmT", tb)], w=[("sq2", ft % 2)])
                mm(ps[bt_][:], ones_b, sq, ft == 0, ft == 3, r=["ones_b", ("sq2", ft % 2)], w=[PS[bt_]])
            rs = rsb[u_]
            ts("dve", rs, ps[bt_][:], 1.0 / 512, EPS, ALU.mult, ALU.add, r=[PS[bt_]], w=[("rs", u_)])
            act(rs, rs, AF.Sqrt, r=[("rs", u_)], w=[("rs", u_)])
            S.op("dve", (lambda rs: lambda e: e.reciprocal(out=rs, in_=rs))(rs), r=[("rs", u_)], w=[("rs", u_)])
            for ft in range(4):
                stt("dve", ssmT[:, ft, tbs], ssmT[:, ft, tbs], vc1[:, 72 + ft:73 + ft], rs, ALU.mult, ALU.mult,
                    r=[("ssmT", tb), ("rs", u_), "vc1"], w=[("ssmT", tb)])
        for ct in range(4):
            dbg_dump("ssmT%d" % ct, ssmT[:, ct, :], [128, T], BF16, [("ssmT", tb) for tb in range(8)])
        S.barrier()
        if stage <= 5:
            S.emit(); return nc

        wout = carve(32, [128, 8, 1024], BF16)
        bct = {"g1": carve(48, [128, 1024]), "a2": carve(52, [128, 1024]), "b2": carve(56, [128, 1024]), "g2": carve(60, [128, 1024])}
        wr = carve(64, [128, 8, 256], BF16)
        wgu = carve(68, [128, 8, 512], BF16)
        wds = carve(76, [128, 2, 1024], BF16)
        rbias = carve(80, [128, 256])
        gatesT = carve(82, [128, 2, T], BF16)
        h2T_d = nc.dram_tensor("scr_h2T", [128, 8, T], BF16, kind="ExternalOutput").ap()
        diag = [carve(81 + 0.5 * i, [128, 128]) for i in range(2)]
        xt2 = [carve(132 + 4 * i, [128, 1024]) for i in range(2)]
        x1t = [carve(140 + 4 * i, [128, 1024]) for i in range(2)]
        h2tok = [carve(152 + 2 * i, [128, 1024], BF16) for i in range(2)]
        h2T = [carve(156 + 2 * i, [128, 8, 128], BF16) for i in range(2)]
        rsc = [carve(176 + i, [128, 256]) for i in range(8)]
        mbf = carve(168, [128, 256], BF16)
        ash = carve(168.5, [128, 256], BF16); ashT = carve(169, [128, 2, 128], BF16)
        sm = carve(185, [128, 128])
        x1p_d = out
        wo_src = I["w_out"].rearrange("(ko p) n -> p ko n", p=128)
        for k in range(8):
            dma("pool", wout[:, k, :], wo_src[:, k, :], w=[("wout", k)])
        WOUT = [("wout", k) for k in range(8)]
        dma("pool", wr, I["w_router"].rearrange("(ko p) n -> p ko n", p=128), w=["wr"])
        dma("pool", wgu[:, :, 0:256], I["w_gate_s"].rearrange("(ko p) n -> p ko n", p=128), w=["wgu0"])
        dma("pool", wgu[:, :, 256:512], I["w_up_s"].rearrange("(ko p) n -> p ko n", p=128), w=["wgu1"])
        dma("pool", wds, I["w_down_s"].rearrange("(ko p) n -> p ko n", p=128), w=["wds"])
        dma("sp", rbias, I["rbias"], w=["rbias"])
        bi = 0
        for name, col in (("g1", mod[:, 16:24]), ("a2", ab[:, 16:24]), ("b2", ab[:, 24:32]), ("g2", mod[:, 40:48])):
            for hf in range(2):
                for j4 in range(4):
                    jj = hf * 4 + j4
                    dg = diag[bi % 2]
                    ts("dve", dg, ident_f[:], col[:, jj:jj + 1], None, ALU.mult, r=["ident_f", "mod", "ab"], w=[("diag", bi % 2)])
                    mm(ps[hf][:, j4 * 128:(j4 + 1) * 128], ones_f[:], dg, True, True, r=["ones_f", ("diag", bi % 2)], w=[PS[hf]])
                    bi += 1
                cp("act", bct[name][:, hf * 512:(hf + 1) * 512], ps[hf][:], r=[PS[hf]], w=["bc_" + name])

        def phaseF_tile(i):
            s = i % 2
            tok = slice(i * 128, (i + 1) * 128)
            x_t, x1, h2t, h2T_ = xt2[s], x1t[s], h2tok[s], h2T[s]
            K = lambda n: (n, s)
            smc = lambda a, b=None: sm[:, (64 * s + a):(64 * s + (a + 1 if b is None else b))]
            dma("sp", x_t, I["x"][tok, :], w=[K("xt2")])
            for nb in range(2):
                for kt in range(8):
                    lhs = attT[:, kt, tok] if kt < 4 else ssmT[:, kt - 4, tok]
                    mm(ps[nb][:], lhs, wout[:, kt, nb * 512:(nb + 1) * 512], kt == 0, kt == 7, r=WOUT, w=[PS[nb]])
                nbs = slice(nb * 512, (nb + 1) * 512)
                tt("dve", x1[:, nbs], ps[nb][:], bct["g1"][:, nbs], ALU.mult, r=[PS[nb], "bc_g1"], w=[K("x1")])
            tt("pool", x1, x1, x_t, ALU.add, r=[K("x1"), K("xt2")], w=[K("x1")])
            act(h2t, x1, AF.Square, r=[K("x1")], w=[K("h2t"), K("sm")], accum_out=smc(0), scale=1.0 / math.sqrt(D))
            ts("dve", smc(1), smc(0), EPS, None, ALU.add, r=[K("sm")], w=[K("sm")])
            act(smc(1), smc(1), AF.Sqrt, r=[K("sm")], w=[K("sm")])
            S.op("dve", lambda e: e.reciprocal(out=smc(1), in_=smc(1)), r=[K("sm")], w=[K("sm")])
            stt("dve", x_t, x1, smc(1), bct["a2"], ALU.mult, ALU.mult, r=[K("x1"), K("sm"), "bc_a2", K("xt2")], w=[K("xt2")])
            tt("pool", h2t, x_t, bct["b2"], ALU.add, r=[K("xt2"), "bc_b2"], w=[K("h2t")])
            if "h2" in dbg and i < 2:
                dbg_dump("h2_%d" % i, h2t, [128, 1024], BF16, [K("h2t")], True)
                dbg_dump("x1_%d" % i, x1, [128, 1024], F32, [K("x1")], True)
            pv = ps[2][:].bitcast(BF16)
            for c in range(8):
                tr(pv[:, c * 128:(c + 1) * 128], h2t[:, c * 128:(c + 1) * 128], ident_b[:], r=[K("h2t"), "ident_b"], w=[PS[2]])
            evac_bf("act", h2T_.rearrange("p a b -> p (a b)"), pv, r=[PS[2]], w=[K("h2T")])
            sc, sbv, msb, Mf, posf, tmp, sgs = rsc[0], rsc[1], rsc[2], rsc[3], rsc[4], rsc[5], rsc[6]
            for c in range(8):
                mm(ps[3][:, 0:256], h2T_[:, c, :], wr[:, c, :], c == 0, c == 7, r=[K("h2T"), "wr"], w=[PS[3]])
            act(sc, ps[3][:, 0:256], AF.Sigmoid, r=[PS[3]], w=["sc"])
            tt("dve", sbv, sc, rbias, ALU.add, r=["sc", "rbias"], w=["sbv"])
            mx8 = carve(184, [128, 8, 8])
            for g in range(8):
                S.op("dve", (lambda g: lambda e: e.max(out=mx8[:, g, :], in_=sbv[:, g * 32:(g + 1) * 32]))(g), r=["sbv"], w=["mx8"])
            gs, g8, gm, pen = smc(8, 16), smc(16, 24), smc(24, 32), smc(32, 40)
            tt("dve", gs, mx8[:, :, 0], mx8[:, :, 1], ALU.add, r=["mx8"], w=[K("sm")])
            S.op("dve", lambda e: e.max(out=g8, in_=gs), r=[K("sm")], w=[K("sm")])
            ts("dve", gm, gs, g8[:, 3:4], None, ALU.is_ge, r=[K("sm")], w=[K("sm")])
            ts("dve", pen, gm, -1.0, 1e30, ALU.add, ALU.mult, r=[K("sm")], w=[K("sm")])
            v8 = lambda t: t.rearrange("p (g e) -> p g e", e=32)
            tt("dve", v8(msb), v8(sbv), gm.unsqueeze(2).to_broadcast([128, 8, 32]), ALU.mult, r=["sbv", K("sm")], w=["msb"])
            tt("dve", v8(msb), v8(msb), pen.unsqueeze(2).to_broadcast([128, 8, 32]), ALU.add, r=["msb", K("sm")], w=["msb"])
            top8 = smc(40, 48)
            S.op("dve", lambda e: e.max(out=top8, in_=msb), r=["msb"], w=[K("sm")])
            ts("dve", Mf, msb, top8[:, 7:8], None, ALU.is_ge, r=["msb", K("sm")], w=["Mf"])
            wsum = smc(2)
            S.op("dve", lambda e: e.scalar_tensor_tensor(out=tmp, in0=Mf, scalar=1.0, in1=sc, op0=ALU.mult, op1=ALU.mult, accum_out=wsum),
                 r=["Mf", "sc", "tmp"], w=["tmp", K("sm")])
            S.op("dve", lambda e: e.reciprocal(out=wsum, in_=wsum), r=[K("sm")], w=[K("sm")])
            ts("dve", wsum, wsum, 2.5, None, ALU.mult, r=[K("sm")], w=[K("sm")])
            ts("dve", mbf, tmp, wsum, None, ALU.mult, r=["tmp", K("sm")], w=["mbf"])
            if "route" in dbg:
                dbg_dump("gd%d" % i, mbf, [128, 256], BF16, ["mbf"], True)
            pv4 = ps[4][:].bitcast(BF16)
            for ec in range(2):
                tr(pv4[:, ec * 128:(ec + 1) * 128], mbf[:, ec * 128:(ec + 1) * 128], ident_b[:], r=["mbf", "ident_b"], w=[PS[4]])
            for ec in range(2):
                evac_bf("dve", gatesT[:, ec, tok], pv4[:, ec * 128:(ec + 1) * 128], r=[PS[4]], w=["gatesT"])
            dma("sp", h2T_d[:, :, tok], h2T_, r=[K("h2T")], w=[("h2T_d", i)])
            for c in range(8):
                mm(ps[5][:], h2T_[:, c, :], wgu[:, c, :], c == 0, c == 7, r=[K("h2T"), "wgu0", "wgu1"], w=[PS[5]])
            act(sgs, ps[5][:, 0:256], AF.Sigmoid, r=[PS[5]], w=["sgs"])
            tt("dve", sgs, sgs, ps[5][:, 0:256], ALU.mult, r=["sgs", PS[5]], w=["sgs"])
            tt("dve", ash, sgs, ps[5][:, 256:512], ALU.mult, r=["sgs", PS[5]], w=["ash"])
            pv3 = ps[3][:].bitcast(BF16)
            for fk in range(2):
                tr(pv3[:, 512 + fk * 128:512 + (fk + 1) * 128], ash[:, fk * 128:(fk + 1) * 128], ident_b[:], r=["ash", "ident_b"], w=[PS[3]])
            evac_bf("dve", ashT.rearrange("p a b -> p (a b)"), pv3[:, 512:768], r=[PS[3]], w=["ashT"])
            for nb in range(2):
                for fk in range(2):
                    mm(ps[6 + nb][:], ashT[:, fk, :], wds[:, fk, nb * 512:(nb + 1) * 512], fk == 0, fk == 1, r=["ashT", "wds"], w=[PS[6 + nb]])
                nbs = slice(nb * 512, (nb + 1) * 512)
                tt("dve", x_t[:, nbs], ps[6 + nb][:], bct["g2"][:, nbs], ALU.mult, r=[PS[6 + nb], "bc_g2", K("xt2")], w=[K("xt2")])
            tt("pool", x1, x1, x_t, ALU.add, r=[K("x1"), K("xt2")], w=[K("x1")])
            dma("sp", x1p_d[tok, :], x1, r=[K("x1")], w=[("x1p_d", i)])

        for i in range(NT):
            phaseF_tile(i)
        S.barrier()
        if stage <= 6:
            S.emit(); return nc

        NEXP = int(os.environ.get("KNE", str(NE)))
        h2Th = carve(0, [128, 8, 2048], BF16)
        wring = [(carve(32 + 12 * r, [128, 8, 256], BF16), carve(36 + 12 * r, [128, 8, 256], BF16), carve(40 + 12 * r, [128, 2, 1024], BF16)) for r in range(3)]
        selb = [carve(68 + 0.25 * i, [128, 128], BF16) for i in range(2)]
        ab_ = [carve(69 + 2 * i, [128, 2, 512], BF16) for i in range(2)]
        accT = carve(100, [128, 8, 2048])
        sgb_ = [carve(164 + 4 * i, [128, 2, 512]) for i in range(2)]
        t2b_ = [carve(172 + 4 * i, [128, 2, 512]) for i in range(2)]
        gsb_ = [carve(180 + i, [128, 512], BF16) for i in range(2)]
        xo = [carve(182 + 4 * i, [128, 1024]) for i in range(2)]
        wge = I["w_gate_e"]; wue = I["w_up_e"]; wde = I["w_down_e"]
        gstep = 0
        for hh in range(2):
            for c in range(8):
                dma("sp", h2Th[:, c, :], h2T_d[:, c, hh * 2048:(hh + 1) * 2048], w=[("h2Th", c)])
            H2 = [("h2Th", c) for c in range(8)]
            for e in range(NEXP):
                r = (hh * NEXP + e) % 3
                wg, wu, wd = wring[r]
                dma("pool", wg, wge[e].rearrange("(ko p) n -> p ko n", p=128), w=[("wg", r)])
                dma("pool", wu, wue[e].rearrange("(ko p) n -> p ko n", p=128), w=[("wu", r)])
                dma("pool", wd, wde[e].rearrange("(ko p) n -> p ko n", p=128), w=[("wd", r)])
                ec, ej = e // 128, e % 128
                sl = selb[e % 2]
                evac_bf("dve", sl, ident_b[:, ej:ej + 1].to_broadcast([128, 128]), r=["ident_b"], w=[("sel", e % 2)])
                for tb in range(4):
                    u_ = gstep % 2
                    gstep += 1
                    tbs = slice(tb * 512, (tb + 1) * 512)
                    gts = slice(hh * 2048 + tb * 512, hh * 2048 + (tb + 1) * 512)
                    sg, a_, gs, t2 = sgb_[u_], ab_[u_], gsb_[u_], t2b_[u_]
                    for fk in range(2):
                        for c in range(8):
                            mm(ps[fk][:], wg[:, c, fk * 128:(fk + 1) * 128], h2Th[:, c, tbs], c == 0, c == 7, r=[("wg", r)] + H2, w=[PS[fk]])
                    for fk in range(2):
                        for c in range(8):
                            mm(ps[2 + fk][:], wu[:, c, fk * 128:(fk + 1) * 128], h2Th[:, c, tbs], c == 0, c == 7, r=[("wu", r)] + H2, w=[PS[2 + fk]])
                    mm(ps[4][:], sl, gatesT[:, ec, gts], True, True, r=[("sel", e % 2), "gatesT"], w=[PS[4]])
                    for fk in range(2):
                        act(sg[:, fk, :], ps[fk][:], AF.Silu, r=[PS[fk]], w=[("sg", u_)])
                    for fk in range(2):
                        tt("dve", t2[:, fk, :], sg[:, fk, :], ps[2 + fk][:], ALU.mult, r=[("sg", u_), PS[2 + fk]], w=[("t2", u_)])
                    act(gs, ps[4][:], AF.Copy, r=[PS[4]], w=[("gs", u_)])
                    for fk in range(2):
                        tt("pool", a_[:, fk, :], t2[:, fk, :], gs, ALU.mult, r=[("t2", u_), ("gs", u_)], w=[("a", u_)])
                    for dc in range(8):
                        bD = 5 + dc % 3
                        for fk in range(2):
                            mm(ps[bD][:], wd[:, fk, dc * 128:(dc + 1) * 128], a_[:, fk, :], fk == 0, fk == 1, r=[("wd", r), ("a", u_)], w=[PS[bD]])
                        if e == 0:
                            if dc % 2 == 0:
                                cp("dve", accT[:, dc, tbs], ps[bD][:], r=[PS[bD]], w=[("acc", dc, tb)])
                            else:
                                act(accT[:, dc, tbs], ps[bD][:], AF.Copy, r=[PS[bD]], w=[("acc", dc, tb)])
                        else:
                            tt("dve", accT[:, dc, tbs], accT[:, dc, tbs], ps[bD][:], ALU.add, r=[PS[bD], ("acc", dc, tb)], w=[("acc", dc, tb)])
            for dc in range(8):
                act(accT[:, dc, :], accT[:, dc, :], AF.Copy, r=[("acc", dc, tb) for tb in range(4)], w=[("acc", dc, tb) for tb in range(4)], scale=mod[:, 40 + dc:41 + dc])
            for tl in range(16):
                i = hh * 16 + tl
                tok = slice(i * 128, (i + 1) * 128)
                xo_ = xo[i % 2]
                dma("sp", xo_, x1p_d[tok, :], w=[("xo", i % 2)])
                for dc in range(8):
                    nb = dc // 4
                    tr(ps[nb][:, (dc % 4) * 128:(dc % 4 + 1) * 128], accT[:, dc, tl * 128:(tl + 1) * 128], ident_f[:],
                       r=[("acc", dc, tl // 4), "ident_f"], w=[PS[nb]])
                for nb in range(2):
                    nbs = slice(nb * 512, (nb + 1) * 512)
                    tt("dve", xo_[:, nbs], xo_[:, nbs], ps[nb][:], ALU.add, r=[("xo", i % 2), PS[nb]], w=[("xo", i % 2)])
                dma("sp", out[tok, :], xo_, r=[("xo", i % 2)], w=[("out", i)])
        S.barrier()
        S.emit()
    return nc


def host_consts():
    ident = np.eye(128, dtype=np.float32)
    inv_freq = (1.0 / (10000.0 ** (np.arange(0, 64, 2, dtype=np.float32) / 64.0))).astype(np.float32)
    ang = np.arange(T, dtype=np.float32)[:, None] * inv_freq[None, :]
    cs = np.concatenate([np.cos(ang), np.sin(ang)], axis=1).astype(np.float32)
    ropecs = np.ascontiguousarray(cs.reshape(NT, 128, 64).transpose(1, 0, 2))
    tri = np.triu(np.ones((128, 128), np.float32)).astype(ml_dtypes.bfloat16)
    tl = np.arange(4)[:, None, None, None, None, None]; t4 = np.arange(4)[None, None, None, :, None, None]
    hc_r = np.arange(32)[None, :, None, None, None, None]
    tau = np.arange(16)[None, None, None, None, :, None]; hc_c = np.arange(32)[None, None, None, None, None, :]
    causal = np.broadcast_to((tau >= 4 * t4 + tl), (4, 32, 1, 4, 16, 32)).reshape(128, 4, 512)
    eye = np.broadcast_to((tau == 4 * t4 + tl) & (hc_r == hc_c), (4, 32, 1, 4, 16, 32)).reshape(128, 4, 512)
    iota_e = np.ascontiguousarray(np.broadcast_to(np.arange(256, dtype=np.float32)[None, :], (128, 256)))
    triu = np.triu(np.ones((128, 128), np.float32), 1).astype(ml_dtypes.bfloat16)
    return dict(ident_f=ident, ident_b=ident.astype(ml_dtypes.bfloat16), ones_f=np.ones((128, 128), np.float32), ropecs=ropecs, tri=tri,

                w3mask=np.ascontiguousarray(causal.astype(np.float32)).astype(ml_dtypes.bfloat16),
                w3eye=np.ascontiguousarray(eye.astype(np.float32)).astype(ml_dtypes.bfloat16))


def make_in_maps(inp, cores):
    cst = host_consts()
    maps = []
    f = lambda a: np.ascontiguousarray(np.asarray(a, dtype=np.float32))
    gqk = np.concatenate([f(inp["q_norm_g"])[0], f(inp["k_norm_g"])[0]])
    gqk = np.ascontiguousarray(np.broadcast_to(gqk[None, :], (128, 128)))
    lamv = np.concatenate([f(inp[k])[0] for k in ("lambda_q1", "lambda_k1", "lambda_q2", "lambda_k2")])
    lamv = np.ascontiguousarray(np.broadcast_to(lamv[None, :], (128, 256)))
    gsub = np.ascontiguousarray(np.broadcast_to(f(inp["subln_g"])[0][None, :], (128, 128)))
    sp = lambda a: np.ascontiguousarray(a.reshape(16, 128).T)
    ssm_cols = np.ascontiguousarray(np.stack([sp(f(inp["ssm_a_re"])[0]), sp(f(inp["ssm_a_im"])[0]),
                                              sp(np.repeat(f(inp["ssm_log_dt"])[0][:, None], 64, 1))], 1))
    bl = lambda a: a.reshape(16, 128, 16).transpose(1, 0, 2)
    ssm_b = np.ascontiguousarray(np.stack([bl(f(inp["ssm_b_re"])[0]), bl(f(inp["ssm_b_im"])[0])], 1))
    cl = lambda a: a.reshape(16, 2, 16, 64).transpose(1, 3, 0, 2).reshape(128, 16, 16)
    ssm_c = np.ascontiguousarray(np.stack([cl(f(inp["ssm_c_re"])[0]), cl(f(inp["ssm_c_im"])[0])], 1))
    ssm_dcol = np.ascontiguousarray(np.tile(f(inp["ssm_d"])[0].reshape(16, 32), (1, 4)).T)
    rbias = np.ascontiguousarray(np.broadcast_to(f(inp["router_bias"])[0][None, :], (128, 256)))
    for b in cores:
        vs1 = np.zeros((128, 128), np.float32)
        vs1[0:8] = f(inp["c"])[b].reshape(8, 128)
        vs1[8:16] = f(inp["norm1_g"])[0].reshape(8, 128)
        vs1[16:24] = f(inp["norm2_g"])[0].reshape(8, 128)
        vs1[24:72] = f(inp["b_ada"])[0].reshape(48, 128)
        vs1[72:76] = f(inp["ssm_norm_g"])[0].reshape(4, 128)
        m = dict(x=f(inp["x"])[b], vs1=vs1, w_ada=f(inp["w_ada"])[0], w_in=f(inp["w_in"])[0], w_glu=f(inp["w_glu"])[0], gqk=gqk, w_out=f(inp["w_out"])[0], w_router=f(inp["w_router"])[0],
                 w_gate_s=f(inp["w_gate_s"])[0], w_up_s=f(inp["w_up_s"])[0], w_down_s=f(inp["w_down_s"])[0], rbias=rbias, lamv=lamv, gsub=gsub, ssm_cols=ssm_cols, ssm_b=ssm_b, ssm_c=ssm_c, ssm_dcol=ssm_dcol)
        if "w_gate_e" in inp:
            m.update(w_gate_e=f(inp["w_gate_e"])[0], w_up_e=f(inp["w_up_e"])[0], w_down_e=f(inp["w_down_e"])[0])
        m.update(cst)
        maps.append(m)
    return maps


def kernel(**inputs):
    nc = build()
    maps = make_in_maps(inputs, list(range(8)))
    res = run_bass_kernel_spmd(nc, maps, core_ids=list(range(8)))
    return np.stack([r["out"] for r in res.results], axis=0)
```

```python
import contextlib
import math
import numpy as np
import ml_dtypes
import concourse.bass as bass
import concourse.mybir as mybir
from concourse.bass_utils import run_bass_kernel_spmd

F32 = mybir.dt.float32
BF16 = mybir.dt.bfloat16
I32 = mybir.dt.int32
U32 = mybir.dt.uint32
ALU = mybir.AluOpType
AF = mybir.ActivationFunctionType
AX = mybir.AxisListType

import os
NOPOOL = os.environ.get("NOPOOL", "1") == "1"
ENG = ("pe", "act", "dve", "pool", "sp")
NDMA = 48
NDMA_HW = 32
EPS = 1e-6
T = 4096
D = 1024
NT = T // 128
CAP = 256
NE = 256


class Sched:
    def __init__(self, nc):
        self.nc = nc
        self.ops = {e: [] for e in ENG}
        self.last_w = {}
        self.readers = {}
        self.known = {e: {} for e in ENG}
        self.known_dma = {e: set() for e in ENG}
        self.dmas = []
        self.sem_last = [None] * NDMA
        self.sem_cnt = [0] * NDMA
        self.next_sem = 0
        self.next_sw = 0
        self.live_dma = set()

    def _deps(self, eng, reads, writes):
        deps = []
        for t in reads:
            r = self.last_w.get(t)
            if r is not None:
                deps.append(r)
        for t in writes:
            r = self.last_w.get(t)
            if r is not None:
                deps.append(r)
            deps.extend(self.readers.get(t, ()))
        waits = []
        for d in deps:
            if d[0] == "e":
                _, e2, idx = d
                if e2 == eng and eng in ("pe", "sp"):
                    continue
                if self.known[eng].get(e2, -1) >= idx:
                    continue
                self.known[eng][e2] = idx
                self.ops[e2][idx]["need"] = True
                waits.append(d)
            else:
                did = d[1]
                if did in self.known_dma[eng]:
                    continue
                self.known_dma[eng].add(did)
                waits.append(d)
        return waits

    def _commit(self, ref, reads, writes):
        for t in reads:
            self.readers.setdefault(t, []).append(ref)
        for t in writes:
            self.last_w[t] = ref
            self.readers[t] = []

    def op(self, eng, fn, r=(), w=()):
        if eng == "pool" and NOPOOL:
            eng = "dve"
        waits = self._deps(eng, r, w)
        idx = len(self.ops[eng])
        self.ops[eng].append(dict(fn=fn, waits=waits, need=False, dma=None))
        self._commit(("e", eng, idx), r, w)

    def dma(self, eng, fn, r=(), w=()):
        waits = self._deps(eng, r, w)
        if eng == "pool":
            s = NDMA_HW + self.next_sw
            self.next_sw = (self.next_sw + 1) % (NDMA - NDMA_HW)
        else:
            s = self.next_sem
            self.next_sem = (s + 1) % NDMA_HW
        prev = self.sem_last[s]
        if prev is not None and prev not in self.known_dma[eng]:
            self.known_dma[eng].add(prev)
            waits.append(("d", prev))
        self.sem_cnt[s] += 16
        did = len(self.dmas)
        self.dmas.append(dict(sem=s, val=self.sem_cnt[s]))
        self.sem_last[s] = did
        self.ops[eng].append(dict(fn=fn, waits=waits, need=False, dma=did))
        self._commit(("d", did), r, w)
        self.live_dma.add(did)

    def barrier(self):
        waits = []
        for e in ENG:
            if e != "sp" and self.ops[e]:
                idx = len(self.ops[e]) - 1
                while idx >= 0 and self.ops[e][idx]["dma"] is not None:
                    idx -= 1
                if idx < 0 or self.known["sp"].get(e, -1) >= idx:
                    continue
                self.known["sp"][e] = idx
                self.ops[e][idx]["need"] = True
                waits.append(("e", e, idx))
        for did in sorted(self.live_dma):
            if did not in self.known_dma["sp"]:
                self.known_dma["sp"].add(did)
                waits.append(("d", did))
        self.live_dma = set()
        idx = len(self.ops["sp"])
        self.ops["sp"].append(dict(fn=None, waits=waits, need=True, dma=None))
        ref = ("e", "sp", idx)
        for e in ENG:
            if e == "sp":
                continue
            self.known[e]["sp"] = idx
            self.ops[e].append(dict(fn=None, waits=[ref], need=False, dma=None))
            for e2 in ENG:
                if e2 != e and self.ops[e2]:
                    self.known[e][e2] = max(self.known[e].get(e2, -1), len(self.ops[e2]) - 1)
            self.known_dma[e] = set(range(len(self.dmas)))
        self.known_dma["sp"] = set(range(len(self.dmas)))
        self.last_w = {}
        self.readers = {}

    def emit(self):
        nc = self.nc
        with contextlib.ExitStack() as st:
            esem = {e: st.enter_context(nc.semaphore("s_" + e)) for e in ENG}
            dsem = [st.enter_context(nc.semaphore("d%d" % i)) for i in range(NDMA)]
            val = {}
            for e in ENG:
                c = 0
                for i, o in enumerate(self.ops[e]):
                    if o["need"]:
                        c += 1
                        val[(e, i)] = c
            block = st.enter_context(nc.Block())

            def run(e, eng):
                for i, o in enumerate(self.ops[e]):
                    for wt in o["waits"]:
                        if wt[0] == "e":
                            eng.wait_ge(esem[wt[1]], val[(wt[1], wt[2])])
                        else:
                            d = self.dmas[wt[1]]
                            eng.wait_ge(dsem[d["sem"]], d["val"])
                    if o["fn"] is None:
                        if o["need"]:
                            eng.sem_inc(esem[e], 1)
                        continue
                    ins = o["fn"](eng)
                    if o["dma"] is not None:
                        ins.then_inc(dsem[self.dmas[o["dma"]]["sem"]], 16)
                    elif o["need"]:
                        ins.then_inc(esem[e], 1)

            @block.tensor
            def _(eng):
                run("pe", eng)

            @block.scalar
            def _(eng):
                run("act", eng)

            @block.vector
            def _(eng):
                run("dve", eng)

            @block.gpsimd
            def _(eng):
                run("pool", eng)

            @block.sync
            def _(eng):
                run("sp", eng)


IN_SPECS = [
    ("x", [T, D], F32), ("vs1", [128, 128], F32),
    ("w_ada", [D, 6 * D], F32), ("w_in", [D, 2048], F32), ("w_glu", [512, 512], F32), ("w_out", [D, D], F32),
    ("w_router", [D, 256], F32), ("w_gate_s", [D, 256], F32), ("w_up_s", [D, 256], F32), ("w_down_s", [256, D], F32),
    ("rbias", [128, 256], F32),
    ("w_gate_e", [NE, D, 256], F32), ("w_up_e", [NE, D, 256], F32), ("w_down_e", [NE, 256, D], F32),
    ("ident_f", [128, 128], F32), ("ident_b", [128, 128], BF16),
    ("ones_f", [128, 128], F32),
    ("ropecs", [128, NT, 64], F32), ("gqk", [128, 128], F32),
    ("lamv", [128, 256], F32), ("gsub", [128, 128], F32), ("tri", [128, 128], BF16),
    ("w3mask", [128, 4, 512], BF16), ("w3eye", [128, 4, 512], BF16),
    ("ssm_cols", [128, 3, 16], F32), ("ssm_b", [128, 2, 16, 16], F32), ("ssm_c", [128, 2, 16, 16], F32), ("ssm_dcol", [128, 16], F32),
]


def build(stage=99, dbg=()):
    nc = bass.Bass("TRN2", target_bir_lowering=False)
    S = Sched(nc)
    I = {}
    for name, shp, dt in IN_SPECS:
        if stage < 7 and name in ("w_gate_e", "w_up_e", "w_down_e"):
            continue
        I[name] = nc.dram_tensor(name, shp, dt, kind="ExternalInput").ap()
    out = nc.dram_tensor("out", [T, D], F32, kind="ExternalOutput").ap()

    def dma(eng, o, i, r=(), w=()):
        S.dma(eng, lambda e: e.dma_start(out=o, in_=i), r, w)

    def mm(o, l, rh, st_, sp_, r=(), w=()):
        S.op("pe", lambda e: e.matmul(o, l, rh, start=st_, stop=sp_), r, w)

    def tr(o, i, idn, r=(), w=()):
        S.op("pe", lambda e: e.transpose(o, i, idn), r, w)

    def act(o, i, func, r=(), w=(), **kw):
        S.op("act", lambda e: e.activation(out=o, in_=i, func=func, **kw), r, w)

    def ts(eng, o, i, s1, s2, op0, op1=None, r=(), w=(), **kw):
        if op1 is None:
            S.op(eng, lambda e: e.tensor_scalar(out=o, in0=i, scalar1=s1, scalar2=None, op0=op0, **kw), r, w)
        else:
            S.op(eng, lambda e: e.tensor_scalar(out=o, in0=i, scalar1=s1, scalar2=s2, op0=op0, op1=op1, **kw), r, w)

    def tt(eng, o, a, b, op, r=(), w=()):
        S.op(eng, lambda e: e.tensor_tensor(out=o, in0=a, in1=b, op=op), r, w)

    def stt(eng, o, a, s, b, op0, op1, r=(), w=()):
        S.op(eng, lambda e: e.scalar_tensor_tensor(out=o, in0=a, scalar=s, in1=b, op0=op0, op1=op1), r, w)

    def cp(eng, o, i, r=(), w=()):
        if eng == "act":
            S.op(eng, lambda e: e.activation(out=o, in_=i, func=AF.Copy), r, w)
        else:
            S.op(eng, lambda e: e.tensor_copy(out=o, in_=i), r, w)

    import os
    KDIS = os.environ.get("KDIS", "").split(",")

    def dbg_dump(name, src, shp, dt, r, force=False):
        if name in dbg or force:
            d = nc.dram_tensor("dbg_" + name, shp, dt, kind="ExternalOutput").ap()
            dma("sp", d, src, r=r, w=["dbg_" + name])

    with contextlib.ExitStack() as st:
        def sb(name, shp, dt=F32):
            return st.enter_context(nc.sbuf_tensor("sb_" + name, shp, dt))

        ps = [st.enter_context(nc.psum_tensor("ps%d" % b, [128, 512], F32)) for b in range(8)]
        PS = [("ps", b) for b in range(8)]
        ident_f = sb("ident_f", [128, 128]); ident_b = sb("ident_b", [128, 128], BF16)
        ones_f = sb("ones_f", [128, 128]); vc1 = sb("vc1", [128, 128])
        ARENA_KIB = 192
        arena = sb("arena", [128, ARENA_KIB * 256])

        def carve(off_kib, shp, dt=F32):
            n = 1
            for d_ in shp[1:]:
                n *= d_
            nbytes = n * (2 if dt == BF16 else 4)
            o4 = int(round(off_kib * 256))
            assert abs(o4 - off_kib * 256) < 1e-9 and nbytes % 4 == 0
            assert o4 * 4 + nbytes <= ARENA_KIB * 1024, (off_kib, shp)
            v = arena[:, o4:o4 + nbytes // 4]
            if dt != F32:
                v = v.bitcast(dt)
            if len(shp) == 2:
                return v
            names = " ".join("d%d" % i for i in range(1, len(shp)))
            kw = {"d%d" % i: shp[i] for i in range(2, len(shp))}
            return v.rearrange("p (%s) -> p %s" % (names, names), **kw)

        dma("sp", ident_f[:], I["ident_f"], w=["ident_f"])
        dma("sp", ident_b[:], I["ident_b"], w=["ident_b"])
        dma("sp", ones_f[:], I["ones_f"], w=["ones_f"])
        oz = sb("oz", [128, 2])
        S.op("dve", lambda e: e.memset(oz[:, 0:1], 1.0), w=["zc"])
        S.op("dve", lambda e: e.memset(oz[:, 1:2], 0.0), r=["zc"], w=["zc"])
        onec = oz[:, 0:1]
        zc = oz[:, 1:2]

        def evac_bf(eng, o, i, r=(), w=()):
            rr = list(r) + ["ones_f", "zc"]
            if True:
                S.op("act", lambda e: e.activation(out=o, in_=i, func=AF.Identity, scale=onec, bias=zc), rr, w)
            else:
                S.op("dve", lambda e: e.tensor_scalar(out=o, in0=i, scalar1=onec, scalar2=zc, op0=ALU.mult, op1=ALU.add), rr, w)

        vs1 = sb("vs1", [128, 128])
        dma("sp", vs1[:], I["vs1"], w=["vs1"])
        act(vs1[0:8, :], vs1[0:8, :], AF.Silu, r=["vs1"], w=["vs1"])
        tr(ps[0][:, 0:128], vs1[:], ident_f[:], r=["vs1", "ident_f"], w=[PS[0]])
        cp("dve", vc1[:], ps[0][:, 0:128], r=[PS[0]], w=["vc1"])
        mod = sb("mod", [128, 48])
        wada = [carve(24 * i, [128, 8, 768]) for i in range(2)]
        wada_src = I["w_ada"].rearrange("(ko p) n -> p ko n", p=128)
        for blk in range(8):
            wt = wada[blk % 2]
            tk = ("wada", blk % 2)
            dma("sp", wt, wada_src[:, :, blk * 768:(blk + 1) * 768], w=[tk])
            for jj in range(6):
                j = blk * 6 + jj
                for k in range(8):
                    mm(ps[1][:, j:j + 1], wt[:, k, jj * 128:(jj + 1) * 128], vc1[:, k:k + 1], k == 0, k == 7, r=[tk, "vc1"], w=[PS[1]])
        tt("dve", mod[:], ps[1][:, 0:48], vc1[:, 24:72], ALU.add, r=[PS[1], "vc1"], w=["mod"])
        ab = sb("ab", [128, 32])
        stt("dve", ab[:, 0:8], mod[:, 8:16], 1.0, vc1[:, 8:16], ALU.add, ALU.mult, r=["mod", "vc1"], w=["ab"])
        cp("dve", ab[:, 8:16], mod[:, 0:8], r=["mod", "ab"], w=["ab"])
        stt("dve", ab[:, 16:24], mod[:, 32:40], 1.0, vc1[:, 16:24], ALU.add, ALU.mult, r=["mod", "vc1", "ab"], w=["ab"])
        cp("dve", ab[:, 24:32], mod[:, 24:32], r=["mod", "ab"], w=["ab"])
        dbg_dump("mod", mod[:], [128, 48], F32, ["mod"])
        S.barrier()
        if stage <= 0:
            S.emit(); return nc

        win = carve(0, [128, 8, 2048], BF16)
        qT = carve(32, [128, 4, T], BF16); kT = carve(64, [128, 4, T], BF16)
        vext = carve(96, [128, NT, 4 * 130], BF16)
        hT = carve(130, [128, 8, 2048], BF16)
        qkr = [carve(162 + 2 * i, [128, 1024], BF16) for i in range(2)]
        xn = [carve(166 + 2 * i, [128, D], BF16) for i in range(2)]
        xt = [carve(170 + 4 * i, [128, D]) for i in range(2)]
        ust = [carve(178 + i, [128, 512], BF16) for i in range(2)]
        sqb = carve(180, [128, 1024]); t1 = carve(184, [128, 1024])
        m1 = sqb[:, 0:512]; m2 = sqb[:, 512:1024]; m3 = carve(188, [128, 512]); m4 = carve(190, [128, 512])
        gqk = sb("gqk", [128, 128])
        stat = sb("stat", [128, 2 * NT]); st16 = sb("st16", [128, NT, 16])
        rcs = [sb("rcs%d" % i, [128, 64]) for i in range(2)]
        u_d = nc.dram_tensor("scr_u", [T, 512], BF16, kind="ExternalOutput").ap()
        win_src = I["w_in"].rearrange("(ko p) n -> p ko n", p=128)
        for k in range(8):
            for hf in range(2):
                if "wincast" not in KDIS:
                    dma("pool", win[:, k, hf * 1024:(hf + 1) * 1024], win_src[:, k, hf * 1024:(hf + 1) * 1024], w=[("win", k, hf)])
        WIN = [("win", k, hf) for k in range(8) for hf in range(2)]
        dma("sp", gqk[:], I["gqk"], w=["gqk"])
        if "memset" not in KDIS:
            S.op("pool", lambda e: e.memset(vext.rearrange("p n (h e) -> p n h e", e=130)[:, :, :, 128:129], 1.0), w=["vext1"])

        def phaseB_tile(i):
            s = i % 2
            x_t, xn_t = xt[s], xn[s]
            tx, tn = ("xt", s), ("xn", s)
            pb = 2 + (i % 2)
            il = i % 16
            ss, rs = stat[:, 2 * i:2 * i + 1], stat[:, 2 * i + 1:2 * i + 2]
            dma("sp", x_t, I["x"][i * 128:(i + 1) * 128, :], w=[tx])
            act(xn_t, x_t, AF.Square, r=[tx], w=[tn, ("stat", i)], accum_out=ss, scale=1.0 / math.sqrt(D))
            ts("dve", rs, ss, EPS, None, ALU.add, r=[("stat", i)], w=[("stat", i)])
            act(rs, rs, AF.Sqrt, r=[("stat", i)], w=[("stat", i)])
            S.op("dve", lambda e: e.reciprocal(out=rs, in_=rs), r=[("stat", i)], w=[("stat", i)])
            act(xn_t, x_t, AF.Copy, r=[tx, ("stat", i)], w=[tn], scale=rs)
            pv = ps[pb][:].bitcast(BF16)
            for c in range(8):
                tr(pv[:, c * 128:(c + 1) * 128], xn_t[:, c * 128:(c + 1) * 128], ident_b[:], r=[tn, "ident_b"], w=[PS[pb]])
            for c in range(8):
                o = hT[:, c, il * 128:(il + 1) * 128]
                src = pv[:, c * 128:(c + 1) * 128]
                if c % 2 == 0:
                    ts("dve", o, src, ab[:, c:c + 1], ab[:, 8 + c:9 + c], ALU.mult, ALU.add, r=[PS[pb], "ab"], w=[("hT", il)])
                else:
                    act(o, src, AF.Identity, r=[PS[pb], "ab"], w=[("hT", il)], scale=ab[:, c:c + 1], bias=ab[:, 8 + c:9 + c])

        def phaseC_tile(i):
            s = i % 2
            il = i % 16
            bq, bk = (0, 1) if s == 0 else (4, 5)
            bt = 2 + s
            tok = slice(i * 128, (i + 1) * 128)
            tl = slice(il * 128, (il + 1) * 128)
            hdep = [("hT", il)]
            dma("sp", rcs[s][:], I["ropecs"][:, i, :], w=[("rcs", s)])
            for c in range(8):
                mm(ps[bq][:], hT[:, c, tl], win[:, c, 0:512], c == 0, c == 7, r=hdep + WIN, w=[PS[bq]])
            for c in range(8):
                mm(ps[bk][:], hT[:, c, tl], win[:, c, 512:1024], c == 0, c == 7, r=hdep + WIN, w=[PS[bk]])
            for c in range(8):
                mm(ps[6][:], hT[:, c, tl], win[:, c, 1024:1536], c == 0, c == 7, r=hdep + WIN, w=[PS[6]])
            for c in range(8):
                mm(ps[7][:], hT[:, c, tl], win[:, c, 1536:2048], c == 0, c == 7, r=hdep + WIN, w=[PS[7]])
            act(vext[:, i, :].rearrange("p (h e) -> p h e", e=130)[:, :, 0:128], ps[6][:].rearrange("p (h e) -> p h e", e=128), AF.Copy,
                r=[PS[6]], w=[("vext", i)])
            cp("dve", ust[s][:], ps[7][:], r=[PS[7]], w=[("ust", s)])
            if "ubmd" not in KDIS:
                dma("sp", u_d[tok, :], ust[s][:], r=[("ust", s)], w=[("u_d", i // 16)])
            KC = int(os.environ.get("KC", "9"))
            if KC < 2:
                return
            act(sqb[:, 0:512], ps[bq][:], AF.Square, r=[PS[bq]], w=["sqb"])
            act(sqb[:, 512:1024], ps[bk][:], AF.Square, r=[PS[bk], "sqb"], w=["sqb"])
            s16 = st16[:, i, :]
            S.op("dve", lambda e: e.tensor_reduce(out=s16, in_=sqb.rearrange("p (a b) -> p a b", b=64), axis=AX.X, op=ALU.add), r=["sqb"], w=[("st16", i)])
            ts("dve", s16, s16, 1.0 / 64, EPS, ALU.mult, ALU.add, r=[("st16", i)], w=[("st16", i)])
            act(s16, s16, AF.Sqrt, r=[("st16", i)], w=[("st16", i)])
            S.op("dve", lambda e: e.reciprocal(out=s16, in_=s16), r=[("st16", i)], w=[("st16", i)])
            tt("dve", t1[:, 0:512].rearrange("p (a b) -> p a b", b=64), ps[bq][:].rearrange("p (a b) -> p a b", b=64),
               st16[:, i, 0:8].unsqueeze(2).to_broadcast([128, 8, 64]), ALU.mult, r=[PS[bq], ("st16", i)], w=["t1"])
            tt("dve", t1[:, 512:1024].rearrange("p (a b) -> p a b", b=64), ps[bk][:].rearrange("p (a b) -> p a b", b=64),
               st16[:, i, 8:16].unsqueeze(2).to_broadcast([128, 8, 64]), ALU.mult, r=[PS[bk], ("st16", i), "t1"], w=["t1"])
            tt("dve", t1.rearrange("p (k a d) -> p k a d", k=2, d=64), t1.rearrange("p (k a d) -> p k a d", k=2, d=64),
               gqk[:].rearrange("p (k d) -> p k d", d=64).unsqueeze(2).to_broadcast([128, 2, 8, 64]), ALU.mult, r=["t1", "gqk"], w=["t1"])
            if KC < 3:
                return
            tv = t1.rearrange("p (a two d) -> p a two d", two=2, d=32)
            ta, tb = tv[:, :, 0, :], tv[:, :, 1, :]
            cosb = rcs[s][:, 0:32].unsqueeze(1).to_broadcast([128, 16, 32])
            sinb = rcs[s][:, 32:64].unsqueeze(1).to_broadcast([128, 16, 32])
            qk_t = qkr[s]
            ov = qk_t.rearrange("p (a two d) -> p a two d", two=2, d=32)
            v3 = lambda t: t.rearrange("p (a d) -> p a d", d=32)
            rc = [("rcs", s)]
            tt("dve", v3(m1), ta, cosb, ALU.mult, r=["t1", "sqb"] + rc, w=["sqb"])
            tt("dve", v3(m2), tb, sinb, ALU.mult, r=["t1", "sqb"] + rc, w=["sqb"])
            tt("dve", ov[:, :, 0, :], v3(m1), v3(m2), ALU.subtract, r=["sqb"], w=[("qkr", s)])
            pe_ = "dve" if "pool" in KDIS else "pool"
            tt(pe_, v3(m3), ta, sinb, ALU.mult, r=["t1"] + rc, w=["m3"])
            tt(pe_, v3(m4), tb, cosb, ALU.mult, r=["t1"] + rc, w=["m4"])
            tt(pe_, ov[:, :, 1, :], v3(m3), v3(m4), ALU.add, r=["m3", "m4", ("qkr", s)], w=[("qkr", s)])
            if KC < 4:
                return
            pv = ps[bt][:].bitcast(BF16)
            for c in range(8):
                tr(pv[:, c * 128:(c + 1) * 128], qk_t[:, c * 128:(c + 1) * 128], ident_b[:], r=[("qkr", s), "ident_b"], w=[PS[bt]])
            pv3 = pv.rearrange("p (c t) -> p c t", t=128)
            KV = os.environ.get("KV", "abc")
            for c in range(4):
                if "b" in KV:
                    evac_bf("act", qT[:, c, tok], pv3[:, c, :], r=[PS[bt]], w=[("qT", i)])
                if "c" in KV:
                    evac_bf("act", kT[:, c, tok], pv3[:, 4 + c, :], r=[PS[bt]], w=[("kT", i)])

        for half in range(2):
            for i in range(16 * half, 16 * half + 16):
                phaseB_tile(i)
            if half == 0:
                for c in range(8):
                    dbg_dump("hT%d" % c, hT[:, c, :], [128, 2048], BF16, [("hT", i) for i in range(16)])
            for i in range(16 * half, 16 * half + 16):
                if stage >= 2:
                    phaseC_tile(i)
        dbg_dump("qTs", qT[:, 1, 1024:2048], [128, 1024], BF16, [("qT", i) for i in range(NT)])
        dbg_dump("kTs", kT[:, 2, 3072:4096], [128, 1024], BF16, [("kT", i) for i in range(NT)])
        for h in range(4):
            dbg_dump("qT%d" % h, qT[:, h, :], [128, T], BF16, [("qT", i) for i in range(NT)])
            dbg_dump("kT%d" % h, kT[:, h, :], [128, T], BF16, [("kT", i) for i in range(NT)])
        for q4 in range(4):
            dbg_dump("vext%d" % q4, vext[:, q4 * 8:(q4 + 1) * 8, :], [128, 8, 520], BF16, [("vext", i) for i in range(NT)] + ["vext1"])
        negb = sb("negb", [128, 4])
        S.op("dve", lambda e: e.tensor_reduce(out=negb[:, 0:1], in_=gqk[:, 0:64], axis=AX.X, op=ALU.max, apply_absolute_value=True), r=["gqk"], w=["negb"])
        S.op("dve", lambda e: e.tensor_reduce(out=negb[:, 1:2], in_=gqk[:, 64:128], axis=AX.X, op=ALU.max, apply_absolute_value=True), r=["gqk", "negb"], w=["negb"])
        stt("dve", negb[:, 2:3], negb[:, 0:1], -8.0, negb[:, 1:2], ALU.mult, ALU.mult, r=["negb"], w=["negb"])
        S.barrier()
        if stage <= 2:
            S.emit(); return nc

        PI = math.pi
        scol = carve(138, [128, 3, 16])
        sw = carve(138.25, [128, 16, 16])
        bbp = carve(139.25, [128, 2, 16, 32])
        ccp = carve(143.25, [128, 2, 16, 32])
        pw = carve(147.25, [128, 2, 16, 17])
        pwn = carve(149.375, [128, 2, 16, 17])
        pwr = carve(151.5, [128, 2, 16, 16])
        a2k = carve(153.5, [128, 2, 16, 8])
        dcol = carve(154.5, [128, 16])
        btmp = carve(154.75, [128, 2, 16, 16])
        ctmp = carve(156.75, [128, 2, 16, 16])
        dma("sp", scol, I["ssm_cols"], w=["scol"])
        dma("sp", btmp, I["ssm_b"], w=["btmp"])
        dma("sp", ctmp, I["ssm_c"], w=["ctmp"])
        dma("sp", dcol, I["ssm_dcol"], w=["dcol"])
        SW = lambda k: sw[:, k, :]
        are, aim, ldt = scol[:, 0, :], scol[:, 1, :], scol[:, 2, :]
        W_ = ["sw"]

        def dv(o, a, b, op):
            tt("dve", o, a, b, op, r=W_ + ["scol"], w=W_)

        def dsc(o, a, s1, s2, op0, op1=None):
            ts("dve", o, a, s1, s2, op0, op1, r=W_ + ["scol"], w=W_)
        act(SW(0), ldt, AF.Exp, r=["scol"], w=W_)
        dv(SW(1), are, SW(0), ALU.mult)
        act(SW(2), SW(1), AF.Exp, r=W_, w=W_)
        dv(SW(3), aim, SW(0), ALU.mult)
        def range_reduce(dst, shift):
            dsc(dst, SW(3), shift, None, ALU.add)
            for _ in range(8):
                dsc(SW(14), dst, PI, None, ALU.is_gt)
                stt("dve", dst, SW(14), -2 * PI, dst, ALU.mult, ALU.add, r=W_, w=W_)
        range_reduce(SW(4), 0.0)
        range_reduce(SW(5), 0.5 * PI)
        act(SW(4), SW(4), AF.Sin, r=W_, w=W_)
        act(SW(5), SW(5), AF.Sin, r=W_, w=W_)
        dv(SW(6), SW(2), SW(5), ALU.mult)
        dv(SW(7), SW(2), SW(4), ALU.mult)
        dv(SW(8), are, are, ALU.mult)
        dv(SW(9), aim, aim, ALU.mult)
        dv(SW(8), SW(8), SW(9), ALU.add)
        S.op("dve", lambda e: e.reciprocal(out=SW(8), in_=SW(8)), r=W_, w=W_)
        dsc(SW(9), SW(6), -1.0, None, ALU.add)
        dv(SW(10), SW(9), are, ALU.mult)
        dv(SW(11), SW(7), aim, ALU.mult)
        dv(SW(10), SW(10), SW(11), ALU.add)
        dv(SW(10), SW(10), SW(8), ALU.mult)
        dv(SW(11), SW(7), are, ALU.mult)
        dv(SW(12), SW(9), aim, ALU.mult)
        dv(SW(11), SW(11), SW(12), ALU.subtract)
        dv(SW(11), SW(11), SW(8), ALU.mult)
        dv(SW(12), SW(2), SW(2), ALU.mult)
        S.op("dve", lambda e: e.reciprocal(out=SW(12), in_=SW(12)), r=W_, w=W_)
        dv(SW(13), SW(6), SW(12), ALU.mult)
        dv(SW(14), SW(7), SW(12), ALU.mult)
        dsc(SW(14), SW(14), -1.0, None, ALU.mult)
        S.op("pool", lambda e: e.memset(bbp, 0.0), w=["bbp"])
        S.op("pool", lambda e: e.memset(ccp, 0.0), w=["ccp"])
        fre_b = SW(10).unsqueeze(2).to_broadcast([128, 16, 16])
        fim_b = SW(11).unsqueeze(2).to_broadcast([128, 16, 16])
        bre, bim = btmp[:, 0], btmp[:, 1]
        t_a = ctmp
        x1_, x2_ = pwr[:, 0], pwr[:, 1]
        tt("dve", x1_, bre, fre_b, ALU.mult, r=["btmp"] + W_, w=["pwr"])
        tt("dve", x2_, bim, fim_b, ALU.mult, r=["btmp"] + W_ + ["pwr"], w=["pwr"])
        tt("dve", x1_, x1_, x2_, ALU.subtract, r=["pwr"], w=["pwr"])
        for hh in range(2):
            cp("dve", bbp[64 * hh:64 * hh + 64, 0, :, 16 * hh:16 * hh + 16], x1_[64 * hh:64 * hh + 64], r=["pwr", "bbp"], w=["bbp"])
        tt("dve", x1_, bim, fre_b, ALU.mult, r=["btmp", "bbp"] + W_ + ["pwr"], w=["pwr"])
        tt("dve", x2_, bre, fim_b, ALU.mult, r=["btmp"] + W_ + ["pwr"], w=["pwr"])
        tt("dve", x1_, x1_, x2_, ALU.add, r=["pwr"], w=["pwr"])
        for hh in range(2):
            cp("dve", bbp[64 * hh:64 * hh + 64, 1, :, 16 * hh:16 * hh + 16], x1_[64 * hh:64 * hh + 64], r=["pwr", "bbp"], w=["bbp"])
            for ri in range(2):
                cp("dve", ccp[64 * hh:64 * hh + 64, ri, :, 16 * hh:16 * hh + 16], ctmp[64 * hh:64 * hh + 64, ri], r=["ctmp", "ccp"], w=["ccp"])
        PWT = ["pw", "pwn", "pwr", "a2k"]

        def cmul_col(ore, oim, xre, xim, yre, yim, tmp1, tmp2):
            tt("dve", tmp1, xre, yre, ALU.mult, r=PWT + W_, w=PWT + W_)
            tt("dve", tmp2, xim, yim, ALU.mult, r=PWT + W_, w=PWT + W_)
            tt("dve", oim, xre, yim, ALU.mult, r=PWT + W_, w=PWT + W_)
            tt("dve", ore, tmp1, tmp2, ALU.subtract, r=PWT + W_, w=PWT + W_)
            tt("dve", tmp1, xim, yre, ALU.mult, r=PWT + W_, w=PWT + W_)
            tt("dve", oim, oim, tmp1, ALU.add, r=PWT + W_, w=PWT + W_)
        S.op("dve", lambda e: e.memset(pw[:, 0, :, 0:1], 1.0), r=PWT, w=PWT)
        S.op("dve", lambda e: e.memset(pw[:, 1, :, 0:1], 0.0), r=PWT, w=PWT)
        S.op("dve", lambda e: e.memset(pwn[:, 0, :, 0:1], 1.0), r=PWT, w=PWT)
        S.op("dve", lambda e: e.memset(pwn[:, 1, :, 0:1], 0.0), r=PWT, w=PWT)
        for k in range(16):
            cmul_col(pw[:, 0, :, k + 1], pw[:, 1, :, k + 1], pw[:, 0, :, k], pw[:, 1, :, k], SW(6), SW(7), SW(0), SW(1))
            cmul_col(pwn[:, 0, :, k + 1], pwn[:, 1, :, k + 1], pwn[:, 0, :, k], pwn[:, 1, :, k], SW(13), SW(14), SW(0), SW(1))
        for tau in range(16):
            cp("dve", pwr[:, :, :, tau], pw[:, :, :, 15 - tau], r=PWT, w=PWT)
        cp("dve", a2k[:, :, :, 0], pw[:, :, :, 16], r=PWT, w=PWT)
        for l in range(7):
            cmul_col(a2k[:, 0, :, l + 1], a2k[:, 1, :, l + 1], a2k[:, 0, :, l], a2k[:, 1, :, l], a2k[:, 0, :, l], a2k[:, 1, :, l], SW(0), SW(1))
        if "ssmpar" in dbg:
            dbg_dump("pw", pw, [128, 2, 16, 17], F32, PWT, True)
            dbg_dump("pwn", pwn, [128, 2, 16, 17], F32, PWT, True)
            dbg_dump("a2k", a2k, [128, 2, 16, 8], F32, PWT, True)
            dbg_dump("bbp", bbp, [128, 2, 16, 32], F32, ["bbp"], True)

        attT = carve(0, [128, 4, T], BF16)
        pt = [carve(130 + i, [128, 512], BF16) for i in range(2)]
        lamv = carve(132, [128, 256]); lamp = carve(133, [128, 128])
        gsub = carve(133.5, [128, 128]); tri = carve(134, [128, 128], BF16)
        osb = [carve(135 + 0.5 * i, [128, 128]) for i in range(2)]
        onb = [carve(136 + 0.25 * i, [128, 128], BF16) for i in range(2)]
        osq = carve(136.5, [128, 128], BF16)
        lamc = sb("lamc", [128, 8])
        ast = carve(137, [128, 128, 4])
        dma("sp", lamv, I["lamv"], w=["lamv"])
        dma("sp", gsub, I["gsub"], w=["gsub"])
        dma("sp", tri, I["tri"], w=["tri"])
        tt("dve", lamp, lamv.rearrange("p (a two d) -> p a two d", two=2, d=64)[:, :, 0, :], lamv.rearrange("p (a two d) -> p a two d", two=2, d=64)[:, :, 1, :],
           ALU.mult, r=["lamv"], w=["lamp"])
        S.op("dve", lambda e: e.tensor_reduce(out=lamc[:, 0:2], in_=lamp.rearrange("p (a d) -> p a d", d=64), axis=AX.X, op=ALU.add), r=["lamp"], w=["lamc"])
        act(lamc[:, 2:4], lamc[:, 0:2], AF.Exp, r=["lamc"], w=["lamc"])
        stt("dve", lamc[:, 4:5], lamc[:, 3:4], -0.2, lamc[:, 2:3], ALU.add, ALU.subtract, r=["lamc"], w=["lamc"])
        ts("dve", gsub, gsub, 0.8, None, ALU.mult, r=["gsub"], w=["gsub"])
        neglam = lamc[:, 4:5]
        step = 0
        for h in range(0 if "att" not in KDIS else 4, 4):
            for Q2 in range(16):
                nkt = 2 * Q2 + 2
                for kt in range(nkt):
                    s = step % 2
                    step += 1
                    for c in range(2):
                        mm(ps[2 * s + c][:, 0:256], kT[64 * c:64 * c + 64, h, kt * 128:(kt + 1) * 128], qT[64 * c:64 * c + 64, h, Q2 * 256:(Q2 + 1) * 256],
                           True, True, r=[("kT", kt), ("qT", 2 * Q2), ("qT", 2 * Q2 + 1)], w=[PS[2 * s + c]])
                    for c in range(2):
                        act(pt[s][:, c * 256:(c + 1) * 256], ps[2 * s + c][:, 0:256], AF.Exp, r=[PS[2 * s + c], "negb", ("pt", s)], w=[("pt", s)], scale=0.125, bias=negb[:, 2:3])
                    KA = int(os.environ.get("KA", "9"))
                    if kt >= 2 * Q2 and KA >= 2:
                        r0 = kt - 2 * Q2
                        pv_ = pt[s].rearrange("p (c q) -> p c q", q=256)[:, :, r0 * 128:(r0 + 1) * 128]
                        tt("pool", pv_, pv_, tri.unsqueeze(1).to_broadcast([128, 2, 128]), ALU.mult, r=[("pt", s), "tri"], w=[("pt", s)])
                    for r in range(2):
                        if 2 * Q2 + r < kt or KA < 3:
                            continue
                        for c in range(2):
                            b = 4 + 2 * c + r
                            mm(ps[b][:, 0:129], pt[s][:, c * 256 + r * 128:c * 256 + (r + 1) * 128], vext[:, kt, h * 130:h * 130 + 129],
                               kt == 0, kt == 2 * Q2 + r, r=[("pt", s), ("vext", kt), "vext1"], w=[PS[b]])
                for r in range(2 if KA >= 4 else 0):
                    qt = 2 * Q2 + r
                    u_ = (h * 32 + qt) % 2
                    a4 = ast[:, qt, :] if h == 0 else ast[:, (h * 32 + qt) % 128, :]
                    tkn = ("ast", (h * 32 + qt) % 128)
                    b0, b1 = 4 + r, 6 + r
                    S.op("dve", (lambda a4, b0: lambda e: e.reciprocal(out=a4[:, 0:1], in_=ps[b0][:, 128:129]))(a4, b0), r=[PS[b0]], w=[tkn])
                    S.op("dve", (lambda a4, b1: lambda e: e.reciprocal(out=a4[:, 1:2], in_=ps[b1][:, 128:129]))(a4, b1), r=[PS[b1], tkn], w=[tkn])
                    tt("dve", a4[:, 1:2], a4[:, 1:2], neglam, ALU.mult, r=[tkn, "lamc"], w=[tkn])
                    ts("dve", osb[u_], ps[b0][:, 0:128], a4[:, 0:1], None, ALU.mult, r=[PS[b0], tkn], w=[("osb", u_)])
                    stt("dve", osb[u_], ps[b1][:, 0:128], a4[:, 1:2], osb[u_], ALU.mult, ALU.add, r=[PS[b1], tkn, ("osb", u_)], w=[("osb", u_)])
                    act(osq, osb[u_], AF.Square, r=[("osb", u_)], w=["osq", tkn], accum_out=a4[:, 2:3], scale=1.0 / math.sqrt(128.0))
                    ts("dve", a4[:, 2:3], a4[:, 2:3], EPS, None, ALU.add, r=[tkn], w=[tkn])
                    act(a4[:, 2:3], a4[:, 2:3], AF.Sqrt, r=[tkn], w=[tkn])
                    S.op("dve", (lambda a4: lambda e: e.reciprocal(out=a4[:, 2:3], in_=a4[:, 2:3]))(a4), r=[tkn], w=[tkn])
                    stt("dve", onb[u_], osb[u_], a4[:, 2:3], gsub, ALU.mult, ALU.mult, r=[("osb", u_), tkn, "gsub"], w=[("onb", u_)])
                    bt = u_
                    pvb = ps[bt][:].bitcast(BF16)
                    tr(pvb[:, 0:128], onb[u_], ident_b[:], r=[("onb", u_), "ident_b"], w=[PS[bt]])
                    evac_bf("act", attT[:, h, qt * 128:(qt + 1) * 128], pvb[:, 0:128], r=[PS[bt]], w=[("attT", h)])
        for h in range(4):
            dbg_dump("attT%d" % h, attT[:, h, :], [128, T], BF16, [("attT", h)])
        S.barrier()
        if stage <= 3:
            S.emit(); return nc

        ubm = carve(32, [128, 2, 16, 512], BF16)
        ybm = carve(64, [128, 2, 16, 512], BF16)
        w3mask = carve(96, [128, 4, 512], BF16); w3eye = carve(100, [128, 4, 512], BF16)
        for jt in range(2):
            for t4 in range(4):
                dma("sp", ubm[:, jt, 4 * t4:4 * t4 + 4, :], u_d[jt * 2048:(jt + 1) * 2048, :].rearrange("(b t) c -> b t c", t=16)[:, 4 * t4:4 * t4 + 4, :],
                    w=["ubm%d%d" % (jt, t4)])
        UBM = ["ubm%d%d" % (jt, t4) for jt in range(2) for t4 in range(4)]
        dma("sp", w3mask, I["w3mask"], w=["w3mask"])
        dma("sp", w3eye, I["w3eye"], w=["w3eye"])
        xsc = {"dve": (carve(172, [128, 16, 32]), carve(174, [128, 16, 32])), "pool": (carve(176, [128, 16, 32]), carve(178, [128, 16, 32]))}
        g1b = [carve(180 + 2 * i, [128, 512]) for i in range(2)]
        g2b = [carve(184 + 2 * i, [128, 512]) for i in range(2)]

        def cprod(eng, ore, oim, are_, aim_, bre_, bim_, negim=False):
            xa, xb = xsc[eng]
            tk = "xsc_" + eng
            rr = PWT + ["bbp", "ccp"]
            tt(eng, xa, are_, bre_, ALU.mult, r=rr + [tk], w=[tk])
            tt(eng, xb, aim_, bim_, ALU.mult, r=rr + [tk], w=[tk])
            tt(eng, ore[0], xa, xb, ALU.subtract, r=[tk], w=[ore[1]])
            tt(eng, xa, are_, bim_, ALU.mult, r=rr + [tk], w=[tk])
            tt(eng, xb, aim_, bre_, ALU.mult, r=rr + [tk], w=[tk])
            if negim:
                tt(eng, xa, xa, xb, ALU.add, r=[tk], w=[tk])
                ts(eng, oim[0], xa, -1.0, None, ALU.mult, r=[tk], w=[oim[1]])
            else:
                tt(eng, oim[0], xa, xb, ALU.add, r=[tk], w=[oim[1]])

        def ssm_j(j):
            s = j % 2
            base = 104 + 16 * s
            utj = carve(base, [128, 4, 256], BF16)
            w1t = carve(base + 2, [128, 2, 512], BF16)
            bh = carve(base + 4, [128, 2, 512], BF16)
            w2 = carve(base + 6, [128, 2, 512], BF16)
            w1l = carve(base + 8, [128, 2, 4, 128], BF16)
            w3 = carve(base + 10, [128, 4, 512], BF16)
            sbase = 160 + 6 * s
            sre = carve(sbase, [128, 257]); sim_ = carve(sbase + 1.25, [128, 257])
            tre = carve(sbase + 2.5, [128, 256]); tim = carve(sbase + 3.5, [128, 256])
            sbb = carve(sbase + 4.5, [128, 2, 258], BF16)
            tim2 = carve(188 + s, [128, 256])
            K = lambda n: (n, s)
            bt = 2 + s
            pv = ps[bt][:].bitcast(BF16)
            ustg = carve(base + 14, [128, 2, 512], BF16)
            for jt in range(2):
                cp("pool", ustg[:, jt, :].rearrange("p (t c) -> p t c", c=32), ubm[:, jt, :, 32 * j:32 * j + 32], r=UBM, w=[K("ustg")])
            for t4 in range(4):
                for jt in range(2):
                    tr(pv[:, (t4 * 2 + jt) * 128:(t4 * 2 + jt + 1) * 128], ustg[:, jt, t4 * 128:(t4 + 1) * 128], ident_b[:],
                       r=[K("ustg"), "ident_b"], w=[PS[bt]])
            evac_bf("act", utj.rearrange("p a b -> p (a b)"), pv, r=[PS[bt]], w=[K("utj")])
            KS = int(os.environ.get("KS", "9"))
            if KS < 2:
                return
            v16 = lambda ap: ap.rearrange("p (t c) -> p t c", c=32)
            PR = lambda tab, ri, lo: tab[:, ri, j, lo:lo + 16].unsqueeze(2).to_broadcast([128, 16, 32])
            BB = lambda tab, ri: tab[:, ri, j, :].unsqueeze(1).to_broadcast([128, 16, 32])
            cprod("dve", (v16(w1t[:, 0, :]), K("w1t")), (v16(w1t[:, 1, :]), K("w1t")), PR(pwr, 0, 0), PR(pwr, 1, 0), BB(bbp, 0), BB(bbp, 1))
            cprod("pool", (v16(bh[:, 0, :]), K("bh")), (v16(bh[:, 1, :]), K("bh")), PR(pwn, 0, 1), PR(pwn, 1, 1), BB(bbp, 0), BB(bbp, 1))
            cprod("pool", (v16(w2[:, 0, :]), K("w2")), (v16(w2[:, 1, :]), K("w2")), PR(pw, 0, 1), PR(pw, 1, 1), BB(ccp, 0), BB(ccp, 1), negim=True)
            bw = s
            pvw = ps[bw][:].bitcast(BF16)
            for ri in range(2):
                for t4 in range(4):
                    tr(pvw[:, (ri * 4 + t4) * 128:(ri * 4 + t4 + 1) * 128], w1t[:, ri, t4 * 128:(t4 + 1) * 128], ident_b[:], r=[K("w1t"), "ident_b"], w=[PS[bw]])
            evac_bf("act", w1l.rearrange("p a b c -> p (a b c)"), pvw, r=[PS[bw]], w=[K("w1l")])
            for t4 in range(4):
                mm(ps[4][:], bh[:, 0, t4 * 128:(t4 + 1) * 128], w2[:, 0, :], True, False, r=[K("bh"), K("w2")], w=[PS[4]])
                mm(ps[4][:], bh[:, 1, t4 * 128:(t4 + 1) * 128], w2[:, 1, :], False, True, r=[K("bh"), K("w2")], w=[PS[4]])
                tt("dve", w3[:, t4, :], ps[4][:], w3mask[:, t4, :], ALU.mult, r=[PS[4], "w3mask"], w=[K("w3")])
                stt("dve", w3[:, t4, :], w3eye[:, t4, :], dcol[:, j:j + 1], w3[:, t4, :], ALU.mult, ALU.add, r=[K("w3"), "w3eye", "dcol"], w=[K("w3")])
            if KS < 3:
                return
            for ri in range(2):
                bv = 5 - ri
                for t4 in range(4):
                    mm(ps[bv][:, 0:256], w1l[:, ri, t4, :], utj[:, t4, :], t4 == 0, t4 == 3, r=[K("w1l"), K("utj")], w=[PS[bv]])
            S.op("dve", lambda e: e.memset(sre[:, 0:1], 0.0), w=[K("sre")])
            S.op("pool", lambda e: e.memset(sim_[:, 0:1], 0.0), w=[K("sim")])
            cp("dve", sre[:, 1:257], ps[5][:, 0:256], r=[PS[5]], w=[K("sre")])
            cp("act", sim_[:, 1:257], ps[4][:, 0:256], r=[PS[4]], w=[K("sim")])
            for l in range(8):
                d = 1 << l
                n = 256 - d
                ar_, ai_ = a2k[:, 0, j, l:l + 1], a2k[:, 1, j, l:l + 1]
                ts("dve", tre[:, 0:n], sim_[:, 1:1 + n], ai_, None, ALU.mult, r=[K("sim")] + PWT, w=[K("tre")])
                stt("dve", tre[:, 0:n], sre[:, 1:1 + n], ar_, tre[:, 0:n], ALU.mult, ALU.subtract, r=[K("sre"), K("tre")] + PWT, w=[K("tre")])
                ts("pool", tim[:, 0:n], sre[:, 1:1 + n], ai_, None, ALU.mult, r=[K("sre")] + PWT, w=[K("tim")])
                ts("pool", tim2[:, 0:n], sim_[:, 1:1 + n], ar_, None, ALU.mult, r=[K("sim")] + PWT, w=[K("tim2")])
                tt("pool", tim[:, 0:n], tim[:, 0:n], tim2[:, 0:n], ALU.add, r=[K("tim"), K("tim2")], w=[K("tim")])
                tt("dve", sre[:, 1 + d:257], sre[:, 1 + d:257], tre[:, 0:n], ALU.add, r=[K("sre"), K("tre")], w=[K("sre")])
                tt("pool", sim_[:, 1 + d:257], sim_[:, 1 + d:257], tim[:, 0:n], ALU.add, r=[K("sim"), K("tim")], w=[K("sim")])
            cp("dve", sbb[:, 0, 0:257], sre[:, 0:257], r=[K("sre")], w=[K("sbb")])
            cp("pool", sbb[:, 1, 0:257], sim_[:, 0:257], r=[K("sim"), K("sbb")], w=[K("sbb")])
            if KS < 4:
                return
            for jt in range(2):
                by = 6 + jt
                bl = slice(jt * 128, (jt + 1) * 128)
                mm(ps[by][:], sbb[:, 0, bl], w2[:, 0, :], True, False, r=[K("sbb"), K("w2")], w=[PS[by]])
                mm(ps[by][:], sbb[:, 1, bl], w2[:, 1, :], False, False, r=[K("sbb"), K("w2")], w=[PS[by]])
                for t4 in range(4):
                    mm(ps[by][:], utj[:, t4, bl], w3[:, t4, :], False, t4 == 3, r=[K("utj"), K("w3")], w=[PS[by]])
                if "ssm_y" in dbg:
                    cp("dve", ybm[:, jt, :, 32 * j:32 * j + 32], ps[by][:].rearrange("p (t c) -> p t c", c=32), r=[PS[by]], w=["ybm"])
                    continue
                g1, g2 = g1b[jt], g2b[jt]
                act(g1, ps[by][:], AF.Square, r=[PS[by]], w=[("g1", jt)])
                ts("dve", g1, g1, 0.044715, 1.0, ALU.mult, ALU.add, r=[("g1", jt)], w=[("g1", jt)])
                tt("dve", g1, g1, ps[by][:], ALU.mult, r=[("g1", jt), PS[by]], w=[("g1", jt)])
                act(g2, g1, AF.Sigmoid, r=[("g1", jt)], w=[("g2", jt)], scale=2.0 * math.sqrt(2.0 / math.pi))
                tt("dve", ybm[:, jt, :, 32 * j:32 * j + 32], g2.rearrange("p (t c) -> p t c", c=32), ps[by][:].rearrange("p (t c) -> p t c", c=32), ALU.mult,
                   r=[("g2", jt), PS[by]], w=["ybm"])

        for j in range(16):
            ssm_j(j)
        if "ybm" in dbg or "ssm_y" in dbg:
            for jt in range(2):
                for t4 in range(4):
                    dbg_dump("ybm%d_%d" % (jt, t4), ybm[:, jt, 4 * t4:4 * t4 + 4, :], [128, 4, 512], BF16, ["ybm"], True)
        S.barrier()
        if stage <= 4:
            S.emit(); return nc

        yT = carve(32, [128, 4, T], BF16)
        ssmT = carve(100, [128, 4, T], BF16)
        wglu = carve(96, [128, 4, 512], BF16)
        ones_b = carve(132, [128, 128], BF16)
        sgb = [carve(133 + i, [128, 512], BF16) for i in range(2)]
        sqb2 = [carve(135 + i, [128, 512], BF16) for i in range(2)]
        rsb = [carve(137 + 2 * i, [128, 512]) for i in range(2)]
        S.op("pool", lambda e: e.memset(ones_b, 1.0), w=["ones_b"])
        dma("pool", wglu, I["w_glu"].rearrange("(ko p) n -> p ko n", p=128), w=["wglu"])
        gi = 0
        for jt in range(2):
            for ct in range(4):
                for half in range(2):
                    b = gi % 4
                    pv = ps[b][:].bitcast(BF16)
                    for t8 in range(8):
                        tr(pv[:, t8 * 128:(t8 + 1) * 128], ybm[:, jt, half * 8 + t8, ct * 128:(ct + 1) * 128], ident_b[:], r=["ybm", "ident_b"], w=[PS[b]])
                    o = yT[:, ct, jt * 2048:(jt + 1) * 2048].rearrange("p (b t) -> p t b", t=16)[:, half * 8:half * 8 + 8, :]
                    src = pv.rearrange("p (t b) -> p t b", b=128)
                    for t8 in range(8):
                        evac_bf("act" if (gi + t8) % 2 == 0 else "dve", o[:, t8, :], src[:, t8, :], r=[PS[b]], w=[("yT", jt)])
                    gi += 1
        for tb in range(8):
            tbs = slice(tb * 512, (tb + 1) * 512)
            jt = tb // 4
            u_ = tb % 2
            bt_ = 6 + u_
            for ft in range(4):
                b = 4 + ft % 2
                for kt in range(4):
                    mm(ps[b][:], wglu[:, kt, ft * 128:(ft + 1) * 128], yT[:, kt, tbs], kt == 0, kt == 3, r=["wglu", ("yT", jt)], w=[PS[b]])
                sg, sq = sgb[ft % 2], sqb2[ft % 2]
                act(sg, ps[b][:], AF.Sigmoid, r=[PS[b]], w=[("sg", ft % 2)])
                tt("dve", ssmT[:, ft, tbs], yT[:, ft, tbs], sg, ALU.mult, r=[("yT", jt), ("sg", ft % 2)], w=[("ssmT", tb)])
                tt("pool", sq, ssmT[:, ft, tbs], ssmT[:, ft, tbs], ALU.mult, r=[("ssmT", tb)], w=[("sq2", ft % 2)])
                mm(ps[bt_][:], ones_b, sq, ft == 0, ft == 3, r=["ones_b", ("sq2", ft % 2)], w=[PS[bt_]])
            rs = rsb[u_]
            ts("dve", rs, ps[bt_][:], 1.0 / 512, EPS, ALU.mult, ALU.add, r=[PS[bt_]], w=[("rs", u_)])
            act(rs, rs, AF.Sqrt, r=[("rs", u_)], w=[("rs", u_)])
            S.op("dve", (lambda rs: lambda e: e.reciprocal(out=rs, in_=rs))(rs), r=[("rs", u_)], w=[("rs", u_)])
            for ft in range(4):
                stt("dve", ssmT[:, ft, tbs], ssmT[:, ft, tbs], vc1[:, 72 + ft:73 + ft], rs, ALU.mult, ALU.mult,
                    r=[("ssmT", tb), ("rs", u_), "vc1"], w=[("ssmT", tb)])
        for ct in range(4):
            dbg_dump("ssmT%d" % ct, ssmT[:, ct, :], [128, T], BF16, [("ssmT", tb) for tb in range(8)])
        S.barrier()
        if stage <= 5:
            S.emit(); return nc

        wout = carve(32, [128, 8, 1024], BF16)
        bct = {"g1": carve(48, [128, 1024]), "a2": carve(52, [128, 1024]), "b2": carve(56, [128, 1024]), "g2": carve(60, [128, 1024])}
        wr = carve(64, [128, 8, 256], BF16)
        wgu = carve(68, [128, 8, 512], BF16)
        wds = carve(76, [128, 2, 1024], BF16)
        rbias = carve(80, [128, 256])
        gatesT = carve(82, [128, 2, T], BF16)
        h2T_d = nc.dram_tensor("scr_h2T", [128, 8, T], BF16, kind="ExternalOutput").ap()
        diag = [carve(81 + 0.5 * i, [128, 128]) for i in range(2)]
        xt2 = [carve(132 + 4 * i, [128, 1024]) for i in range(2)]
        x1t = [carve(140 + 4 * i, [128, 1024]) for i in range(2)]
        h2tok = [carve(152 + 2 * i, [128, 1024], BF16) for i in range(2)]
        h2T = [carve(156 + 2 * i, [128, 8, 128], BF16) for i in range(2)]
        rsc = [carve(176 + i, [128, 256]) for i in range(8)]
        mbf = carve(168, [128, 256], BF16)
        ash = carve(168.5, [128, 256], BF16); ashT = carve(169, [128, 2, 128], BF16)
        sm = carve(185, [128, 128])
        x1p_d = out
        wo_src = I["w_out"].rearrange("(ko p) n -> p ko n", p=128)
        for k in range(8):
            dma("pool", wout[:, k, :], wo_src[:, k, :], w=[("wout", k)])
        WOUT = [("wout", k) for k in range(8)]
        dma("pool", wr, I["w_router"].rearrange("(ko p) n -> p ko n", p=128), w=["wr"])
        dma("pool", wgu[:, :, 0:256], I["w_gate_s"].rearrange("(ko p) n -> p ko n", p=128), w=["wgu0"])
        dma("pool", wgu[:, :, 256:512], I["w_up_s"].rearrange("(ko p) n -> p ko n", p=128), w=["wgu1"])
        dma("pool", wds, I["w_down_s"].rearrange("(ko p) n -> p ko n", p=128), w=["wds"])
        dma("sp", rbias, I["rbias"], w=["rbias"])
        bi = 0
        for name, col in (("g1", mod[:, 16:24]), ("a2", ab[:, 16:24]), ("b2", ab[:, 24:32]), ("g2", mod[:, 40:48])):
            for hf in range(2):
                for j4 in range(4):
                    jj = hf * 4 + j4
                    dg = diag[bi % 2]
                    ts("dve", dg, ident_f[:], col[:, jj:jj + 1], None, ALU.mult, r=["ident_f", "mod", "ab"], w=[("diag", bi % 2)])
                    mm(ps[hf][:, j4 * 128:(j4 + 1) * 128], ones_f[:], dg, True, True, r=["ones_f", ("diag", bi % 2)], w=[PS[hf]])
                    bi += 1
                cp("act", bct[name][:, hf * 512:(hf + 1) * 512], ps[hf][:], r=[PS[hf]], w=["bc_" + name])

        def phaseF_tile(i):
            s = i % 2
            tok = slice(i * 128, (i + 1) * 128)
            x_t, x1, h2t, h2T_ = xt2[s], x1t[s], h2tok[s], h2T[s]
            K = lambda n: (n, s)
            smc = lambda a, b=None: sm[:, (64 * s + a):(64 * s + (a + 1 if b is None else b))]
            dma("sp", x_t, I["x"][tok, :], w=[K("xt2")])
            for nb in range(2):
                for kt in range(8):
                    lhs = attT[:, kt, tok] if kt < 4 else ssmT[:, kt - 4, tok]
                    mm(ps[nb][:], lhs, wout[:, kt, nb * 512:(nb + 1) * 512], kt == 0, kt == 7, r=WOUT, w=[PS[nb]])
                nbs = slice(nb * 512, (nb + 1) * 512)
                tt("dve", x1[:, nbs], ps[nb][:], bct["g1"][:, nbs], ALU.mult, r=[PS[nb], "bc_g1"], w=[K("x1")])
            tt("pool", x1, x1, x_t, ALU.add, r=[K("x1"), K("xt2")], w=[K("x1")])
            act(h2t, x1, AF.Square, r=[K("x1")], w=[K("h2t"), K("sm")], accum_out=smc(0), scale=1.0 / math.sqrt(D))
            ts("dve", smc(1), smc(0), EPS, None, ALU.add, r=[K("sm")], w=[K("sm")])
            act(smc(1), smc(1), AF.Sqrt, r=[K("sm")], w=[K("sm")])
            S.op("dve", lambda e: e.reciprocal(out=smc(1), in_=smc(1)), r=[K("sm")], w=[K("sm")])
            stt("dve", x_t, x1, smc(1), bct["a2"], ALU.mult, ALU.mult, r=[K("x1"), K("sm"), "bc_a2", K("xt2")], w=[K("xt2")])
            tt("pool", h2t, x_t, bct["b2"], ALU.add, r=[K("xt2"), "bc_b2"], w=[K("h2t")])
            if "h2" in dbg and i < 2:
                dbg_dump("h2_%d" % i, h2t, [128, 1024], BF16, [K("h2t")], True)
                dbg_dump("x1_%d" % i, x1, [128, 1024], F32, [K("x1")], True)
            pv = ps[2][:].bitcast(BF16)
            for c in range(8):
                tr(pv[:, c * 128:(c + 1) * 128], h2t[:, c * 128:(c + 1) * 128], ident_b[:], r=[K("h2t"), "ident_b"], w=[PS[2]])
            evac_bf("act", h2T_.rearrange("p a b -> p (a b)"), pv, r=[PS[2]], w=[K("h2T")])
            sc, sbv, msb, Mf, posf, tmp, sgs = rsc[0], rsc[1], rsc[2], rsc[3], rsc[4], rsc[5], rsc[6]
            for c in range(8):
                mm(ps[3][:, 0:256], h2T_[:, c, :], wr[:, c, :], c == 0, c == 7, r=[K("h2T"), "wr"], w=[PS[3]])
            act(sc, ps[3][:, 0:256], AF.Sigmoid, r=[PS[3]], w=["sc"])
            tt("dve", sbv, sc, rbias, ALU.add, r=["sc", "rbias"], w=["sbv"])
            mx8 = carve(184, [128, 8, 8])
            for g in range(8):
                S.op("dve", (lambda g: lambda e: e.max(out=mx8[:, g, :], in_=sbv[:, g * 32:(g + 1) * 32]))(g), r=["sbv"], w=["mx8"])
            gs, g8, gm, pen = smc(8, 16), smc(16, 24), smc(24, 32), smc(32, 40)
            tt("dve", gs, mx8[:, :, 0], mx8[:, :, 1], ALU.add, r=["mx8"], w=[K("sm")])
            S.op("dve", lambda e: e.max(out=g8, in_=gs), r=[K("sm")], w=[K("sm")])
            ts("dve", gm, gs, g8[:, 3:4], None, ALU.is_ge, r=[K("sm")], w=[K("sm")])
            ts("dve", pen, gm, -1.0, 1e30, ALU.add, ALU.mult, r=[K("sm")], w=[K("sm")])
            v8 = lambda t: t.rearrange("p (g e) -> p g e", e=32)
            tt("dve", v8(msb), v8(sbv), gm.unsqueeze(2).to_broadcast([128, 8, 32]), ALU.mult, r=["sbv", K("sm")], w=["msb"])
            tt("dve", v8(msb), v8(msb), pen.unsqueeze(2).to_broadcast([128, 8, 32]), ALU.add, r=["msb", K("sm")], w=["msb"])
            top8 = smc(40, 48)
            S.op("dve", lambda e: e.max(out=top8, in_=msb), r=["msb"], w=[K("sm")])
            ts("dve", Mf, msb, top8[:, 7:8], None, ALU.is_ge, r=["msb", K("sm")], w=["Mf"])
            wsum = smc(2)
            S.op("dve", lambda e: e.scalar_tensor_tensor(out=tmp, in0=Mf, scalar=1.0, in1=sc, op0=ALU.mult, op1=ALU.mult, accum_out=wsum),
                 r=["Mf", "sc", "tmp"], w=["tmp", K("sm")])
            S.op("dve", lambda e: e.reciprocal(out=wsum, in_=wsum), r=[K("sm")], w=[K("sm")])
            ts("dve", wsum, wsum, 2.5, None, ALU.mult, r=[K("sm")], w=[K("sm")])
            ts("dve", mbf, tmp, wsum, None, ALU.mult, r=["tmp", K("sm")], w=["mbf"])
            if "route" in dbg:
                dbg_dump("gd%d" % i, mbf, [128, 256], BF16, ["mbf"], True)
            pv4 = ps[4][:].bitcast(BF16)
            for ec in range(2):
                tr(pv4[:, ec * 128:(ec + 1) * 128], mbf[:, ec * 128:(ec + 1) * 128], ident_b[:], r=["mbf", "ident_b"], w=[PS[4]])
            for ec in range(2):
                evac_bf("dve", gatesT[:, ec, tok], pv4[:, ec * 128:(ec + 1) * 128], r=[PS[4]], w=["gatesT"])
            dma("sp", h2T_d[:, :, tok], h2T_, r=[K("h2T")], w=[("h2T_d", i)])
            for c in range(8):
                mm(ps[5][:], h2T_[:, c, :], wgu[:, c, :], c == 0, c == 7, r=[K("h2T"), "wgu0", "wgu1"], w=[PS[5]])
            act(sgs, ps[5][:, 0:256], AF.Sigmoid, r=[PS[5]], w=["sgs"])
            tt("dve", sgs, sgs, ps[5][:, 0:256], ALU.mult, r=["sgs", PS[5]], w=["sgs"])
            tt("dve", ash, sgs, ps[5][:, 256:512], ALU.mult, r=["sgs", PS[5]], w=["ash"])
            pv3 = ps[3][:].bitcast(BF16)
            for fk in range(2):
                tr(pv3[:, 512 + fk * 128:512 + (fk + 1) * 128], ash[:, fk * 128:(fk + 1) * 128], ident_b[:], r=["ash", "ident_b"], w=[PS[3]])
            evac_bf("dve", ashT.rearrange("p a b -> p (a b)"), pv3[:, 512:768], r=[PS[3]], w=["ashT"])
            for nb in range(2):
                for fk in range(2):
                    mm(ps[6 + nb][:], ashT[:, fk, :], wds[:, fk, nb * 512:(nb + 1) * 512], fk == 0, fk == 1, r=["ashT", "wds"], w=[PS[6 + nb]])
                nbs = slice(nb * 512, (nb + 1) * 512)
                tt("dve", x_t[:, nbs], ps[6 + nb][:], bct["g2"][:, nbs], ALU.mult, r=[PS[6 + nb], "bc_g2", K("xt2")], w=[K("xt2")])
            tt("pool", x1, x1, x_t, ALU.add, r=[K("x1"), K("xt2")], w=[K("x1")])
            dma("sp", x1p_d[tok, :], x1, r=[K("x1")], w=[("x1p_d", i)])

        for i in range(NT):
            phaseF_tile(i)
        S.barrier()
        if stage <= 6:
            S.emit(); return nc

        NEXP = int(os.environ.get("KNE", str(NE)))
        h2Th = carve(0, [128, 8, 2048], BF16)
        wring = [(carve(32 + 12 * r, [128, 8, 256], BF16), carve(36 + 12 * r, [128, 8, 256], BF16), carve(40 + 12 * r, [128, 2, 1024], BF16)) for r in range(3)]
        selb = [carve(68 + 0.25 * i, [128, 128], BF16) for i in range(2)]
        ab_ = [carve(69 + 2 * i, [128, 2, 512], BF16) for i in range(2)]
        accT = carve(100, [128, 8, 2048])
        sgb_ = [carve(164 + 4 * i, [128, 2, 512]) for i in range(2)]
        t2b_ = [carve(172 + 4 * i, [128, 2, 512]) for i in range(2)]
        gsb_ = [carve(180 + i, [128, 512], BF16) for i in range(2)]
        xo = [carve(182 + 4 * i, [128, 1024]) for i in range(2)]
        wge = I["w_gate_e"]; wue = I["w_up_e"]; wde = I["w_down_e"]
        gstep = 0
        for hh in range(2):
            for c in range(8):
                dma("sp", h2Th[:, c, :], h2T_d[:, c, hh * 2048:(hh + 1) * 2048], w=[("h2Th", c)])
            H2 = [("h2Th", c) for c in range(8)]
            for e0 in range(0, NEXP, 2):
                pair = []
                for q in range(2):
                    e = e0 + q
                    r = (hh * NEXP + e) % 3
                    wg, wu, wd = wring[r]
                    dma("pool", wg, wge[e].rearrange("(ko p) n -> p ko n", p=128), w=[("wg", r)])
                    dma("pool", wu, wue[e].rearrange("(ko p) n -> p ko n", p=128), w=[("wu", r)])
                    dma("pool", wd, wde[e].rearrange("(ko p) n -> p ko n", p=128), w=[("wd", r)])
                    ej = e % 128
                    sl = selb[q]
                    evac_bf("dve", sl, ident_b[:, ej:ej + 1].to_broadcast([128, 128]), r=["ident_b"], w=[("sel", q)])
                    pair.append((e, r, wg, wu, wd, sl))
                for tb in range(4):
                    tbs = slice(tb * 512, (tb + 1) * 512)
                    gts = slice(hh * 2048 + tb * 512, hh * 2048 + (tb + 1) * 512)
                    for q in range(2):
                        e, r, wg, wu, wd, sl = pair[q]
                        ec = e // 128
                        u_ = q
                        sg, a_, gs, t2 = sgb_[u_], ab_[u_], gsb_[u_], t2b_[u_]
                        for fk in range(2):
                            for c in range(8):
                                mm(ps[fk][:], wg[:, c, fk * 128:(fk + 1) * 128], h2Th[:, c, tbs], c == 0, c == 7, r=[("wg", r)] + H2, w=[PS[fk]])
                        for fk in range(2):
                            for c in range(8):
                                mm(ps[2 + fk][:], wu[:, c, fk * 128:(fk + 1) * 128], h2Th[:, c, tbs], c == 0, c == 7, r=[("wu", r)] + H2, w=[PS[2 + fk]])
                        mm(ps[4][:], sl, gatesT[:, ec, gts], True, True, r=[("sel", q), "gatesT"], w=[PS[4]])
                        for fk in range(2):
                            act(sg[:, fk, :], ps[fk][:], AF.Silu, r=[PS[fk]], w=[("sg", u_)])
                        for fk in range(2):
                            tt("dve", t2[:, fk, :], sg[:, fk, :], ps[2 + fk][:], ALU.mult, r=[("sg", u_), PS[2 + fk]], w=[("t2", u_)])
                        act(gs, ps[4][:], AF.Copy, r=[PS[4]], w=[("gs", u_)])
                        for fk in range(2):
                            tt("pool", a_[:, fk, :], t2[:, fk, :], gs, ALU.mult, r=[("t2", u_), ("gs", u_)], w=[("a", u_)])
                    for dc in range(8):
                        bD = 5 + dc % 3
                        k = 0
                        for q in range(2):
                            for fk in range(2):
                                mm(ps[bD][:], pair[q][4][:, fk, dc * 128:(dc + 1) * 128], ab_[q][:, fk, :], k == 0, k == 3,
                                   r=[("wd", pair[q][1]), ("a", q)], w=[PS[bD]])
                                k += 1
                        if e0 == 0:
                            if dc % 2 == 0:
                                cp("dve", accT[:, dc, tbs], ps[bD][:], r=[PS[bD]], w=[("acc", dc, tb)])
                            else:
                                act(accT[:, dc, tbs], ps[bD][:], AF.Copy, r=[PS[bD]], w=[("acc", dc, tb)])
                        else:
                            tt("dve", accT[:, dc, tbs], accT[:, dc, tbs], ps[bD][:], ALU.add, r=[PS[bD], ("acc", dc, tb)], w=[("acc", dc, tb)])
            for dc in range(8):
                act(accT[:, dc, :], accT[:, dc, :], AF.Copy, r=[("acc", dc, tb) for tb in range(4)], w=[("acc", dc, tb) for tb in range(4)], scale=mod[:, 40 + dc:41 + dc])
            for tl in range(16):
                i = hh * 16 + tl
                tok = slice(i * 128, (i + 1) * 128)
                xo_ = xo[i % 2]
                dma("sp", xo_, x1p_d[tok, :], w=[("xo", i % 2)])
                for dc in range(8):
                    nb = dc // 4
                    tr(ps[nb][:, (dc % 4) * 128:(dc % 4 + 1) * 128], accT[:, dc, tl * 128:(tl + 1) * 128], ident_f[:],
                       r=[("acc", dc, tl // 4), "ident_f"], w=[PS[nb]])
                for nb in range(2):
                    nbs = slice(nb * 512, (nb + 1) * 512)
                    tt("dve", xo_[:, nbs], xo_[:, nbs], ps[nb][:], ALU.add, r=[("xo", i % 2), PS[nb]], w=[("xo", i % 2)])
                dma("sp", out[tok, :], xo_, r=[("xo", i % 2)], w=[("out", i)])
        S.barrier()
        S.emit()
    return nc


def host_consts():
    ident = np.eye(128, dtype=np.float32)
    inv_freq = (1.0 / (10000.0 ** (np.arange(0, 64, 2, dtype=np.float32) / 64.0))).astype(np.float32)
    ang = np.arange(T, dtype=np.float32)[:, None] * inv_freq[None, :]
    cs = np.concatenate([np.cos(ang), np.sin(ang)], axis=1).astype(np.float32)
    ropecs = np.ascontiguousarray(cs.reshape(NT, 128, 64).transpose(1, 0, 2))
    tri = np.triu(np.ones((128, 128), np.float32)).astype(ml_dtypes.bfloat16)
    tl = np.arange(4)[:, None, None, None, None, None]; t4 = np.arange(4)[None, None, None, :, None, None]
    hc_r = np.arange(32)[None, :, None, None, None, None]
    tau = np.arange(16)[None, None, None, None, :, None]; hc_c = np.arange(32)[None, None, None, None, None, :]
    causal = np.broadcast_to((tau >= 4 * t4 + tl), (4, 32, 1, 4, 16, 32)).reshape(128, 4, 512)
    eye = np.broadcast_to((tau == 4 * t4 + tl) & (hc_r == hc_c), (4, 32, 1, 4, 16, 32)).reshape(128, 4, 512)
    iota_e = np.ascontiguousarray(np.broadcast_to(np.arange(256, dtype=np.float32)[None, :], (128, 256)))
    triu = np.triu(np.ones((128, 128), np.float32), 1).astype(ml_dtypes.bfloat16)
    return dict(ident_f=ident, ident_b=ident.astype(ml_dtypes.bfloat16), ones_f=np.ones((128, 128), np.float32), ropecs=ropecs, tri=tri,

                w3mask=np.ascontiguousarray(causal.astype(np.float32)).astype(ml_dtypes.bfloat16),
                w3eye=np.ascontiguousarray(eye.astype(np.float32)).astype(ml_dtypes.bfloat16))


def make_in_maps(inp, cores):
    cst = host_consts()
    maps = []
    f = lambda a: np.ascontiguousarray(np.asarray(a, dtype=np.float32))
    gqk = np.concatenate([f(inp["q_norm_g"])[0], f(inp["k_norm_g"])[0]])
    gqk = np.ascontiguousarray(np.broadcast_to(gqk[None, :], (128, 128)))
    lamv = np.concatenate([f(inp[k])[0] for k in ("lambda_q1", "lambda_k1", "lambda_q2", "lambda_k2")])
    lamv = np.ascontiguousarray(np.broadcast_to(lamv[None, :], (128, 256)))
    gsub = np.ascontiguousarray(np.broadcast_to(f(inp["subln_g"])[0][None, :], (128, 128)))
    sp = lambda a: np.ascontiguousarray(a.reshape(16, 128).T)
    ssm_cols = np.ascontiguousarray(np.stack([sp(f(inp["ssm_a_re"])[0]), sp(f(inp["ssm_a_im"])[0]),
                                              sp(np.repeat(f(inp["ssm_log_dt"])[0][:, None], 64, 1))], 1))
    bl = lambda a: a.reshape(16, 128, 16).transpose(1, 0, 2)
    ssm_b = np.ascontiguousarray(np.stack([bl(f(inp["ssm_b_re"])[0]), bl(f(inp["ssm_b_im"])[0])], 1))
    cl = lambda a: a.reshape(16, 2, 16, 64).transpose(1, 3, 0, 2).reshape(128, 16, 16)
    ssm_c = np.ascontiguousarray(np.stack([cl(f(inp["ssm_c_re"])[0]), cl(f(inp["ssm_c_im"])[0])], 1))
    ssm_dcol = np.ascontiguousarray(np.tile(f(inp["ssm_d"])[0].reshape(16, 32), (1, 4)).T)
    rbias = np.ascontiguousarray(np.broadcast_to(f(inp["router_bias"])[0][None, :], (128, 256)))
    for b in cores:
        vs1 = np.zeros((128, 128), np.float32)
        vs1[0:8] = f(inp["c"])[b].reshape(8, 128)
        vs1[8:16] = f(inp["norm1_g"])[0].reshape(8, 128)
        vs1[16:24] = f(inp["norm2_g"])[0].reshape(8, 128)
        vs1[24:72] = f(inp["b_ada"])[0].reshape(48, 128)
        vs1[72:76] = f(inp["ssm_norm_g"])[0].reshape(4, 128)
        m = dict(x=f(inp["x"])[b], vs1=vs1, w_ada=f(inp["w_ada"])[0], w_in=f(inp["w_in"])[0], w_glu=f(inp["w_glu"])[0], gqk=gqk, w_out=f(inp["w_out"])[0], w_router=f(inp["w_router"])[0],
                 w_gate_s=f(inp["w_gate_s"])[0], w_up_s=f(inp["w_up_s"])[0], w_down_s=f(inp["w_down_s"])[0], rbias=rbias, lamv=lamv, gsub=gsub, ssm_cols=ssm_cols, ssm_b=ssm_b, ssm_c=ssm_c, ssm_dcol=ssm_dcol)
        if "w_gate_e" in inp:
            m.update(w_gate_e=f(inp["w_gate_e"])[0], w_up_e=f(inp["w_up_e"])[0], w_down_e=f(inp["w_down_e"])[0])
        m.update(cst)
        maps.append(m)
    return maps


def kernel(**inputs):
    nc = build()
    maps = make_in_maps(inputs, list(range(8)))
    res = run_bass_kernel_spmd(nc, maps, core_ids=list(range(8)))
    return np.stack([r["out"] for r in res.results], axis=0)
```
